# Optimizing a Trainium2 kernel written in Bass

```python
import jax, jax.numpy as jnp
from jax import lax
import numpy as np

D_MODEL = 2048
BATCH = 8
SEQ = 2048
DEPTH = 1

NSA_HEADS = 16
NSA_HEAD_DIM = 64
NSA_KV_GROUPS = 4
NSA_WIDTH = NSA_HEADS * NSA_HEAD_DIM
NSA_KV_WIDTH = NSA_KV_GROUPS * NSA_HEAD_DIM
CMP_LEN = 32
CMP_STRIDE = 16
SEL_LEN = 64
N_SEL = 8
WINDOW = 512
Q_BLOCK = 64
LRU_WIDTH = D_MODEL - NSA_WIDTH
LRU_BLOCKS = 16
LRU_BLOCK_W = LRU_WIDTH // LRU_BLOCKS
CONV_W = 4
RG_C = 8.0
N_EXPERTS = 32
TOP_K = 4
D_FF_EXPERT = D_MODEL
SWIGLU_LIMIT = 7.0
SWIGLU_ALPHA = 1.702
MOE_BLOCK = 256
NORM_EPS = 1e-6
NEG_INF = -1e30
SEL_FORCE = 1e30
IN_WIDTH = NSA_WIDTH + 6 * NSA_KV_WIDTH + 3 * NSA_HEADS + 2 * LRU_WIDTH

kernel_name = 'hymba_nsa_rglru_moe_adaln_layer'


def rms_norm(x, g):
    xf = x.astype(jnp.float32)
    y = xf * lax.rsqrt(jnp.mean(xf * xf, axis=-1, keepdims=True) + NORM_EPS)
    return (y * g.astype(jnp.float32)).astype(x.dtype)


def masked_softmax(s, mask):
    return jax.nn.softmax(jnp.where(mask, s, NEG_INF), axis=-1) * mask


def compress_blocks(k, pe, w):
    B, S, G, HD = k.shape
    n_sub = CMP_LEN // CMP_STRIDE
    sub = k.reshape(B, S // CMP_STRIDE, CMP_STRIDE, G, HD)
    n_cmp = S // CMP_STRIDE - n_sub + 1
    blocks = jnp.concatenate([sub[:, j:j + n_cmp] for j in range(n_sub)], axis=2)
    blocks = blocks + pe[None, None, :, None, :]
    flat = blocks.transpose(0, 1, 3, 2, 4).reshape(B, n_cmp, G, CMP_LEN * HD)
    return flat @ w


def nsa_mixer(q, k_c, v_c, k_s, v_s, k_w, v_w, gate_logits, pe_k, pe_v, w_ck, w_cv, q_gain, k_gain):
    B, S, H, HD = q.shape
    G = NSA_KV_GROUPS
    HG = H // G
    dt = q.dtype
    scale = NSA_HEAD_DIM ** -0.5
    qg = rms_norm(q, q_gain).reshape(B, S, G, HG, HD)
    t_all = jnp.arange(S)

    kc = rms_norm(compress_blocks(k_c, pe_k, w_ck), k_gain[0])
    vc = compress_blocks(v_c, pe_v, w_cv)
    n_cmp = kc.shape[1]
    cmp_start = jnp.arange(n_cmp) * CMP_STRIDE
    m_c = (cmp_start + CMP_LEN - 1)[None, :] <= t_all[:, None]
    s_c = jnp.einsum('bsghd,bcgd->bghsc', qg, kc).astype(jnp.float32) * scale
    p_c = masked_softmax(s_c, m_c)
    o_c = jnp.einsum('bghsc,bcgd->bsghd', p_c.astype(dt), vc)

    n_blk = S // SEL_LEN
    n_sel = min(N_SEL, n_blk)
    sel_start = jnp.arange(n_blk) * SEL_LEN
    overlap = ((cmp_start[:, None] < sel_start[None, :] + SEL_LEN)
               & (cmp_start[:, None] + CMP_LEN > sel_start[None, :])).astype(jnp.float32)
    imp = jnp.einsum('bghsc,cj->bgsj', p_c, overlap)
    q_blk = t_all // SEL_LEN
    j = jnp.arange(n_blk)
    valid = j[None, :] <= q_blk[:, None]
    forced = (j[None, :] == 0) | (j[None, :] == q_blk[:, None]) | (j[None, :] == q_blk[:, None] - 1)
    imp = jnp.where(forced, SEL_FORCE, jnp.where(valid, imp, -SEL_FORCE))
    _, sel_idx = lax.top_k(imp, n_sel)

    k_blocks = rms_norm(k_s, k_gain[1]).reshape(B, n_blk, SEL_LEN, G, HD).transpose(0, 3, 1, 2, 4)
    v_blocks = v_s.reshape(B, n_blk, SEL_LEN, G, HD).transpose(0, 3, 1, 2, 4)
    pad = ((0, 0), (WINDOW, 0), (0, 0), (0, 0))
    k_wp = jnp.pad(rms_norm(k_w, k_gain[2]), pad)
    v_wp = jnp.pad(v_w, pad)
    b_ix = jnp.arange(B)[:, None, None, None]
    g_ix = jnp.arange(G)[None, :, None, None]
    n_keys = n_sel * SEL_LEN

    def query_block(qs):
        qc = lax.dynamic_slice_in_dim(qg, qs, Q_BLOCK, axis=1)
        t = qs + jnp.arange(Q_BLOCK)
        ic = lax.dynamic_slice_in_dim(sel_idx, qs, Q_BLOCK, axis=2)
        kg = k_blocks[b_ix, g_ix, ic].reshape(B, G, Q_BLOCK, n_keys, HD)
        vg = v_blocks[b_ix, g_ix, ic].reshape(B, G, Q_BLOCK, n_keys, HD)
        pos = (ic[..., None] * SEL_LEN + jnp.arange(SEL_LEN)).reshape(B, G, Q_BLOCK, n_keys)
        m_s = (pos <= t[None, None, :, None])[:, :, None]
        s_s = jnp.einsum('bqghd,bgqkd->bghqk', qc, kg).astype(jnp.float32) * scale
        o_s = jnp.einsum('bghqk,bgqkd->bqghd', masked_softmax(s_s, m_s).astype(dt), vg)
        kwc = lax.dynamic_slice_in_dim(k_wp, qs, WINDOW + Q_BLOCK, axis=1)
        vwc = lax.dynamic_slice_in_dim(v_wp, qs, WINDOW + Q_BLOCK, axis=1)
        kpos = qs - WINDOW + jnp.arange(WINDOW + Q_BLOCK)
        m_w = ((kpos[None, :] <= t[:, None]) & (kpos[None, :] > t[:, None] - WINDOW)
               & (kpos[None, :] >= 0))
        s_w = jnp.einsum('bqghd,bkgd->bghqk', qc, kwc).astype(jnp.float32) * scale
        o_w = jnp.einsum('bghqk,bkgd->bqghd', masked_softmax(s_w, m_w).astype(dt), vwc)
        return o_s, o_w

    o_s, o_w = lax.map(query_block, jnp.arange(S // Q_BLOCK) * Q_BLOCK)
    o_s = jnp.moveaxis(o_s, 0, 1).reshape(B, S, G, HG, HD)
    o_w = jnp.moveaxis(o_w, 0, 1).reshape(B, S, G, HG, HD)
    g = jax.nn.sigmoid(gate_logits.astype(jnp.float32)).astype(dt).reshape(B, S, G, HG, 3)
    o = g[..., 0:1] * o_c + g[..., 1:2] * o_s + g[..., 2:3] * o_w
    return o.reshape(B, S, H * HD)


def _linear_combine(e1, e2):
    a1, b1 = e1
    a2, b2 = e2
    return a1 * a2, a2 * b1 + b2


def rglru_mixer(xr, xg, conv_w, conv_b, w_rg, b_rg, w_ig, b_ig, lam):
    B, S, W = xr.shape
    xp = jnp.pad(xr, ((0, 0), (CONV_W - 1, 0), (0, 0)))
    xc = conv_b + sum(xp[:, k:k + S] * conv_w[k] for k in range(CONV_W))
    xb = xc.reshape(B, S, LRU_BLOCKS, LRU_BLOCK_W)
    r = jax.nn.sigmoid(jnp.einsum('bsnc,ncd->bsnd', xb, w_rg) + b_rg).reshape(B, S, W)
    i = jax.nn.sigmoid(jnp.einsum('bsnc,ncd->bsnd', xb, w_ig) + b_ig).reshape(B, S, W)
    log_a = (-RG_C * jax.nn.softplus(-lam.astype(jnp.float32))) * r.astype(jnp.float32)
    a = jnp.exp(log_a)
    u = jnp.sqrt(-jnp.expm1(2.0 * log_a)) * (i * xc).astype(jnp.float32)
    _, h = lax.associative_scan(_linear_combine, (a, u), axis=1)
    return h.astype(xr.dtype) * jax.nn.gelu(xg)


def moe_ffn(h, w_router, b_router, w1, b1, w2, b2):
    B, S, D = h.shape
    N = B * S
    A = N * TOP_K
    hf = h.reshape(N, D)
    logits = (hf @ w_router + b_router).astype(jnp.float32)
    top_v, top_e = lax.top_k(logits, TOP_K)
    gates = jax.nn.softmax(top_v, axis=-1)
    e_flat = top_e.reshape(A).astype(jnp.int32)
    tok_flat = jnp.arange(A, dtype=jnp.int32) // TOP_K
    g_flat = gates.reshape(A)
    order = jnp.argsort(e_flat)
    e_s, tok_s, g_s = e_flat[order], tok_flat[order], g_flat[order]
    counts = jnp.bincount(e_flat, length=N_EXPERTS)
    starts = jnp.cumsum(counts) - counts
    pcounts = (counts + MOE_BLOCK - 1) // MOE_BLOCK * MOE_BLOCK
    pends = jnp.cumsum(pcounts)
    pstarts = pends - pcounts
    dest = pstarts[e_s] + jnp.arange(A, dtype=jnp.int32) - starts[e_s]
    n_blocks = (A + N_EXPERTS * (MOE_BLOCK - 1) + MOE_BLOCK - 1) // MOE_BLOCK
    P = n_blocks * MOE_BLOCK
    buf_tok = jnp.zeros((P,), jnp.int32).at[dest].set(tok_s)
    buf_g = jnp.zeros((P,), jnp.float32).at[dest].set(g_s)
    blk_e = jnp.minimum(jnp.searchsorted(pends, jnp.arange(n_blocks) * MOE_BLOCK, side='right'),
                        N_EXPERTS - 1)

    def expert_block(args):
        e, tok, g = args
        xb = hf[tok]
        u = xb @ w1[e] + b1[e]
        u_glu, u_lin = jnp.split(u, 2, axis=-1)
        u_glu = jnp.minimum(u_glu, SWIGLU_LIMIT)
        u_lin = jnp.clip(u_lin, -SWIGLU_LIMIT, SWIGLU_LIMIT)
        act = u_glu * jax.nn.sigmoid(SWIGLU_ALPHA * u_glu) * (u_lin + 1)
        y = act @ w2[e] + b2[e]
        return y * g[:, None].astype(y.dtype)

    yb = lax.map(expert_block, (blk_e, buf_tok.reshape(n_blocks, MOE_BLOCK),
                                buf_g.reshape(n_blocks, MOE_BLOCK)))
    out = jnp.zeros((N, D), yb.dtype).at[buf_tok].add(yb.reshape(P, D))
    return out.reshape(B, S, D).astype(h.dtype)


def hybrid_layer(x, mod, g_norm1, w_in, pe_cmp_k, pe_cmp_v, w_cmp_k, w_cmp_v, q_gain, k_gain,
                 conv_w, conv_b, w_rg, b_rg, w_ig, b_ig, lru_lambda, g_out_nsa, g_out_lru,
                 w_out, g_norm2, w_router, b_router, w_e1, b_e1, w_e2, b_e2):
    B, S, D = x.shape
    shift1, scale1, gate1, shift2, scale2, gate2 = [m[:, None, :] for m in jnp.split(mod, 6, axis=-1)]
    h = rms_norm(x, g_norm1) * (1 + scale1) + shift1
    z = h @ w_in
    sizes = [NSA_WIDTH] + [NSA_KV_WIDTH] * 6 + [3 * NSA_HEADS, LRU_WIDTH, LRU_WIDTH]
    q, k_c, v_c, k_s, v_s, k_w, v_w, gl, xr, xg = jnp.split(z, np.cumsum(sizes)[:-1].tolist(), axis=-1)
    kv = lambda t: t.reshape(B, S, NSA_KV_GROUPS, NSA_HEAD_DIM)
    o_nsa = nsa_mixer(q.reshape(B, S, NSA_HEADS, NSA_HEAD_DIM), kv(k_c), kv(v_c), kv(k_s), kv(v_s),
                      kv(k_w), kv(v_w), gl, pe_cmp_k, pe_cmp_v, w_cmp_k, w_cmp_v, q_gain, k_gain)
    o_lru = rglru_mixer(xr, xg, conv_w, conv_b, w_rg, b_rg, w_ig, b_ig, lru_lambda)
    mix = jnp.concatenate([rms_norm(o_nsa, g_out_nsa), rms_norm(o_lru, g_out_lru)], axis=-1) @ w_out
    x = x + gate1 * mix
    h2 = rms_norm(x, g_norm2) * (1 + scale2) + shift2
    return x + gate2 * moe_ffn(h2, w_router, b_router, w_e1, b_e1, w_e2, b_e2)


def setup_inputs(seed: int = 0) -> dict:
    key = jax.random.key(seed)
    ks = jax.random.split(key, 30)
    L = DEPTH
    nrm = lambda k, shape, s: jax.random.normal(k, shape, jnp.float32) * s
    u = jax.random.uniform(ks[18], (L, LRU_WIDTH), jnp.float32, minval=0.9, maxval=0.999)
    a0 = u ** (1.0 / RG_C)
    return {
        'x': nrm(ks[0], (BATCH, SEQ, D_MODEL), 1.0),
        'c': nrm(ks[1], (BATCH, D_MODEL), 1.0),
        'w_ada': nrm(ks[2], (L, D_MODEL, 6 * D_MODEL), 0.5 * D_MODEL ** -0.5),
        'b_ada': nrm(ks[3], (L, 6 * D_MODEL), 0.02),
        'g_norm1': 1.0 + nrm(ks[4], (L, D_MODEL), 0.02),
        'w_in': nrm(ks[5], (L, D_MODEL, IN_WIDTH), D_MODEL ** -0.5),
        'pe_cmp_k': nrm(ks[6], (L, CMP_LEN, NSA_HEAD_DIM), 0.1),
        'pe_cmp_v': nrm(ks[7], (L, CMP_LEN, NSA_HEAD_DIM), 0.1),
        'w_cmp_k': nrm(ks[8], (L, CMP_LEN * NSA_HEAD_DIM, NSA_HEAD_DIM), (CMP_LEN * NSA_HEAD_DIM) ** -0.5),
        'w_cmp_v': nrm(ks[9], (L, CMP_LEN * NSA_HEAD_DIM, NSA_HEAD_DIM), (CMP_LEN * NSA_HEAD_DIM) ** -0.5),
        'q_gain': 1.0 + nrm(ks[10], (L, NSA_HEAD_DIM), 0.02),
        'k_gain': 1.0 + nrm(ks[11], (L, 3, NSA_HEAD_DIM), 0.02),
        'conv_w': nrm(ks[12], (L, CONV_W, LRU_WIDTH), CONV_W ** -0.5),
        'conv_b': nrm(ks[13], (L, LRU_WIDTH), 0.02),
        'w_rg': nrm(ks[14], (L, LRU_BLOCKS, LRU_BLOCK_W, LRU_BLOCK_W), LRU_BLOCK_W ** -0.5),
        'b_rg': nrm(ks[15], (L, LRU_BLOCKS, LRU_BLOCK_W), 0.02),
        'w_ig': nrm(ks[16], (L, LRU_BLOCKS, LRU_BLOCK_W, LRU_BLOCK_W), LRU_BLOCK_W ** -0.5),
        'b_ig': nrm(ks[17], (L, LRU_BLOCKS, LRU_BLOCK_W), 0.02),
        'lru_lambda': jnp.log(a0) - jnp.log1p(-a0),
        'g_out_nsa': 1.0 + nrm(ks[19], (L, NSA_WIDTH), 0.02),
        'g_out_lru': 1.0 + nrm(ks[20], (L, LRU_WIDTH), 0.02),
        'w_out': nrm(ks[21], (L, D_MODEL, D_MODEL), D_MODEL ** -0.5),
        'g_norm2': 1.0 + nrm(ks[22], (L, D_MODEL), 0.02),
        'w_router': nrm(ks[23], (L, D_MODEL, N_EXPERTS), D_MODEL ** -0.5),
        'b_router': nrm(ks[24], (L, N_EXPERTS), 0.01),
        'w_e1': nrm(ks[25], (L, N_EXPERTS, D_MODEL, 2 * D_FF_EXPERT), D_MODEL ** -0.5),
        'b_e1': nrm(ks[26], (L, N_EXPERTS, 2 * D_FF_EXPERT), 0.02),
        'w_e2': nrm(ks[27], (L, N_EXPERTS, D_FF_EXPERT, D_MODEL), D_FF_EXPERT ** -0.5),
        'b_e2': nrm(ks[28], (L, N_EXPERTS, D_MODEL), 0.02),
    }


def reference(x, c, w_ada, b_ada, g_norm1, w_in, pe_cmp_k, pe_cmp_v, w_cmp_k, w_cmp_v, q_gain, k_gain,
              conv_w, conv_b, w_rg, b_rg, w_ig, b_ig, lru_lambda, g_out_nsa, g_out_lru, w_out,
              g_norm2, w_router, b_router, w_e1, b_e1, w_e2, b_e2):
    for l in range(DEPTH):
        mod = jax.nn.silu(c) @ w_ada[l] + b_ada[l]
        x = hybrid_layer(x, mod, g_norm1[l], w_in[l], pe_cmp_k[l], pe_cmp_v[l], w_cmp_k[l], w_cmp_v[l],
                         q_gain[l], k_gain[l], conv_w[l], conv_b[l], w_rg[l], b_rg[l], w_ig[l], b_ig[l],
                         lru_lambda[l], g_out_nsa[l], g_out_lru[l], w_out[l], g_norm2[l],
                         w_router[l], b_router[l], w_e1[l], b_e1[l], w_e2[l], b_e2[l])
    return x
```

```python
from contextlib import ExitStack, contextmanager

import numpy as np
import concourse.bass as bass
import concourse.mybir as mybir
from concourse.bass_utils import run_bass_kernel_spmd

F32 = mybir.dt.float32
F32R = mybir.dt.float32r
BF16 = mybir.dt.bfloat16
I32 = mybir.dt.int32
AF = mybir.ActivationFunctionType
ALU = mybir.AluOpType

S_LEN = 2048
D = 2048
NT = 16
NE = 32
CAPW = 512
NW = 2
CAP = CAPW * NW
NST = CAPW // 128
EPS = 1e-6
NEG = -30000.0
WCOL = 256


class Buf:
    __slots__ = ("name", "w", "r")

    def __init__(self, name=""):
        self.name = name
        self.w = None
        self.r = {}


class Sched:
    def __init__(self, nc, stack, n_dma_slots=12):
        self.nc = nc
        self.eng = {"pe": nc.tensor, "act": nc.scalar, "dve": nc.vector, "pool": nc.gpsimd, "sp": nc.sync}
        self.sem = {k: stack.enter_context(nc.semaphore("s_" + k)) for k in self.eng}
        self.cnt = {k: 0 for k in self.eng}
        self.waited = {k: {} for k in self.eng}
        self.slots = {}
        for q in ("sp", "pool"):
            self.slots[q] = [[stack.enter_context(nc.semaphore(f"d_{q}{i}")), 0] for i in range(n_dma_slots)]
        self.slot_i = {q: 0 for q in self.slots}
        self.nwaits = 0

    def _wait(self, e, ev):
        if ev is None:
            return
        sem, val = ev
        if e == "pe" and sem is self.sem["pe"]:
            return
        key = id(sem)
        if self.waited[e].get(key, 0) >= val:
            return
        self.eng[e].wait_ge(sem, val)
        self.waited[e][key] = val
        self.nwaits += 1

    def _deps(self, e, reads, writes):
        for b in reads:
            self._wait(e, b.w)
        for b in writes:
            self._wait(e, b.w)
            for ev in b.r.values():
                self._wait(e, ev)

    def _mark(self, ev, reads, writes):
        sem, val = ev
        for b in reads:
            b.r[id(sem)] = ev
        for b in writes:
            b.w = ev
            b.r = {}

    def op(self, e, fn, reads=(), writes=()):
        self._deps(e, reads, writes)
        ins = fn(self.eng[e])
        self.cnt[e] += 1
        ins.then_inc(self.sem[e], 1)
        ev = (self.sem[e], self.cnt[e])
        self._mark(ev, reads, writes)
        return ev

    def dma(self, q, out, in_, reads=(), writes=(), fn=None):
        slots = self.slots[q]
        i = self.slot_i[q]
        self.slot_i[q] = (i + 1) % len(slots)
        sem, uses = slots[i]
        if uses:
            self._wait(q, (sem, 16 * uses))
        self._deps(q, reads, writes)
        if fn is None:
            ins = self.eng[q].dma_start(out=out, in_=in_)
        else:
            ins = fn(self.eng[q])
        ins.then_inc(sem, 16)
        slots[i][1] = uses + 1
        ev = (sem, 16 * (uses + 1))
        self._mark(ev, reads, writes)
        return ev

    def barrier(self):
        evs = [(self.sem[k], self.cnt[k]) for k in self.eng if self.cnt[k] > 0]
        for q in self.slots:
            for sem, uses in self.slots[q]:
                if uses:
                    evs.append((sem, 16 * uses))
        for e in self.eng:
            for ev in evs:
                self._wait(e, ev)

    def finish(self, bufs):
        for b in bufs:
            self._wait("sp", b.w)
            for ev in b.r.values():
                self._wait("sp", ev)


class Ring:
    def __init__(self, items):
        self.items = items
        self.i = 0

    def next(self):
        it = self.items[self.i]
        self.i = (self.i + 1) % len(self.items)
        return it


def r32(ap):
    return ap.bitcast(F32R)


def f32(ap):
    return ap.bitcast(F32)


def build_nc(upto="all", dbg=False):
    nc = bass.Bass("TRN2", target_bir_lowering=False)
    nc.dge_precook = False
    order = ["mod", "inproj", "lru", "nsa", "nsaout", "outproj", "router", "experts", "combine", "all"]
    lvl = order.index(upto)

    def din(n, shp, dt=F32):
        return nc.dram_tensor(n, list(shp), dt, kind="ExternalInput").ap()

    dbg_sets = {"mod": ["mod_s"], "inproj": ["zT_s", "ztm_s"], "lru": ["onT_s"], "nsa": ["onsa_s"], "nsaout": ["onT_s"],
                "outproj": ["x1_s"], "router": ["rinfo_s"], "experts": [], "combine": [], "all": []}

    def dscr(n, shp, dt=F32):
        ext = dbg and n in dbg_sets[upto]
        return nc.dram_tensor(n, list(shp), dt, kind=("ExternalOutput" if ext else "Internal")).ap()

    x_d = din("x", [S_LEN, D])
    csil_d = din("csil", [128, 16])
    wada_d = din("w_ada", [D, 6 * D])
    bada_d = din("b_ada", [1, 6 * D])
    g1c_d = din("g1c", [128, 16])
    wA_d = din("wA", [D, 4096])
    wB_d = din("wB", [D, 768])
    wck_d = din("wck", [128, 32, 128])
    wcv_d = din("wcv", [128, 32, 64])
    pek2_d = din("pek2", [128, 32, 2])
    pevT_d = din("pevT", [128, 32])
    qg_d = din("qg", [128, 1])
    kg_d = din("kg", [128, 3])
    cw_d = din("cw", [128, 8, 4])
    cb_d = din("cb", [128, 8])
    wrg_d = din("wrg", [128, 8, 128])
    wig_d = din("wig", [128, 8, 128])
    brg_d = din("brg", [128, 8])
    big_d = din("big", [128, 8])
    lam_d = din("lam", [128, 8])
    gon_d = din("gon", [1, 1024])
    gol_d = din("gol", [128, 8])
    wout_d = din("w_out", [D, D])
    g2_d = din("g2", [1, D])
    wr_d = din("wr", [128, 16, NE])
    br_d = din("br", [1, NE])
    we1_d = din("w_e1", [NE, D, 2 * D]) if lvl >= 7 else None
    b1c_d = din("b1c", [128, NE, 32])
    we2_d = din("w_e2", [NE, D, D]) if lvl >= 7 else None
    be2_d = din("b_e2", [NE, D])
    ident_d = din("ident", [128, 128])
    bones_d = din("bones", [128, 128])
    cmask_d = din("cmask", [128, 4, 512])
    wmask_d = din("wmask", [128, 4, 512])
    ccm_d = din("ccm", [128, 4, 512])
    esel_d = din("esel", [32, 16, 128])
    selv_d = din("selv", [128, 16, 32])
    self_d = din("self", [128, 16, 32])
    triu_d = din("triu", [128, 128])
    iotas_d = din("iotas", [128, CAP])
    ebase_d = din("ebase", [128, NE])
    ones_d = din("ones", [128, 128])
    vaug0_d = din("vaug0", [128, NT, 2, 66])
    vcaug0_d = din("vcaug0", [128, 98])

    out_d = nc.dram_tensor("out", [S_LEN, D], F32, kind="ExternalOutput").ap()
    mod_d = dscr("mod_s", [1, 6 * D])
    zT_d = dscr("zT_s", [4096, S_LEN])
    ztm_d = dscr("ztm_s", [S_LEN, 768])
    onsa_d = dscr("onsa_s", [S_LEN, 1024])
    onT_d = dscr("onT_s", [D, S_LEN])
    x1_d = dscr("x1_s", [S_LEN, D])
    Y_d = dscr("Y_s", [NE * CAP, D])
    rinfo_d = dscr("rinfo_s", [S_LEN, 8])

    b_mod = Buf("mod_d")
    b_zT = [Buf(f"zT{i}") for i in range(32)]
    b_ztm = [Buf(f"ztm{i}") for i in range(NT)]
    b_onsa = [[Buf() for _ in range(2)] for _ in range(NT)]
    b_onT = [Buf(f"onT{i}") for i in range(16)]
    b_x1 = [Buf(f"x1{i}") for i in range(NT)]
    b_Y = [[Buf() for _ in range(NST * NW)] for _ in range(NE)]
    b_out = [Buf(f"out{i}") for i in range(NT)]
    b_rinfo = Buf("rinfo")
    final_bufs = []

    with ExitStack() as top:
        S = Sched(nc, top)

        uniq = [0]

        def mk(stack):
            def sb(n, shp, dt=F32):
                uniq[0] += 1
                return stack.enter_context(nc.sbuf_tensor(f"sb{uniq[0]}_{n}", list(shp), dt))
            return sb

        sbt = mk(top)

        @contextmanager
        def phase():
            with ExitStack() as ph_:
                yield mk(ph_)
                S.barrier()
        P = [(top.enter_context(nc.psum_tensor(f"P{i}", [128, 512], F32)), Buf(f"P{i}")) for i in range(8)]

        ident = sbt("ident", [128, 128]); b_ident = Buf("ident")
        S.dma("sp", ident[:], ident_d[:, :], writes=[b_ident])
        identr = sbt("identr", [128, 128], F32R); b_identr = Buf("identr")
        S.dma("sp", identr[:], r32(ident_d[:, :]), writes=[b_identr])
        def mkwring(sb_, n=3):
            return Ring([(sb_(f"wch{i}", [128, 16, WCOL], F32R), Buf(f"wch{i}")) for i in range(n)])

        def wsrc(w2d, c):
            return r32(w2d[:, c * WCOL:(c + 1) * WCOL]).rearrange("(kt p) f -> p kt f", p=128)

        class WStream:
            def __init__(self, wring, srcs, ahead=2):
                self.wring = wring
                self.srcs = srcs
                self.loaded = []
                self.ahead = ahead

            def get(self, k):
                while len(self.loaded) <= min(k + self.ahead, len(self.srcs) - 1):
                    t, b = self.wring.next()
                    S.dma("sp", t[:], self.srcs[len(self.loaded)], writes=[b])
                    self.loaded.append((t, b))
                return self.loaded[k]

        with phase() as sb:
            cs = sb("cs", [128, 16]); b_cs = Buf()
            S.dma("sp", cs[:], csil_d[:, :], writes=[b_cs])
            scr = sb("scr", [128, 16], F32R); b_scr = Buf()
            S.op("act", lambda e: e.activation(out=scr[:], in_=cs[:], func=AF.Silu), reads=[b_cs], writes=[b_scr])
            barow = Ring([(sb(f"barow{i}", [1, WCOL]), Buf()) for i in range(3)])
            mrow = Ring([(sb(f"mrow{i}", [1, WCOL]), Buf()) for i in range(3)])
            ws = WStream(mkwring(sb), [wsrc(wada_d, c) for c in range(6 * D // WCOL)])
            for c in range(6 * D // WCOL):
                wt, bw = ws.get(c)
                bt, bb = barow.next()
                S.dma("sp", bt[:], bada_d[:, c * WCOL:(c + 1) * WCOL], writes=[bb])
                pt, bp = P[c % 2]
                for kt in range(16):
                    S.op("pe", lambda e: e.matmul(out=pt[0:1, 0:WCOL], lhsT=scr[:, kt:kt + 1], rhs=wt[:, kt, :],
                                                  start=(kt == 0), stop=(kt == 15)),
                         reads=[b_scr, bw], writes=[bp])
                mt, bm = mrow.next()
                S.op("dve", lambda e: e.tensor_tensor(out=mt[:], in0=pt[0:1, 0:WCOL], in1=bt[:], op=ALU.add),
                     reads=[bp, bb], writes=[bm])
                S.dma("sp", mod_d[:, c * WCOL:(c + 1) * WCOL], mt[:], reads=[bm], writes=[b_mod])
        if lvl == 0:
            final_bufs.append(b_mod)

        def mod_cols(stack_sb, name, off):
            t = stack_sb(name, [128, 16]); b = Buf()
            S.dma("sp", None, None, reads=[b_mod], writes=[b],
                  fn=lambda e: e.dma_start(out=t[:], in_=mod_d[:, off:off + D].rearrange("o (c p) -> p (o c)", p=128),
                                           allow_slow_non_contiguous=True))
            return t, b

        def bc_load(t_ap, row_ap, reads, b):
            S.dma("sp", t_ap, row_ap.partition_broadcast(128), reads=reads, writes=[b])

        if lvl >= 1:
            with phase() as sb:
                sh1, b_sh1 = mod_cols(sb, "sh1", 0)
                sc1, b_sc1 = mod_cols(sb, "sc1", D)
                g1c = sb("g1c", [128, 16]); b_g1c = Buf()
                S.dma("sp", g1c[:], g1c_d[:, :], writes=[b_g1c])
                A1c = sb("A1c", [128, 16]); b_A1c = Buf()
                S.op("dve", lambda e: e.scalar_tensor_tensor(out=A1c[:], in0=sc1[:], scalar=1.0, in1=g1c[:], op0=ALU.add, op1=ALU.mult),
                     reads=[b_sc1, b_g1c], writes=[b_A1c])
                hT = sb("hT", [128, 16, S_LEN], F32R)
                b_hT = [Buf(f"hT{i}") for i in range(NT)]
                tmp = ExitStack(); sb2 = mk(tmp)
                xring = Ring([(sb2(f"xin{i}", [128, D]), Buf()) for i in range(2)])
                xs = sb2("xs", [128, D]); b_xs = Buf()
                sqj = sb2("sqj", [128, D]); b_sqj = Buf()
                ss = sb2("ss", [128, 1]); b_ss = Buf()
                rs = sb2("rs", [128, 1]); b_rs = Buf()
                for tt in range(NT):
                    xt, bx = xring.next()
                    S.dma("sp", xt[:], x_d[tt * 128:(tt + 1) * 128, :], writes=[bx])
                    S.op("act", lambda e: e.activation(out=sqj[:], in_=xt[:], func=AF.Square, accum_out=ss[:]),
                         reads=[bx], writes=[b_sqj, b_ss])
                    S.op("dve", lambda e: e.tensor_scalar(out=rs[:], in0=ss[:], scalar1=1.0 / D, scalar2=EPS, op0=ALU.mult, op1=ALU.add),
                         reads=[b_ss], writes=[b_rs])
                    S.op("act", lambda e: e.activation(out=rs[:], in_=rs[:], func=AF.Sqrt), reads=[b_rs], writes=[b_rs])
                    S.op("dve", lambda e: e.reciprocal(out=rs[:], in_=rs[:]), reads=[b_rs], writes=[b_rs])
                    S.op("dve", lambda e: e.tensor_scalar(out=xs[:], in0=xt[:], scalar1=rs[:, 0:1], scalar2=None, op0=ALU.mult),
                         reads=[bx, b_rs], writes=[b_xs])
                    for g in range(4):
                        pt, bp = P[4 + g % 4]
                        for j in range(4):
                            dc = g * 4 + j
                            S.op("pe", lambda e: e.transpose(out=pt[:, j * 128:(j + 1) * 128], in_=xs[:, dc * 128:(dc + 1) * 128], identity=ident[:]),
                                 reads=[b_xs, b_ident], writes=[bp])
                        for j in range(4):
                            dc = g * 4 + j
                            eng = "dve" if j % 2 == 0 else "pool"
                            if eng == "pool":
                                S.op("act", lambda e: e.activation(out=hT[:, dc, tt * 128:(tt + 1) * 128], in_=pt[:, j * 128:(j + 1) * 128],
                                                                   func=AF.Identity, scale=A1c[:, dc:dc + 1], bias=sh1[:, dc:dc + 1]),
                                     reads=[bp, b_A1c, b_sh1], writes=[b_hT[tt]])
                            else:
                                S.op("dve", lambda e: e.tensor_scalar(out=hT[:, dc, tt * 128:(tt + 1) * 128], in0=pt[:, j * 128:(j + 1) * 128],
                                                                      scalar1=A1c[:, dc:dc + 1], scalar2=sh1[:, dc:dc + 1], op0=ALU.mult, op1=ALU.add),
                                     reads=[bp, b_A1c, b_sh1], writes=[b_hT[tt]])
                S.barrier()
                tmp.close()
                stg = Ring([(sb(f"stgA{i}", [128, 512]), Buf()) for i in range(4)])
                srcs = [wsrc(wA_d, c) for c in range(16)] + [wsrc(wB_d, c) for c in range(3)]
                ws = WStream(mkwring(sb), srcs)
                pi = 0
                for c in range(16):
                    wt, bw = ws.get(c)
                    for ft in range(2):
                        rowtile = c * 2 + ft
                        for tc in range(4):
                            pt, bp = P[pi % 4]; pi += 1
                            for kt in range(16):
                                S.op("pe", lambda e: e.matmul(out=pt[:], lhsT=wt[:, kt, ft * 128:(ft + 1) * 128], rhs=hT[:, kt, tc * 512:(tc + 1) * 512],
                                                              start=(kt == 0), stop=(kt == 15)),
                                     reads=[bw] + b_hT[tc * 4:(tc + 1) * 4], writes=[bp])
                            st_, bs = stg.next()
                            if pi % 2 == 0:
                                S.op("act", lambda e: e.copy(out=st_[:], in_=pt[:]), reads=[bp], writes=[bs])
                            else:
                                S.op("dve", lambda e: e.tensor_copy(out=st_[:], in_=pt[:]), reads=[bp], writes=[bs])
                            S.dma("sp", zT_d[rowtile * 128:(rowtile + 1) * 128, tc * 512:(tc + 1) * 512], st_[:], reads=[bs], writes=[b_zT[rowtile]])
                for cb_ in range(3):
                    wt, bw = ws.get(16 + cb_)
                    for tt in range(NT):
                        pt, bp = P[pi % 4]; pi += 1
                        for kt in range(16):
                            S.op("pe", lambda e: e.matmul(out=pt[:, 0:WCOL], lhsT=hT[:, kt, tt * 128:(tt + 1) * 128], rhs=wt[:, kt, :],
                                                          start=(kt == 0), stop=(kt == 15)),
                                 reads=[bw, b_hT[tt]], writes=[bp])
                        st_, bs = stg.next()
                        S.op("act", lambda e: e.copy(out=st_[:, 0:WCOL], in_=pt[:, 0:WCOL]), reads=[bp], writes=[bs])
                        S.dma("sp", ztm_d[tt * 128:(tt + 1) * 128, cb_ * WCOL:(cb_ + 1) * WCOL], st_[:, 0:WCOL], reads=[bs], writes=[b_ztm[tt]])
            if lvl == 1:
                final_bufs += b_zT + b_ztm

        if lvl >= 2:
            with phase() as sb:

                def ld(name, shp, src, dt=F32):
                    t = sb(name, shp, dt); b = Buf()
                    S.dma("sp", t[:], src, writes=[b])
                    return t, b
                cw, b_cw = ld("cw", [128, 8, 4], cw_d[:, :, :])
                cb, b_cb = ld("cb", [128, 8], cb_d[:, :])
                wrg, b_wrg = ld("wrg", [128, 8, 128], r32(wrg_d[:, :, :]), F32R)
                wig, b_wig = ld("wig", [128, 8, 128], r32(wig_d[:, :, :]), F32R)
                brg, b_brg = ld("brg", [128, 8], brg_d[:, :])
                big, b_big = ld("big", [128, 8], big_d[:, :])
                lam, b_lam = ld("lam", [128, 8], lam_d[:, :])
                gol, b_gol = ld("gol", [128, 8], gol_d[:, :])
                ones_r, b_ones = ld("ones_r", [128, 128], r32(ones_d[:, :]), F32R)
                clam = sb("clam", [128, 8]); b_clam = Buf()
                clam2 = sb("clam2", [128, 8]); b_clam2 = Buf()
                S.op("act", lambda e: e.activation(out=clam[:], in_=lam[:], func=AF.Sigmoid), reads=[b_lam], writes=[b_clam])
                S.op("act", lambda e: e.activation(out=clam[:], in_=clam[:], func=AF.Ln), reads=[b_clam], writes=[b_clam])
                S.op("dve", lambda e: e.tensor_scalar(out=clam2[:], in0=clam[:], scalar1=16.0, scalar2=None, op0=ALU.mult), reads=[b_clam], writes=[b_clam2])
                S.op("dve", lambda e: e.tensor_scalar(out=clam[:], in0=clam[:], scalar1=8.0, scalar2=None, op0=ALU.mult), reads=[b_clam, b_clam2], writes=[b_clam])
                olru = sb("olru", [128, 8, S_LEN]); b_olru = [Buf() for _ in range(8)]
                xrp_r = Ring([(sb(f"xrp{i}", [128, 3 + S_LEN]), Buf()) for i in range(2)])
                xg_r = Ring([(sb(f"xg{i}", [128, S_LEN]), Buf()) for i in range(2)])
                xc = sb("xc", [128, S_LEN], F32R); b_xc = Buf()
                xcacc = sb("xcacc", [128, S_LEN])
                rt = sb("rt", [128, S_LEN]); b_rt = Buf()
                it = sb("it", [128, S_LEN]); b_it = Buf()
                at = sb("at", [128, S_LEN]); b_at = Buf()
                ut = sb("ut", [128, S_LEN]); b_ut = Buf()
                ht = sb("ht", [128, S_LEN]); b_ht = Buf()
                gt = sb("gt", [128, S_LEN]); b_gt = Buf()
                osq = sb("osq", [128, S_LEN], F32R); b_osq = Buf()
                for ct in range(8):
                    xrp, bxr = xrp_r.next()
                    xg, bxg = xg_r.next()
                    S.op("pool", lambda e: e.memset(xrp[:, 0:3], 0.0), writes=[bxr])
                    S.dma("sp", xrp[:, 3:3 + S_LEN], zT_d[2048 + ct * 128:2048 + (ct + 1) * 128, :], reads=[b_zT[16 + ct]], writes=[bxr])
                    S.dma("sp", xg[:], zT_d[3072 + ct * 128:3072 + (ct + 1) * 128, :], reads=[b_zT[24 + ct]], writes=[bxg])
                    xcf = xcacc[:]
                    S.op("dve", lambda e: e.tensor_scalar(out=xcf, in0=xrp[:, 0:S_LEN], scalar1=cw[:, ct, 0:1], scalar2=cb[:, ct:ct + 1], op0=ALU.mult, op1=ALU.add),
                         reads=[bxr, b_cw, b_cb], writes=[b_xc])
                    for k in range(1, 4):
                        outap = xc[:] if k == 3 else xcf
                        S.op("dve", lambda e: e.scalar_tensor_tensor(out=outap, in0=xrp[:, k:k + S_LEN], scalar=cw[:, ct, k:k + 1], in1=xcf, op0=ALU.mult, op1=ALU.add),
                             reads=[bxr, b_cw, b_xc], writes=[b_xc])
                    xcf = f32(xc[:])
                    for (wg, bwg, bg, bbg, dst, bdst) in ((wrg, b_wrg, brg, b_brg, rt, b_rt), (wig, b_wig, big, b_big, it, b_it)):
                        for tc in range(4):
                            pt, bp = P[tc % 2]
                            S.op("pe", lambda e: e.matmul(out=pt[:], lhsT=wg[:, ct, :], rhs=xc[:, tc * 512:(tc + 1) * 512], start=True, stop=True),
                                 reads=[bwg, b_xc], writes=[bp])
                            S.op("act", lambda e: e.activation(out=dst[:, tc * 512:(tc + 1) * 512], in_=pt[:], func=AF.Sigmoid, bias=bg[:, ct:ct + 1]),
                                 reads=[bp, bbg], writes=[bdst])
                    S.op("act", lambda e: e.activation(out=at[:], in_=rt[:], func=AF.Exp, scale=clam[:, ct:ct + 1]), reads=[b_rt, b_clam], writes=[b_at])
                    S.op("act", lambda e: e.activation(out=ut[:], in_=rt[:], func=AF.Exp, scale=clam2[:, ct:ct + 1]), reads=[b_rt, b_clam2], writes=[b_ut])
                    S.op("act", lambda e: e.activation(out=ut[:], in_=ut[:], func=AF.Sqrt, scale=-1.0, bias=1.0), reads=[b_ut], writes=[b_ut])
                    S.op("dve", lambda e: e.tensor_tensor(out=ut[:], in0=ut[:], in1=it[:], op=ALU.mult), reads=[b_ut, b_it], writes=[b_ut])
                    S.op("dve", lambda e: e.tensor_tensor(out=ut[:], in0=ut[:], in1=xcf, op=ALU.mult), reads=[b_ut, b_xc], writes=[b_ut])
                    S.op("dve", lambda e: e.tensor_tensor_scan(out=ht[:], data0=at[:], data1=ut[:], initial=0.0, op0=ALU.mult, op1=ALU.add),
                         reads=[b_at, b_ut], writes=[b_ht])
                    S.op("act", lambda e: e.activation(out=gt[:], in_=xg[:], func=AF.Square), reads=[bxg], writes=[b_gt])
                    S.op("dve", lambda e: e.tensor_scalar(out=gt[:], in0=gt[:], scalar1=0.044715, scalar2=1.0, op0=ALU.mult, op1=ALU.add), reads=[b_gt], writes=[b_gt])
                    S.op("dve", lambda e: e.tensor_tensor(out=gt[:], in0=gt[:], in1=xg[:], op=ALU.mult), reads=[b_gt, bxg], writes=[b_gt])
                    S.op("act", lambda e: e.activation(out=gt[:], in_=gt[:], func=AF.Sigmoid, scale=1.5957691216057308), reads=[b_gt], writes=[b_gt])
                    S.op("dve", lambda e: e.tensor_tensor(out=gt[:], in0=gt[:], in1=xg[:], op=ALU.mult), reads=[b_gt, bxg], writes=[b_gt])
                    S.op("dve", lambda e: e.tensor_tensor(out=olru[:, ct, :], in0=gt[:], in1=ht[:], op=ALU.mult), reads=[b_gt, b_ht], writes=[b_olru[ct]])
                    S.op("act", lambda e: e.activation(out=osq[:], in_=olru[:, ct, :], func=AF.Square), reads=[b_olru[ct]], writes=[b_osq])
                    for tc in range(4):
                        pt, bp = P[4 + tc]
                        S.op("pe", lambda e: e.matmul(out=pt[:], lhsT=ones_r[:], rhs=osq[:, tc * 512:(tc + 1) * 512], start=(ct == 0), stop=(ct == 7)),
                             reads=[b_ones, b_osq], writes=[bp])
                rb = rt; b_rb = b_rt
                for tc in range(4):
                    pt, bp = P[4 + tc]
                    S.op("dve", lambda e: e.tensor_scalar(out=rb[:, tc * 512:(tc + 1) * 512], in0=pt[:], scalar1=1.0 / 1024, scalar2=EPS, op0=ALU.mult, op1=ALU.add),
                         reads=[bp], writes=[b_rb])
                S.op("act", lambda e: e.activation(out=rb[:], in_=rb[:], func=AF.Sqrt), reads=[b_rb], writes=[b_rb])
                S.op("dve", lambda e: e.reciprocal(out=rb[:], in_=rb[:]), reads=[b_rb], writes=[b_rb])
                ostg = Ring([(at, b_at), (ut, b_ut)])
                for ct in range(8):
                    st_, bs = ostg.next()
                    S.op("dve", lambda e: e.scalar_tensor_tensor(out=st_[:], in0=olru[:, ct, :], scalar=gol[:, ct:ct + 1], in1=rb[:], op0=ALU.mult, op1=ALU.mult),
                         reads=[b_olru[ct], b_gol, b_rb], writes=[bs])
                    S.dma("sp", onT_d[1024 + ct * 128:1024 + (ct + 1) * 128, :], st_[:], reads=[bs], writes=[b_onT[8 + ct]])
            if lvl == 2:
                final_bufs += b_onT[8:]

        if lvl >= 3:
            with phase() as sb:
                def ld(name, shp, src, dt=F32, reads=()):
                    t = sb(name, shp, dt); b = Buf()
                    S.dma("sp", t[:], src, reads=list(reads), writes=[b])
                    return t, b
                bones, b_bones = ld("bones", [128, 128], r32(bones_d[:, :]), F32R)
                cmask, b_cmask = ld("cmask", [128, 4, 512], r32(cmask_d[:, :, :]), F32R)
                wmask, b_wmask = ld("wmask", [128, 4, 512], r32(wmask_d[:, :, :]), F32R)
                ccm, b_ccm = ld("ccm", [128, 4, 512], r32(ccm_d[:, :, :]), F32R)
                esel, b_esel = ld("esel", [32, 16, 128], r32(esel_d[:, :, :]), F32R)
                selv, b_selv = ld("selv", [128, 16, 32], selv_d[:, :, :])
                self_, b_self = ld("self", [128, 16, 32], self_d[:, :, :])
                qg, b_qg = ld("qg", [128, 1], qg_d[:, :])
                kg, b_kg = ld("kg", [128, 3], kg_d[:, :])
                ones_r, b_ones = ld("ones_r", [128, 128], r32(ones_d[:, :]), F32R)
                qgs = sb("qgs", [128, 1]); b_qgs = Buf()
                S.op("dve", lambda e: e.tensor_scalar(out=qgs[:], in0=qg[:], scalar1=0.125, scalar2=None, op0=ALU.mult), reads=[b_qg], writes=[b_qgs])
                gsig, b_gsig = ld("gsig", [128, NT, 48], ztm_d[:, 512:560].rearrange("(t p) c -> p t c", p=128), reads=b_ztm)
                S.op("act", lambda e: e.activation(out=gsig[:], in_=gsig[:], func=AF.Sigmoid), reads=[b_gsig], writes=[b_gsig])

                qn = sb("qn", [128, 4, S_LEN], F32R); b_qn = [Buf() for _ in range(4)]
                kns = sb("kns", [128, S_LEN], F32R); b_kns = Buf()
                knw = sb("knw", [128, S_LEN], F32R); b_knw = Buf()
                kcn = sb("kcn", [128, 128], F32R); b_kcn = Buf()
                vcaug = sb("vcaug", [128, 2, 98], F32R); b_vcaug = Buf()
                vs = sb("vs", [128, NT, 2, 66], F32R); b_vs = Buf()
                vw = sb("vw", [128, NT, 2, 66], F32R); b_vw = Buf()

                for pair in range(2):
                    g0 = 2 * pair
                    with phase() as sp_:
                        wck, b_wck = sp_("wck", [128, 32, 128], F32R), Buf()
                        S.dma("sp", wck[:], r32(wck_d[:, :, :]), writes=[b_wck])
                        wcv, b_wcv = sp_("wcv", [128, 32, 64], F32R), Buf()
                        S.dma("sp", wcv[:], r32(wcv_d[:, :, :]), writes=[b_wcv])
                        pek2, b_pek2 = sp_("pek2", [128, 32, 2], F32R), Buf()
                        S.dma("sp", pek2[:], r32(pek2_d[:, :, :]), writes=[b_pek2])
                        pevT, b_pevT = sp_("pevT", [128, 32], F32R), Buf()
                        S.dma("sp", pevT[:], r32(pevT_d[:, :]), writes=[b_pevT])
                        raw = sp_("raw", [128, S_LEN]); b_raw = Buf()
                        sqt = sp_("sqt", [128, S_LEN + 16], F32R); b_sqt = Buf()
                        rr = sp_("rr", [128, 512]); b_rr = Buf()
                        kcraw = sp_("kcraw", [128, 128]); b_kcraw = Buf()
                        biasc = sp_("biasc", [128, 1]); b_biasc = Buf()
                        biasv = sp_("biasv", [1, 64], F32R); b_biasv = Buf()

                        def rms_feat(rows, gcol_ap, b_gcol, dst_fn, b_dst):
                            for hf, r0 in enumerate(rows):
                                S.dma("sp", raw[hf * 64:(hf + 1) * 64, :], zT_d[r0:r0 + 64, :], reads=[b_zT[r0 // 128]], writes=[b_raw])
                            S.op("act", lambda e: e.activation(out=sqt[:, 0:S_LEN], in_=raw[:], func=AF.Square), reads=[b_raw], writes=[b_sqt])
                            for tc in range(4):
                                pt, bp = P[6 + tc % 2]
                                S.op("pe", lambda e: e.matmul(out=pt[:], lhsT=bones[:], rhs=sqt[:, tc * 512:(tc + 1) * 512], start=True, stop=True),
                                     reads=[b_bones, b_sqt], writes=[bp])
                                S.op("dve", lambda e: e.tensor_scalar(out=rr[:], in0=pt[:], scalar1=1.0 / 64, scalar2=EPS, op0=ALU.mult, op1=ALU.add), reads=[bp], writes=[b_rr])
                                S.op("act", lambda e: e.activation(out=rr[:], in_=rr[:], func=AF.Sqrt), reads=[b_rr], writes=[b_rr])
                                S.op("dve", lambda e: e.reciprocal(out=rr[:], in_=rr[:]), reads=[b_rr], writes=[b_rr])
                                S.op("dve", lambda e: e.scalar_tensor_tensor(out=dst_fn(tc), in0=raw[:, tc * 512:(tc + 1) * 512], scalar=gcol_ap, in1=rr[:], op0=ALU.mult, op1=ALU.mult),
                                     reads=[b_raw, b_gcol, b_rr], writes=[b_dst])

                        for i in range(4):
                            ha, hb = 4 * g0 + i, 4 * (g0 + 1) + i
                            rms_feat([ha * 64, hb * 64], qgs[:, 0:1], b_qgs, lambda tc: qn[:, i, tc * 512:(tc + 1) * 512], b_qn[i])
                        rms_feat([1536 + g0 * 64, 1536 + g0 * 64 + 64], kg[:, 1:2], b_kg, lambda tc: kns[:, tc * 512:(tc + 1) * 512], b_kns)
                        rms_feat([1792 + g0 * 64, 1792 + g0 * 64 + 64], kg[:, 2:3], b_kg, lambda tc: knw[:, tc * 512:(tc + 1) * 512], b_knw)

                        S.dma("sp", sqt[:, 0:S_LEN], r32(zT_d[1024 + g0 * 64:1024 + g0 * 64 + 128, :]), reads=[b_zT[8 + pair]], writes=[b_sqt])
                        S.dma("sp", sqt[:, S_LEN:S_LEN + 16], r32(vaug0_d[:, 0, 0, 0:16]), writes=[b_sqt])
                        pk, bpk = P[6]
                        pb, bpb = P[7]
                        for j in range(32):
                            S.op("pe", lambda e: e.matmul(out=pk[:, 0:128], lhsT=wck[:, j, :], rhs=sqt[:, j:j + 16 * 127 + 1:16],
                                                          start=(j == 0), stop=(j == 31)),
                                 reads=[b_wck, b_sqt], writes=[bpk])
                        for j in range(32):
                            S.op("pe", lambda e: e.matmul(out=pb[:, 0:2], lhsT=wck[:, j, :], rhs=pek2[:, j, :],
                                                          start=(j == 0), stop=(j == 31)),
                                 reads=[b_wck, b_pek2], writes=[bpb])
                        S.op("dve", lambda e: e.tensor_copy(out=biasc[:], in_=pb[:, 0:1]), reads=[bpb], writes=[b_biasc])
                        S.op("dve", lambda e: e.tensor_scalar(out=kcraw[:], in0=pk[:, 0:128], scalar1=biasc[:, 0:1], scalar2=None, op0=ALU.add),
                             reads=[bpk, b_biasc], writes=[b_kcraw])
                        sq128 = sp_("sq128", [128, 128], F32R); b_sq128 = Buf()
                        S.op("act", lambda e: e.activation(out=sq128[:], in_=kcraw[:], func=AF.Square), reads=[b_kcraw], writes=[b_sq128])
                        S.op("pe", lambda e: e.matmul(out=pk[:, 128:256], lhsT=bones[:], rhs=sq128[:], start=True, stop=True), reads=[b_bones, b_sq128], writes=[bpk])
                        S.op("dve", lambda e: e.tensor_scalar(out=rr[:, 0:128], in0=pk[:, 128:256], scalar1=1.0 / 64, scalar2=EPS, op0=ALU.mult, op1=ALU.add), reads=[bpk], writes=[b_rr])
                        S.op("act", lambda e: e.activation(out=rr[:, 0:128], in_=rr[:, 0:128], func=AF.Sqrt), reads=[b_rr], writes=[b_rr])
                        S.op("dve", lambda e: e.reciprocal(out=rr[:, 0:128], in_=rr[:, 0:128]), reads=[b_rr], writes=[b_rr])
                        S.op("dve", lambda e: e.scalar_tensor_tensor(out=kcn[:], in0=kcraw[:], scalar=kg[:, 0:1], in1=rr[:, 0:128], op0=ALU.mult, op1=ALU.mult),
                             reads=[b_kcraw, b_kg, b_rr], writes=[b_kcn])

                        S.dma("sp", sqt[:, 0:S_LEN], r32(zT_d[1280 + g0 * 64:1280 + g0 * 64 + 128, :]), reads=[b_zT[10 + pair]], writes=[b_sqt])
                        pbv, bpbv = P[7]
                        for j in range(32):
                            S.op("pe", lambda e: e.matmul(out=pbv[0:1, 0:64], lhsT=pevT[0:64, j:j + 1], rhs=wcv[0:64, j, :], start=(j == 0), stop=(j == 31)),
                                 reads=[b_pevT, b_wcv], writes=[bpbv])
                        S.op("dve", lambda e: e.tensor_copy(out=biasv[:], in_=pbv[0:1, 0:64]), reads=[bpbv], writes=[b_biasv])
                        for gi in range(2):
                            S.dma("sp", vcaug[:, gi, :], r32(vcaug0_d[:, :]), writes=[b_vcaug])
                        for hf in range(2):
                            hs = slice(hf * 64, (hf + 1) * 64)
                            pv, bpv = P[4 + hf]
                            for j in range(32):
                                S.op("pe", lambda e: e.matmul(out=pv[:, 0:64], lhsT=sqt[hs, j:j + 16 * 127 + 1:16], rhs=wcv[hs, j, :], start=(j == 0), stop=False),
                                     reads=[b_sqt, b_wcv], writes=[bpv])
                            S.op("pe", lambda e: e.matmul(out=pv[:, 0:64], lhsT=ones_r[0:1, :], rhs=biasv[0:1, :], start=False, stop=True),
                                 reads=[b_ones, b_biasv], writes=[bpv])
                            S.op("act", lambda e: e.copy(out=vcaug[:, hf, 0:64], in_=pv[:, 0:64]), reads=[bpv], writes=[b_vcaug])
                        for (vt, bvt, c0) in ((vs, b_vs, 0), (vw, b_vw, 256)):
                            S.dma("sp", vt[:], r32(vaug0_d[:, :, :, :]), writes=[bvt])
                            for gi in range(2):
                                S.dma("sp", vt[:, :, gi, 0:64],
                                      r32(ztm_d[:, c0 + (g0 + gi) * 64:c0 + (g0 + gi) * 64 + 64]).rearrange("(t p) d -> p t d", p=128),
                                      reads=b_ztm, writes=[bvt])

                    with phase() as sa:
                        oacc = sa("oacc", [128, NT, 512]); b_oacc = [Buf() for _ in range(NT)]
                        impacc = sa("impacc", [128, NT, 2, 32]); b_imp = [Buf() for _ in range(NT)]
                        negselT = sa("negselT", [32, 2, S_LEN], F32R); b_nst = [Buf() for _ in range(2)]
                        ptr_ = Ring([(sa(f"PT{i}", [128, 512], F32R), Buf()) for i in range(3)])
                        small = sa("small", [128, 8]); b_small = Buf()
                        coef = sa("coef", [128, 4]); b_coef = Buf()
                        top8 = sa("top8", [128, 8]); b_top8 = Buf()
                        impp = sa("impp", [128, 32]); b_impp = Buf()
                        negs = sa("negs", [128, 32]); b_negs = Buf()

                        def heads():
                            for i in range(4):
                                for hf in range(2):
                                    yield i, hf, 4 * (g0 + hf) + i, hf * 4 + i, slice(hf * 64, (hf + 1) * 64)

                        nb = 0
                        for (i, hf, h, hl, hs) in heads():
                            for qc in range(4):
                                pt, bp = P[nb % 2]
                                po, bpo = P[2 + nb % 2]
                                nb += 1
                                S.op("pe", lambda e: e.matmul(out=pt[:], lhsT=kcn[hs, :], rhs=qn[hs, i, qc * 512:(qc + 1) * 512], start=True, stop=False),
                                     reads=[b_kcn, b_qn[i]], writes=[bp])
                                S.op("pe", lambda e: e.matmul(out=pt[:], lhsT=identr[:], rhs=ccm[:, qc, :], start=False, stop=True),
                                     reads=[b_identr, b_ccm], writes=[bp])
                                PT, bPT = ptr_.next()
                                S.op("act", lambda e: e.activation(out=PT[:], in_=pt[:], func=AF.Exp), reads=[bp], writes=[bPT])
                                for j in range(4):
                                    S.op("pe", lambda e: e.matmul(out=po[:, j * 128:j * 128 + 98], lhsT=PT[:, j * 128:(j + 1) * 128], rhs=vcaug[:, hf, :], start=True, stop=True),
                                         reads=[bPT, b_vcaug], writes=[bpo])
                                pov = po[:].rearrange("p (j c) -> p j c", j=4)
                                S.op("dve", lambda e: e.tensor_scalar(out=small[:, 0:4], in0=pov[:, :, 64], scalar1=1e-30, scalar2=None, op0=ALU.add), reads=[bpo], writes=[b_small])
                                S.op("dve", lambda e: e.reciprocal(out=small[:, 0:4], in_=small[:, 0:4]), reads=[b_small], writes=[b_small])
                                S.op("dve", lambda e: e.tensor_tensor(out=coef[:], in0=small[:, 0:4], in1=gsig[:, qc * 4:(qc + 1) * 4, h * 3], op=ALU.mult),
                                     reads=[b_small, b_gsig], writes=[b_coef])
                                for j in range(4):
                                    tt = qc * 4 + j
                                    S.op("dve", lambda e: e.tensor_scalar(out=oacc[:, tt, hl * 64:(hl + 1) * 64], in0=po[:, j * 128:j * 128 + 64], scalar1=coef[:, j:j + 1], scalar2=None, op0=ALU.mult),
                                         reads=[bpo, b_coef], writes=[b_oacc[tt]])
                                    if i == 0:
                                        S.op("dve", lambda e: e.tensor_scalar(out=impacc[:, tt, hf, :], in0=po[:, j * 128 + 66:j * 128 + 98], scalar1=small[:, j:j + 1], scalar2=None, op0=ALU.mult),
                                             reads=[bpo, b_small], writes=[b_imp[tt]])
                                    else:
                                        S.op("dve", lambda e: e.scalar_tensor_tensor(out=impacc[:, tt, hf, :], in0=po[:, j * 128 + 66:j * 128 + 98], scalar=small[:, j:j + 1], in1=impacc[:, tt, hf, :], op0=ALU.mult, op1=ALU.add),
                                             reads=[bpo, b_small, b_imp[tt]], writes=[b_imp[tt]])
                        for hf in range(2):
                            for tt in range(NT):
                                S.op("dve", lambda e: e.tensor_tensor(out=impp[:], in0=impacc[:, tt, hf, :], in1=selv[:, tt, :], op=ALU.mult), reads=[b_imp[tt], b_selv], writes=[b_impp])
                                S.op("dve", lambda e: e.tensor_tensor(out=impp[:], in0=impp[:], in1=self_[:, tt, :], op=ALU.add), reads=[b_impp, b_self], writes=[b_impp])
                                S.op("dve", lambda e: e.max(out=top8[:], in_=impp[:]), reads=[b_impp], writes=[b_top8])
                                S.op("dve", lambda e: e.tensor_scalar(out=negs[:], in0=impp[:], scalar1=top8[:, 7:8], scalar2=NEG, op0=ALU.is_lt, op1=ALU.mult),
                                     reads=[b_impp, b_top8], writes=[b_negs])
                                pt, bp = P[6 + (tt // 4) % 2]
                                S.op("pe", lambda e: e.transpose(out=pt[0:32, (tt % 4) * 128:(tt % 4 + 1) * 128], in_=negs[:], identity=ident[:]),
                                     reads=[b_negs, b_ident], writes=[bp])
                                if tt % 4 == 3:
                                    S.op("act", lambda e: e.copy(out=negselT[:, hf, (tt // 4) * 512:(tt // 4 + 1) * 512], in_=pt[0:32, :]), reads=[bp], writes=[b_nst[hf]])
                        ns = 0
                        for (i, hf, h, hl, hs) in heads():
                            for qc in range(4):
                                for br in (1, 2):
                                    kn, b_kn, vt, bvt = (kns, b_kns, vs, b_vs) if br == 1 else (knw, b_knw, vw, b_vw)
                                    kts = list(range(0, 4 * qc + 4)) if br == 1 else list(range(max(0, 4 * qc - 4), 4 * qc + 4))
                                    for n, kt in enumerate(kts):
                                        pt, bp = P[ns % 2]
                                        ns += 1
                                        S.op("pe", lambda e: e.matmul(out=pt[:], lhsT=kn[hs, kt * 128:(kt + 1) * 128], rhs=qn[hs, i, qc * 512:(qc + 1) * 512], start=True, stop=False),
                                             reads=[b_kn, b_qn[i]], writes=[bp])
                                        if br == 1:
                                            diag = kt >= 4 * qc
                                            S.op("pe", lambda e: e.matmul(out=pt[:], lhsT=esel[:, kt, :], rhs=negselT[:, hf, qc * 512:(qc + 1) * 512], start=False, stop=not diag),
                                                 reads=[b_esel, b_nst[hf]], writes=[bp])
                                            if diag:
                                                S.op("pe", lambda e: e.matmul(out=pt[:], lhsT=identr[:], rhs=cmask[:, kt - 4 * qc, :], start=False, stop=True),
                                                     reads=[b_identr, b_cmask], writes=[bp])
                                        else:
                                            r = kt - (4 * qc - 4)
                                            mk_, bmk = (wmask[:, r, :], b_wmask) if r < 4 else (cmask[:, r - 4, :], b_cmask)
                                            S.op("pe", lambda e: e.matmul(out=pt[:], lhsT=identr[:], rhs=mk_, start=False, stop=True),
                                                 reads=[b_identr, bmk], writes=[bp])
                                        PT, bPT = ptr_.next()
                                        S.op("act", lambda e: e.activation(out=PT[:], in_=pt[:], func=AF.Exp), reads=[bp], writes=[bPT])
                                        for j in range(4):
                                            pa, bpa = P[2 + j]
                                            S.op("pe", lambda e: e.matmul(out=pa[:, 0:66], lhsT=PT[:, j * 128:(j + 1) * 128], rhs=vt[:, kt, hf, :],
                                                                          start=(n == 0), stop=(n == len(kts) - 1)),
                                                 reads=[bPT, bvt], writes=[bpa])
                                    for j in range(4):
                                        tt = qc * 4 + j
                                        pa, bpa = P[2 + j]
                                        S.op("dve", lambda e: e.reciprocal(out=small[:, 4 + j:5 + j], in_=pa[:, 64:65]), reads=[bpa], writes=[b_small])
                                        S.op("dve", lambda e: e.tensor_tensor(out=small[:, 4 + j:5 + j], in0=small[:, 4 + j:5 + j], in1=gsig[:, tt, h * 3 + br:h * 3 + br + 1], op=ALU.mult),
                                             reads=[b_small, b_gsig], writes=[b_small])
                                        S.op("dve", lambda e: e.scalar_tensor_tensor(out=oacc[:, tt, hl * 64:(hl + 1) * 64], in0=pa[:, 0:64], scalar=small[:, 4 + j:5 + j], in1=oacc[:, tt, hl * 64:(hl + 1) * 64], op0=ALU.mult, op1=ALU.add),
                                             reads=[bpa, b_small, b_oacc[tt]], writes=[b_oacc[tt]])
                        for tt in range(NT):
                            S.dma("sp", onsa_d[tt * 128:(tt + 1) * 128, pair * 512:(pair + 1) * 512], oacc[:, tt, :], reads=[b_oacc[tt]], writes=[b_onsa[tt][pair]])
            if lvl == 3:
                final_bufs += [b for bb in b_onsa for b in bb]

        if lvl >= 4:
            with phase() as sb:
                gonb = sb("gonb", [128, 1024]); b_gonb = Buf()
                S.dma("sp", gonb[:], gon_d[0:1, :].partition_broadcast(128), writes=[b_gonb])
                oring = Ring([(sb(f"ot{i}", [128, 1024]), Buf()) for i in range(2)])
                sqj = sb("sqjF", [128, 1024]); b_sqj = Buf()
                ss = sb("ssF", [128, 1]); b_ss = Buf()
                stgF = Ring([(sb(f"stgF{i}", [128, 8, 512]), Buf()) for i in range(2)])
                st_, bs = None, None
                for tt in range(NT):
                    ot, bo = oring.next()
                    S.dma("sp", ot[:], onsa_d[tt * 128:(tt + 1) * 128, :], reads=b_onsa[tt], writes=[bo])
                    S.op("act", lambda e: e.activation(out=sqj[:], in_=ot[:], func=AF.Square, accum_out=ss[:]), reads=[bo], writes=[b_sqj, b_ss])
                    S.op("dve", lambda e: e.tensor_scalar(out=ss[:], in0=ss[:], scalar1=1.0 / 1024, scalar2=EPS, op0=ALU.mult, op1=ALU.add), reads=[b_ss], writes=[b_ss])
                    S.op("act", lambda e: e.activation(out=ss[:], in_=ss[:], func=AF.Sqrt), reads=[b_ss], writes=[b_ss])
                    S.op("dve", lambda e: e.reciprocal(out=ss[:], in_=ss[:]), reads=[b_ss], writes=[b_ss])
                    S.op("dve", lambda e: e.scalar_tensor_tensor(out=ot[:], in0=ot[:], scalar=ss[:, 0:1], in1=gonb[:], op0=ALU.mult, op1=ALU.mult),
                         reads=[bo, b_ss, b_gonb], writes=[bo])
                    if tt % 4 == 0:
                        st_, bs = stgF.next()
                    for half in range(2):
                        pt, bp = P[(2 * tt + half) % 4]
                        for j in range(4):
                            ft = half * 4 + j
                            S.op("pe", lambda e: e.transpose(out=pt[:, j * 128:(j + 1) * 128], in_=ot[:, ft * 128:(ft + 1) * 128], identity=ident[:]),
                                 reads=[bo, b_ident], writes=[bp])
                        S.op("act", lambda e: e.copy(out=st_[:, half * 4:(half + 1) * 4, (tt % 4) * 128:(tt % 4 + 1) * 128], in_=pt[:].rearrange("p (a b) -> p a b", a=4)),
                             reads=[bp], writes=[bs])
                    if tt % 4 == 3:
                        tq = tt // 4
                        S.dma("sp", onT_d[0:1024, tq * 512:(tq + 1) * 512].rearrange("(f p) t -> p f t", p=128), st_[:], reads=[bs], writes=b_onT[0:8])
            if lvl == 4:
                final_bufs += b_onT

        if lvl >= 5:
            with phase() as sb:
                onT = sb("onT", [128, 16, S_LEN], F32R); b_onTs = Buf()
                for kt in range(16):
                    S.dma("sp", onT[:, kt, :], r32(onT_d[kt * 128:(kt + 1) * 128, :]), reads=[b_onT[kt]], writes=[b_onTs])
                ws = WStream(mkwring(sb), [wsrc(wout_d, c) for c in range(D // WCOL)])
                g1ring = Ring([(sb(f"g1b{i}", [128, WCOL]), Buf()) for i in range(2)])
                xcr = Ring([(sb(f"xc{i}", [128, WCOL]), Buf()) for i in range(3)])
                ocr = Ring([(sb(f"oc{i}", [128, WCOL]), Buf()) for i in range(3)])
                pi = 0
                for c in range(D // WCOL):
                    wt, bw = ws.get(c)
                    g1b, bg1 = g1ring.next()
                    S.dma("sp", g1b[:], mod_d[0:1, 2 * D + c * WCOL:2 * D + (c + 1) * WCOL].partition_broadcast(128), reads=[b_mod], writes=[bg1])
                    for tt in range(NT):
                        pt, bp = P[pi % 4]; pi += 1
                        for kt in range(16):
                            S.op("pe", lambda e: e.matmul(out=pt[:, 0:WCOL], lhsT=onT[:, kt, tt * 128:(tt + 1) * 128], rhs=wt[:, kt, :], start=(kt == 0), stop=(kt == 15)),
                                 reads=[b_onTs, bw], writes=[bp])
                        xt, bx = xcr.next()
                        S.dma("sp", xt[:], x_d[tt * 128:(tt + 1) * 128, c * WCOL:(c + 1) * WCOL], writes=[bx])
                        ot, bo = ocr.next()
                        S.op("dve", lambda e: e.tensor_tensor(out=ot[:], in0=pt[:, 0:WCOL], in1=g1b[:], op=ALU.mult), reads=[bp, bg1], writes=[bo])
                        S.op("dve", lambda e: e.tensor_tensor(out=ot[:], in0=ot[:], in1=xt[:], op=ALU.add), reads=[bo, bx], writes=[bo])
                        S.dma("sp", x1_d[tt * 128:(tt + 1) * 128, c * WCOL:(c + 1) * WCOL], ot[:], reads=[bo], writes=[b_x1[tt]])
            if lvl == 5:
                final_bufs += b_x1

        if lvl >= 6:
            with phase() as sbo:
                h2b = sbo("h2b", [128, NT, D], BF16); b_h2b = [Buf() for _ in range(NT)]
                maskall = sbo("maskall", [128, NT, NE], F32R); b_mask = [Buf() for _ in range(NT)]
                posall = sbo("posall", [128, NT, NE]); b_pos = [Buf() for _ in range(NT)]
                sidx = sbo("sidx", [128, NT, 4], I32); b_sidx = Buf()
                gk = sbo("gk", [128, NT, 4]); b_gk = Buf()
                with phase() as sb:
                    def ld(name, shp, src, dt=F32, reads=()):
                        t = sb(name, shp, dt); b = Buf()
                        S.dma("sp", t[:], src, reads=list(reads), writes=[b])
                        return t, b
                    A2b, b_A2b = ld("A2b", [128, D], mod_d[0:1, 4 * D:5 * D].partition_broadcast(128), reads=[b_mod])
                    g2b, b_g2b = ld("g2b", [128, D], g2_d[0:1, :].partition_broadcast(128))
                    B2b, b_B2b = ld("B2b", [128, D], mod_d[0:1, 3 * D:4 * D].partition_broadcast(128), reads=[b_mod])
                    S.op("dve", lambda e: e.scalar_tensor_tensor(out=A2b[:], in0=A2b[:], scalar=1.0, in1=g2b[:], op0=ALU.add, op1=ALU.mult),
                         reads=[b_A2b, b_g2b], writes=[b_A2b])
                    wr, b_wr = ld("wr", [128, 16, NE], wr_d[:, :, :])
                    brb, b_brb = ld("brb", [128, NE], br_d[0:1, :].partition_broadcast(128))
                    triu, b_triu = ld("triu", [128, 128], r32(triu_d[:, :]), F32R)
                    ones_r, b_ones = ld("ones_r", [128, 128], r32(ones_d[:, :]), F32R)
                    ebase, b_ebase = ld("ebase", [128, NE], ebase_d[:, :])
                    x1r = Ring([(sb(f"x1t{i}", [128, D]), Buf()) for i in range(2)])
                    sqj = sb("sqjH", [128, D]); b_sqj = Buf()
                    ss = sb("ssH", [128, 1]); b_ss = Buf()
                    h2f = sb("h2f", [128, D]); b_h2f = Buf()
                    h2T = sb("h2T", [128, 16, 128]); b_h2T = Buf()
                    lg = sb("lg", [128, NE]); b_lg = Buf()
                    t8 = sb("t8", [128, 8]); b_t8 = Buf()
                    ex = sb("ex", [128, NE]); b_ex = Buf()
                    nm = sb("nm", [128, 1]); b_nm = Buf()
                    gd = sb("gd", [128, 1]); b_gd = Buf()
                    e4 = sb("e4", [128, 4]); b_e4 = Buf()
                    slot = sb("slot", [128, NE]); b_slot = Buf()
                    oh = sb("oh", [128, NE]); b_oh = Buf()
                    sf = sb("sf", [128, 4]); b_sf = Buf()
                    rinfo = sb("rinfo", [128, NT, 8]); b_ri = Buf()
                    for tt in range(NT):
                        xt, bx = x1r.next()
                        S.dma("sp", xt[:], x1_d[tt * 128:(tt + 1) * 128, :], reads=[b_x1[tt]], writes=[bx])
                        S.op("act", lambda e: e.activation(out=sqj[:], in_=xt[:], func=AF.Square, accum_out=ss[:]), reads=[bx], writes=[b_sqj, b_ss])
                        S.op("dve", lambda e: e.tensor_scalar(out=ss[:], in0=ss[:], scalar1=1.0 / D, scalar2=EPS, op0=ALU.mult, op1=ALU.add), reads=[b_ss], writes=[b_ss])
                        S.op("act", lambda e: e.activation(out=ss[:], in_=ss[:], func=AF.Sqrt), reads=[b_ss], writes=[b_ss])
                        S.op("dve", lambda e: e.reciprocal(out=ss[:], in_=ss[:]), reads=[b_ss], writes=[b_ss])
                        S.op("dve", lambda e: e.scalar_tensor_tensor(out=h2f[:], in0=xt[:], scalar=ss[:, 0:1], in1=A2b[:], op0=ALU.mult, op1=ALU.mult),
                             reads=[bx, b_ss, b_A2b], writes=[b_h2f])
                        S.op("dve", lambda e: e.tensor_tensor(out=h2f[:], in0=h2f[:], in1=B2b[:], op=ALU.add), reads=[b_h2f, b_B2b], writes=[b_h2f])
                        S.op("act", lambda e: e.copy(out=h2b[:, tt, :], in_=h2f[:]), reads=[b_h2f], writes=[b_h2b[tt]])
                        for g in range(4):
                            pt, bp = P[g]
                            for j in range(4):
                                dc = g * 4 + j
                                S.op("pe", lambda e: e.transpose(out=pt[:, j * 128:(j + 1) * 128], in_=h2f[:, dc * 128:(dc + 1) * 128], identity=ident[:]),
                                     reads=[b_h2f, b_ident], writes=[bp])
                            if g % 2 == 0:
                                S.op("act", lambda e: e.copy(out=h2T[:, g * 4:(g + 1) * 4, :], in_=pt[:].rearrange("p (a b) -> p a b", a=4)), reads=[bp], writes=[b_h2T])
                            else:
                                S.op("dve", lambda e: e.tensor_copy(out=h2T[:, g * 4:(g + 1) * 4, :], in_=pt[:].rearrange("p (a b) -> p a b", a=4)), reads=[bp], writes=[b_h2T])
                        pl, bpl = P[4]
                        for dc in range(16):
                            S.op("pe", lambda e: e.matmul(out=pl[:, 0:NE], lhsT=h2T[:, dc, :], rhs=wr[:, dc, :], start=(dc == 0), stop=(dc == 15)),
                                 reads=[b_h2T, b_wr], writes=[bpl])
                        S.op("dve", lambda e: e.tensor_tensor(out=lg[:], in0=pl[:, 0:NE], in1=brb[:], op=ALU.add), reads=[bpl, b_brb], writes=[b_lg])
                        S.op("dve", lambda e: e.max(out=t8[:], in_=lg[:]), reads=[b_lg], writes=[b_t8])
                        S.op("dve", lambda e: e.tensor_scalar(out=maskall[:, tt, :], in0=lg[:], scalar1=t8[:, 3:4], scalar2=None, op0=ALU.is_ge),
                             reads=[b_lg, b_t8], writes=[b_mask[tt]])
                        S.op("dve", lambda e: e.tensor_scalar(out=nm[:], in0=t8[:, 0:1], scalar1=-1.0, scalar2=None, op0=ALU.mult), reads=[b_t8], writes=[b_nm])
                        S.op("act", lambda e: e.activation(out=e4[:], in_=t8[:, 0:4], func=AF.Exp, bias=nm[:, 0:1], accum_out=gd[:]), reads=[b_t8, b_nm], writes=[b_e4, b_gd])
                        S.op("dve", lambda e: e.reciprocal(out=gd[:], in_=gd[:]), reads=[b_gd], writes=[b_gd])
                        S.op("dve", lambda e: e.tensor_scalar(out=gk[:, tt, :], in0=e4[:], scalar1=gd[:, 0:1], scalar2=None, op0=ALU.mult), reads=[b_e4, b_gd], writes=[b_gk])
                        pp, bpp = P[5 + tt % 2]
                        for t2 in range(tt):
                            S.op("pe", lambda e: e.matmul(out=pp[:, 0:NE], lhsT=ones_r[:], rhs=maskall[:, t2, :], start=(t2 == 0), stop=False),
                                 reads=[b_ones, b_mask[t2]], writes=[bpp])
                        S.op("pe", lambda e: e.matmul(out=pp[:, 0:NE], lhsT=triu[:], rhs=maskall[:, tt, :], start=(tt == 0), stop=True),
                             reads=[b_triu, b_mask[tt]], writes=[bpp])
                        S.op("act", lambda e: e.copy(out=posall[:, tt, :], in_=pp[:, 0:NE]), reads=[bpp], writes=[b_pos[tt]])
                        S.op("dve", lambda e: e.tensor_tensor(out=slot[:], in0=posall[:, tt, :], in1=ebase[:], op=ALU.add), reads=[b_pos[tt], b_ebase], writes=[b_slot])
                        for k in range(4):
                            S.op("dve", lambda e: e.tensor_scalar(out=oh[:], in0=lg[:], scalar1=t8[:, k:k + 1], scalar2=None, op0=ALU.is_equal), reads=[b_lg, b_t8], writes=[b_oh])
                            S.op("dve", lambda e: e.tensor_tensor(out=oh[:], in0=oh[:], in1=slot[:], op=ALU.mult), reads=[b_oh, b_slot], writes=[b_oh])
                            S.op("dve", lambda e: e.reduce_sum(out=sf[:, k:k + 1], in_=oh[:], axis=mybir.AxisListType.X), reads=[b_oh], writes=[b_sf])
                        S.op("dve", lambda e: e.tensor_copy(out=sidx[:, tt, :], in_=sf[:]), reads=[b_sf], writes=[b_sidx])
                        if dbg:
                            S.op("dve", lambda e: e.tensor_copy(out=rinfo[:, tt, 0:4], in_=sf[:]), reads=[b_sf], writes=[b_ri])
                            S.op("dve", lambda e: e.tensor_copy(out=rinfo[:, tt, 4:8], in_=gk[:, tt, :]), reads=[b_gk], writes=[b_ri])
                    if dbg:
                        S.dma("sp", rinfo_d.rearrange("(t p) c -> p t c", p=128), rinfo[:], reads=[b_ri], writes=[b_rinfo])
                if lvl == 6:
                    final_bufs += [b_rinfo]

                if lvl >= 7:
                    with phase() as sb:
                        iotas = sb("iotas", [128, CAP]); b_iotas = Buf()
                        S.dma("sp", iotas[:], iotas_d[:, :], writes=[b_iotas])
                        b1c = sb("b1c", [128, NE, 32]); b_b1c = Buf()
                        S.dma("sp", b1c[:], b1c_d[:, :, :], writes=[b_b1c])
                        ones_r = sb("ones_rE", [1, 128], F32R); b_ones = Buf()
                        S.dma("sp", ones_r[:], r32(ones_d[0:1, :]), writes=[b_ones])
                        Gt = sb("Gt", [128, NT, CAPW], BF16); b_Gt = Buf()
                        xbT = sb("xbT", [128, 16, CAPW], F32R); b_xbT = Buf()
                        tact = sb("tact", [128, 16, CAPW], F32R); b_tact = [Buf() for _ in range(16)]
                        ug = sb("ug", [128, CAPW]); b_ug = Buf()
                        sg = sb("sg", [128, CAPW]); b_sg = Buf()
                        g2ring = Ring([(sb(f"g2b{i}", [128, WCOL]), Buf()) for i in range(2)])
                        b2ring = Ring([(sb(f"b2r{i}", [1, WCOL], F32R), Buf()) for i in range(2)])
                        yring = Ring([(sb(f"yst{i}", [128, WCOL]), Buf()) for i in range(3)])
                        srcs = []
                        for w_ in range(NW):
                            for e_ in range(NE):
                                srcs += [wsrc(we1_d[e_], c) for c in range(2 * D // WCOL)]
                                srcs += [wsrc(we2_d[e_], c) for c in range(D // WCOL)]
                        ws = WStream(mkwring(sb, 2), srcs, ahead=1)
                        wi = 0
                        pi = 0
                        for w_, e_ in [(w_, e_) for w_ in range(NW) for e_ in range(NE)]:
                            for tt in range(NT):
                                S.op("dve", lambda e: e.tensor_scalar(out=Gt[:, tt, :], in0=iotas[:, w_ * CAPW:(w_ + 1) * CAPW], scalar1=posall[:, tt, e_:e_ + 1], scalar2=f32(maskall[:, tt, e_:e_ + 1]),
                                                                      op0=ALU.is_equal, op1=ALU.mult),
                                     reads=[b_iotas, b_pos[tt], b_mask[tt]], writes=[b_Gt])
                            for dc in range(16):
                                pt, bp = P[pi % 4]; pi += 1
                                for tt in range(NT):
                                    S.op("pe", lambda e: e.matmul(out=pt[:, 0:CAPW], lhsT=h2b[:, tt, dc * 128:(dc + 1) * 128], rhs=Gt[:, tt, :], start=(tt == 0), stop=(tt == NT - 1)),
                                         reads=[b_h2b[tt], b_Gt], writes=[bp])
                                if dc % 2 == 0:
                                    S.op("act", lambda e: e.copy(out=xbT[:, dc, :], in_=pt[:, 0:CAPW]), reads=[bp], writes=[b_xbT])
                                else:
                                    S.op("dve", lambda e: e.tensor_copy(out=xbT[:, dc, :], in_=pt[:, 0:CAPW]), reads=[bp], writes=[b_xbT])
                            for c in range(2 * D // WCOL):
                                wt, bw = ws.get(wi); wi += 1
                                for ftl in range(2):
                                    F = c * 2 + ftl
                                    pt, bp = P[pi % 4]; pi += 1
                                    for kt in range(16):
                                        S.op("pe", lambda e: e.matmul(out=pt[:, 0:CAPW], lhsT=wt[:, kt, ftl * 128:(ftl + 1) * 128], rhs=xbT[:, kt, :], start=(kt == 0), stop=(kt == 15)),
                                             reads=[bw, b_xbT], writes=[bp])
                                    if F < 16:
                                        S.op("dve", lambda e: e.tensor_scalar(out=ug[:], in0=pt[:, 0:CAPW], scalar1=b1c[:, e_, F:F + 1], scalar2=7.0, op0=ALU.add, op1=ALU.min),
                                             reads=[bp, b_b1c], writes=[b_ug])
                                        S.op("act", lambda e: e.activation(out=sg[:], in_=ug[:], func=AF.Sigmoid, scale=1.702), reads=[b_ug], writes=[b_sg])
                                        S.op("dve", lambda e: e.tensor_tensor(out=tact[:, F, :], in0=ug[:], in1=sg[:], op=ALU.mult), reads=[b_ug, b_sg], writes=[b_tact[F]])
                                    else:
                                        Fl = F - 16
                                        S.op("dve", lambda e: e.tensor_scalar(out=ug[:], in0=pt[:, 0:CAPW], scalar1=b1c[:, e_, F:F + 1], scalar2=7.0, op0=ALU.add, op1=ALU.min),
                                             reads=[bp, b_b1c], writes=[b_ug])
                                        S.op("dve", lambda e: e.tensor_scalar(out=ug[:], in0=ug[:], scalar1=-7.0, scalar2=1.0, op0=ALU.max, op1=ALU.add), reads=[b_ug], writes=[b_ug])
                                        S.op("dve", lambda e: e.tensor_tensor(out=tact[:, Fl, :], in0=f32(tact[:, Fl, :]), in1=ug[:], op=ALU.mult), reads=[b_ug, b_tact[Fl]], writes=[b_tact[Fl]])
                            for c in range(D // WCOL):
                                wt, bw = ws.get(wi); wi += 1
                                g2b_, bg2 = g2ring.next()
                                S.dma("sp", g2b_[:], mod_d[0:1, 5 * D + c * WCOL:5 * D + (c + 1) * WCOL].partition_broadcast(128), reads=[b_mod], writes=[bg2])
                                b2r, bb2 = b2ring.next()
                                S.dma("sp", b2r[:], r32(be2_d[e_:e_ + 1, c * WCOL:(c + 1) * WCOL]), writes=[bb2])
                                for st in range(NST):
                                    pt, bp = P[pi % 4]; pi += 1
                                    for kt in range(16):
                                        S.op("pe", lambda e: e.matmul(out=pt[:, 0:WCOL], lhsT=tact[:, kt, st * 128:(st + 1) * 128], rhs=wt[:, kt, :], start=(kt == 0), stop=False),
                                             reads=[b_tact[kt], bw], writes=[bp])
                                    S.op("pe", lambda e: e.matmul(out=pt[:, 0:WCOL], lhsT=ones_r[0:1, :], rhs=b2r[0:1, :], start=False, stop=True),
                                         reads=[b_ones, bb2], writes=[bp])
                                    yt, by = yring.next()
                                    S.op("dve", lambda e: e.tensor_tensor(out=yt[:], in0=pt[:, 0:WCOL], in1=g2b_[:], op=ALU.mult), reads=[bp, bg2], writes=[by])
                                    r0 = e_ * CAP + w_ * CAPW + st * 128
                                    S.dma("sp", Y_d[r0:r0 + 128, c * WCOL:(c + 1) * WCOL], yt[:], reads=[by], writes=[b_Y[e_][w_ * NST + st]])
                    if lvl == 7:
                        final_bufs += [b for bb in b_Y for b in bb]

                if lvl >= 8:
                    with phase() as sb:
                        x1r = Ring([(sb(f"x1c{i}", [128, D]), Buf()) for i in range(2)])
                        ykr = Ring([(sb(f"yk{i}", [128, D]), Buf()) for i in range(4)])
                        allY = [b for bb in b_Y for b in bb]
                        for tt in range(NT):
                            xt, bx = x1r.next()
                            S.dma("sp", xt[:], x1_d[tt * 128:(tt + 1) * 128, :], reads=[b_x1[tt]], writes=[bx])
                            for k in range(4):
                                yk, byk = ykr.next()
                                S.dma("pool", None, None, reads=allY + [b_sidx], writes=[byk],
                                      fn=lambda e: e.indirect_dma_start(out=yk[:, :], out_offset=None, in_=Y_d[:, :],
                                                                        in_offset=bass.IndirectOffsetOnAxis(ap=sidx[:, tt, k:k + 1], axis=0)))
                                S.op("dve", lambda e: e.scalar_tensor_tensor(out=xt[:], in0=yk[:], scalar=gk[:, tt, k:k + 1], in1=xt[:], op0=ALU.mult, op1=ALU.add),
                                     reads=[byk, b_gk, bx], writes=[bx])
                            S.dma("sp", out_d[tt * 128:(tt + 1) * 128, :], xt[:], reads=[bx], writes=[b_out[tt]])
                    final_bufs += b_out

        S.finish(final_bufs)
        S.barrier()
    return nc


def _consts():
    c = {}
    c["ident"] = np.eye(128, dtype=np.float32)
    bo = np.zeros((128, 128), np.float32); bo[:64, :64] = 1; bo[64:, 64:] = 1
    c["bones"] = bo
    c["ones"] = np.ones((128, 128), np.float32)
    p = np.arange(128)[:, None]; col = np.arange(512)[None, :]
    cm = np.zeros((128, 4, 512), np.float32); wm = np.zeros((128, 4, 512), np.float32); cc = np.zeros((128, 4, 512), np.float32)
    for r in range(4):
        cm[:, r, :] = np.where(r * 128 + p <= col, 0.0, NEG)
        wm[:, r, :] = np.where(p + r * 128 > col, 0.0, NEG)
        cc[:, r, :] = np.where(16 * p + 31 <= r * 512 + col, 0.0, NEG)
    c["cmask"], c["wmask"], c["ccm"] = cm, wm, cc
    es = np.zeros((32, 16, 128), np.float32)
    for kt in range(16):
        for pp in range(128):
            es[2 * kt + pp // 64, kt, pp] = 1.0
    c["esel"] = es
    t = np.arange(S_LEN); qb = t // 64; j = np.arange(32)
    valid = j[None, :] <= qb[:, None]
    forced = (j[None, :] == 0) | (j[None, :] == qb[:, None]) | (j[None, :] == qb[:, None] - 1)
    V = (valid & ~forced).astype(np.float32)
    Fm = np.where(forced, 1e30, np.where(valid, 0.0, -1e30)).astype(np.float32)
    c["selv"] = np.ascontiguousarray(V.reshape(16, 128, 32).transpose(1, 0, 2))
    c["self"] = np.ascontiguousarray(Fm.reshape(16, 128, 32).transpose(1, 0, 2))
    cs = np.arange(128) * 16; ss = np.arange(32) * 64
    ov = ((cs[:, None] < ss[None, :] + 64) & (cs[:, None] + 32 > ss[None, :])).astype(np.float32)
    vc0 = np.zeros((128, 98), np.float32); vc0[:, 64] = 1.0; vc0[:, 66:98] = ov
    c["vcaug0"] = vc0
    va0 = np.zeros((128, NT, 2, 66), np.float32); va0[..., 64] = 1.0
    c["vaug0"] = va0
    c["triu"] = np.triu(np.ones((128, 128), np.float32), k=1)
    c["iotas"] = np.broadcast_to(np.arange(CAP, dtype=np.float32)[None, :], (128, CAP)).copy()
    c["ebase"] = np.broadcast_to((np.arange(NE, dtype=np.float32) * CAP)[None, :], (128, NE)).copy()
    return c


def _cols(v, n=None):
    v = np.asarray(v, np.float32).reshape(-1, 128)
    return np.ascontiguousarray(v.T)


def prep_shared(inp):
    f = lambda k: np.asarray(inp[k], np.float32)[0]
    sh = dict(_consts())
    w_in = f("w_in")
    sh["wA"] = np.ascontiguousarray(np.concatenate([w_in[:, 0:1024], w_in[:, 1024:1280], w_in[:, 1280:1536], w_in[:, 1536:1792],
                                                    w_in[:, 2048:2304], w_in[:, 2608:3632], w_in[:, 3632:4656]], axis=1))
    wB = np.zeros((D, 768), np.float32)
    wB[:, 0:256] = w_in[:, 1792:2048]; wB[:, 256:512] = w_in[:, 2304:2560]; wB[:, 512:560] = w_in[:, 2560:2608]
    sh["wB"] = wB
    sh["w_ada"] = f("w_ada"); sh["b_ada"] = f("b_ada").reshape(1, -1)
    sh["g1c"] = _cols(f("g_norm1"))
    wck = f("w_cmp_k").reshape(32, 64, 64).transpose(1, 0, 2)
    wcv = f("w_cmp_v").reshape(32, 64, 64).transpose(1, 0, 2)
    wbd = np.zeros((128, 32, 128), np.float32)
    wbd[0:64, :, 0:64] = wck; wbd[64:128, :, 64:128] = wck
    sh["wck"] = wbd
    sh["wcv"] = np.ascontiguousarray(np.concatenate([wcv, wcv], axis=0))
    pek = f("pe_cmp_k").T; pev = f("pe_cmp_v").T
    pek = np.concatenate([pek, pek], axis=0)
    sh["pek2"] = np.ascontiguousarray(np.stack([pek, pek], axis=-1))
    sh["pevT"] = np.ascontiguousarray(np.concatenate([pev, pev], axis=0))
    qg = f("q_gain"); sh["qg"] = np.concatenate([qg, qg]).reshape(128, 1).copy()
    kgn = f("k_gain").T; sh["kg"] = np.ascontiguousarray(np.concatenate([kgn, kgn], axis=0))
    sh["cw"] = np.ascontiguousarray(f("conv_w").reshape(4, 8, 128).transpose(2, 1, 0))
    sh["cb"] = _cols(f("conv_b"))
    for nm, key in (("wrg", "w_rg"), ("wig", "w_ig")):
        w = f(key)
        bd = np.zeros((128, 8, 128), np.float32)
        for ct in range(8):
            bd[0:64, ct, 0:64] = w[2 * ct]; bd[64:128, ct, 64:128] = w[2 * ct + 1]
        sh[nm] = bd
    sh["brg"] = _cols(f("b_rg").reshape(-1)); sh["big"] = _cols(f("b_ig").reshape(-1))
    sh["lam"] = _cols(f("lru_lambda"))
    sh["gon"] = f("g_out_nsa").reshape(1, -1); sh["gol"] = _cols(f("g_out_lru"))
    sh["w_out"] = f("w_out"); sh["g2"] = f("g_norm2").reshape(1, -1)
    sh["wr"] = np.ascontiguousarray(f("w_router").reshape(16, 128, NE).transpose(1, 0, 2))
    sh["br"] = f("b_router").reshape(1, -1)
    sh["w_e1"] = f("w_e1"); sh["w_e2"] = f("w_e2")
    sh["b1c"] = np.ascontiguousarray(f("b_e1").reshape(NE, 32, 128).transpose(2, 0, 1))
    sh["b_e2"] = f("b_e2")
    return sh


def core_inputs(inp, sh, b):
    m = dict(sh)
    m["x"] = np.ascontiguousarray(np.asarray(inp["x"], np.float32)[b])
    m["csil"] = _cols(np.asarray(inp["c"], np.float32)[b])
    return m


_NC_CACHE = {}


def kernel(**inputs):
    sh = prep_shared(inputs)
    if "nc" not in _NC_CACHE:
        _NC_CACHE["nc"] = build_nc("all", False)
    nc = _NC_CACHE["nc"]
    in_maps = [core_inputs(inputs, sh, b) for b in range(8)]
    res = run_bass_kernel_spmd(nc, in_maps, core_ids=list(range(8)))
    return np.stack([np.asarray(r["out"], np.float32) for r in res.results], axis=0)
```

```python
from contextlib import ExitStack, contextmanager

import numpy as np
import concourse.bass as bass
import concourse.mybir as mybir
from concourse.bass_utils import run_bass_kernel_spmd

F32 = mybir.dt.float32
F32R = mybir.dt.float32r
BF16 = mybir.dt.bfloat16
I32 = mybir.dt.int32
AF = mybir.ActivationFunctionType
ALU = mybir.AluOpType

S_LEN = 2048
D = 2048
NT = 16
NE = 32
CAP = 1024
NST = CAP // 128
EPS = 1e-6
NEG = -30000.0
WCOL = 256


class Buf:
    __slots__ = ("name", "w", "r")

    def __init__(self, name=""):
        self.name = name
        self.w = None
        self.r = {}


class Sched:
    def __init__(self, nc, stack, n_dma_slots=12):
        self.nc = nc
        self.eng = {"pe": nc.tensor, "act": nc.scalar, "dve": nc.vector, "pool": nc.gpsimd, "sp": nc.sync}
        self.sem = {k: stack.enter_context(nc.semaphore("s_" + k)) for k in self.eng}
        self.cnt = {k: 0 for k in self.eng}
        self.waited = {k: {} for k in self.eng}
        self.slots = {}
        for q in ("sp", "pool"):
            self.slots[q] = [[stack.enter_context(nc.semaphore(f"d_{q}{i}")), 0] for i in range(n_dma_slots)]
        self.slot_i = {q: 0 for q in self.slots}
        self.nwaits = 0

    def _wait(self, e, ev):
        if ev is None:
            return
        sem, val = ev
        if e == "pe" and sem is self.sem["pe"]:
            return
        key = id(sem)
        if self.waited[e].get(key, 0) >= val:
            return
        self.eng[e].wait_ge(sem, val)
        self.waited[e][key] = val
        self.nwaits += 1

    def _deps(self, e, reads, writes):
        for b in reads:
            self._wait(e, b.w)
        for b in writes:
            self._wait(e, b.w)
            for ev in b.r.values():
                self._wait(e, ev)

    def _mark(self, ev, reads, writes):
        sem, val = ev
        for b in reads:
            b.r[id(sem)] = ev
        for b in writes:
            b.w = ev
            b.r = {}

    def op(self, e, fn, reads=(), writes=()):
        self._deps(e, reads, writes)
        ins = fn(self.eng[e])
        self.cnt[e] += 1
        ins.then_inc(self.sem[e], 1)
        ev = (self.sem[e], self.cnt[e])
        self._mark(ev, reads, writes)
        return ev

    def dma(self, q, out, in_, reads=(), writes=(), fn=None):
        slots = self.slots[q]
        i = self.slot_i[q]
        self.slot_i[q] = (i + 1) % len(slots)
        sem, uses = slots[i]
        if uses:
            self._wait(q, (sem, 16 * uses))
        self._deps(q, reads, writes)
        if fn is None:
            ins = self.eng[q].dma_start(out=out, in_=in_)
        else:
            ins = fn(self.eng[q])
        ins.then_inc(sem, 16)
        slots[i][1] = uses + 1
        ev = (sem, 16 * (uses + 1))
        self._mark(ev, reads, writes)
        return ev

    def barrier(self):
        evs = [(self.sem[k], self.cnt[k]) for k in self.eng if self.cnt[k] > 0]
        for q in self.slots:
            for sem, uses in self.slots[q]:
                if uses:
                    evs.append((sem, 16 * uses))
        for e in self.eng:
            for ev in evs:
                self._wait(e, ev)

    def finish(self, bufs):
        for b in bufs:
            self._wait("sp", b.w)
            for ev in b.r.values():
                self._wait("sp", ev)


class Ring:
    def __init__(self, items):
        self.items = items
        self.i = 0

    def next(self):
        it = self.items[self.i]
        self.i = (self.i + 1) % len(self.items)
        return it


def r32(ap):
    return ap.bitcast(F32R)


def f32(ap):
    return ap.bitcast(F32)


def build_nc(upto="all", dbg=False):
    nc = bass.Bass("TRN2", target_bir_lowering=False)
    nc.dge_precook = False
    order = ["mod", "inproj", "lru", "nsa", "nsaout", "outproj", "router", "experts", "combine", "all"]
    lvl = order.index(upto)

    def din(n, shp, dt=F32):
        return nc.dram_tensor(n, list(shp), dt, kind="ExternalInput").ap()

    dbg_sets = {"mod": ["mod_s"], "inproj": ["zT_s", "ztm_s"], "lru": ["onT_s"], "nsa": ["onsa_s"], "nsaout": ["onT_s"],
                "outproj": ["x1_s"], "router": ["rinfo_s"], "experts": [], "combine": [], "all": []}

    def dscr(n, shp, dt=F32):
        ext = dbg and n in dbg_sets[upto]
        return nc.dram_tensor(n, list(shp), dt, kind=("ExternalOutput" if ext else "Internal")).ap()

    x_d = din("x", [S_LEN, D])
    csil_d = din("csil", [128, 16])
    wada_d = din("w_ada", [D, 6 * D])
    bada_d = din("b_ada", [1, 6 * D])
    g1c_d = din("g1c", [128, 16])
    wA_d = din("wA", [D, 4096])
    wB_d = din("wB", [D, 768])
    wck_d = din("wck", [128, 32, 128])
    wcv_d = din("wcv", [128, 32, 64])
    pek2_d = din("pek2", [128, 32, 2])
    pevT_d = din("pevT", [128, 32])
    qg_d = din("qg", [128, 1])
    kg_d = din("kg", [128, 3])
    cw_d = din("cw", [128, 8, 4])
    cb_d = din("cb", [128, 8])
    wrg_d = din("wrg", [128, 8, 128])
    wig_d = din("wig", [128, 8, 128])
    brg_d = din("brg", [128, 8])
    big_d = din("big", [128, 8])
    lam_d = din("lam", [128, 8])
    gon_d = din("gon", [1, 1024])
    gol_d = din("gol", [128, 8])
    wout_d = din("w_out", [D, D])
    g2_d = din("g2", [1, D])
    wr_d = din("wr", [128, 16, NE])
    br_d = din("br", [1, NE])
    we1_d = din("w_e1", [NE, D, 2 * D]) if lvl >= 7 else None
    b1c_d = din("b1c", [128, NE, 32])
    we2_d = din("w_e2", [NE, D, D]) if lvl >= 7 else None
    be2_d = din("b_e2", [NE, D])
    ident_d = din("ident", [128, 128])
    bones_d = din("bones", [128, 128])
    cmask_d = din("cmask", [128, 4, 512])
    wmask_d = din("wmask", [128, 4, 512])
    ccm_d = din("ccm", [128, 4, 512])
    esel_d = din("esel", [32, 16, 128])
    selv_d = din("selv", [128, 16, 32])
    self_d = din("self", [128, 16, 32])
    triu_d = din("triu", [128, 128])
    iotas_d = din("iotas", [128, CAP])
    ebase_d = din("ebase", [128, NE])
    ones_d = din("ones", [128, 128])
    tokid_d = din("tokid", [128, NT, 2], I32)
    oobidx_d = din("oobidx", [128, 2 * NE * CAP // 128], I32)
    vaug0_d = din("vaug0", [128, NT, 2, 66])
    vcaug0_d = din("vcaug0", [128, 98])

    out_d = nc.dram_tensor("out", [S_LEN, D], F32, kind="ExternalOutput").ap()
    mod_d = dscr("mod_s", [1, 6 * D])
    zT_d = dscr("zT_s", [4096, S_LEN])
    ztm_d = dscr("ztm_s", [S_LEN, 768])
    onsa_d = dscr("onsa_s", [S_LEN, 1024])
    onT_d = dscr("onT_s", [D, S_LEN])
    x1_d = dscr("x1_s", [S_LEN, D])
    Y_d = dscr("Y_s", [NE * CAP, D])
    rinfo_d = dscr("rinfo_s", [S_LEN, 8])
    h2_d = dscr("h2_s", [S_LEN, D])
    tokidx_d = dscr("tokidx_s", [NE * CAP, 2], I32)
    b_h2d = [Buf() for _ in range(NT)]
    b_tokidx = Buf("tokidx")

    b_mod = Buf("mod_d")
    b_zT = [Buf(f"zT{i}") for i in range(32)]
    b_ztm = [Buf(f"ztm{i}") for i in range(NT)]
    b_onsa = [[Buf() for _ in range(2)] for _ in range(NT)]
    b_onT = [Buf(f"onT{i}") for i in range(16)]
    b_x1 = [Buf(f"x1{i}") for i in range(NT)]
    b_Y = [[Buf() for _ in range(NST)] for _ in range(NE)]
    b_out = [Buf(f"out{i}") for i in range(NT)]
    b_rinfo = Buf("rinfo")
    final_bufs = []

    with ExitStack() as top:
        S = Sched(nc, top)
        reg_slot = nc.gpsimd.to_reg(NE * CAP - 1)
        reg_tok = nc.gpsimd.to_reg(S_LEN - 1)

        uniq = [0]

        def mk(stack):
            def sb(n, shp, dt=F32):
                uniq[0] += 1
                return stack.enter_context(nc.sbuf_tensor(f"sb{uniq[0]}_{n}", list(shp), dt))
            return sb

        sbt = mk(top)

        @contextmanager
        def phase():
            with ExitStack() as ph_:
                yield mk(ph_)
                S.barrier()
        P = [(top.enter_context(nc.psum_tensor(f"P{i}", [128, 512], F32)), Buf(f"P{i}")) for i in range(8)]

        ident = sbt("ident", [128, 128]); b_ident = Buf("ident")
        S.dma("sp", ident[:], ident_d[:, :], writes=[b_ident])
        identr = sbt("identr", [128, 128], F32R); b_identr = Buf("identr")
        S.dma("sp", identr[:], r32(ident_d[:, :]), writes=[b_identr])
        def mkwring(sb_, n=3):
            return Ring([(sb_(f"wch{i}", [128, 16, WCOL], F32R), Buf(f"wch{i}")) for i in range(n)])

        def wsrc(w2d, c):
            return r32(w2d[:, c * WCOL:(c + 1) * WCOL]).rearrange("(kt p) f -> p kt f", p=128)

        class WStream:
            def __init__(self, wring, srcs, ahead=2):
                self.wring = wring
                self.srcs = srcs
                self.loaded = []
                self.ahead = ahead

            def get(self, k):
                while len(self.loaded) <= min(k + self.ahead, len(self.srcs) - 1):
                    t, b = self.wring.next()
                    S.dma("sp", t[:], self.srcs[len(self.loaded)], writes=[b])
                    self.loaded.append((t, b))
                return self.loaded[k]

        with phase() as sb:
            cs = sb("cs", [128, 16]); b_cs = Buf()
            S.dma("sp", cs[:], csil_d[:, :], writes=[b_cs])
            scr = sb("scr", [128, 16], F32R); b_scr = Buf()
            S.op("act", lambda e: e.activation(out=scr[:], in_=cs[:], func=AF.Silu), reads=[b_cs], writes=[b_scr])
            barow = Ring([(sb(f"barow{i}", [1, WCOL]), Buf()) for i in range(3)])
            mrow = Ring([(sb(f"mrow{i}", [1, WCOL]), Buf()) for i in range(3)])
            ws = WStream(mkwring(sb), [wsrc(wada_d, c) for c in range(6 * D // WCOL)])
            for c in range(6 * D // WCOL):
                wt, bw = ws.get(c)
                bt, bb = barow.next()
                S.dma("sp", bt[:], bada_d[:, c * WCOL:(c + 1) * WCOL], writes=[bb])
                pt, bp = P[c % 2]
                for kt in range(16):
                    S.op("pe", lambda e: e.matmul(out=pt[0:1, 0:WCOL], lhsT=scr[:, kt:kt + 1], rhs=wt[:, kt, :],
                                                  start=(kt == 0), stop=(kt == 15)),
                         reads=[b_scr, bw], writes=[bp])
                mt, bm = mrow.next()
                S.op("dve", lambda e: e.tensor_tensor(out=mt[:], in0=pt[0:1, 0:WCOL], in1=bt[:], op=ALU.add),
                     reads=[bp, bb], writes=[bm])
                S.dma("sp", mod_d[:, c * WCOL:(c + 1) * WCOL], mt[:], reads=[bm], writes=[b_mod])
        if lvl == 0:
            final_bufs.append(b_mod)

        def mod_cols(stack_sb, name, off):
            t = stack_sb(name, [128, 16]); b = Buf()
            S.dma("sp", None, None, reads=[b_mod], writes=[b],
                  fn=lambda e: e.dma_start(out=t[:], in_=mod_d[:, off:off + D].rearrange("o (c p) -> p (o c)", p=128),
                                           allow_slow_non_contiguous=True))
            return t, b

        def bc_load(t_ap, row_ap, reads, b):
            S.dma("sp", t_ap, row_ap.partition_broadcast(128), reads=reads, writes=[b])

        if lvl >= 1:
            with phase() as sb:
                sh1, b_sh1 = mod_cols(sb, "sh1", 0)
                sc1, b_sc1 = mod_cols(sb, "sc1", D)
                g1c = sb("g1c", [128, 16]); b_g1c = Buf()
                S.dma("sp", g1c[:], g1c_d[:, :], writes=[b_g1c])
                A1c = sb("A1c", [128, 16]); b_A1c = Buf()
                S.op("dve", lambda e: e.scalar_tensor_tensor(out=A1c[:], in0=sc1[:], scalar=1.0, in1=g1c[:], op0=ALU.add, op1=ALU.mult),
                     reads=[b_sc1, b_g1c], writes=[b_A1c])
                hT = sb("hT", [128, 16, S_LEN], F32R)
                b_hT = [Buf(f"hT{i}") for i in range(NT)]
                tmp = ExitStack(); sb2 = mk(tmp)
                xring = Ring([(sb2(f"xin{i}", [128, D]), Buf()) for i in range(2)])
                xs = sb2("xs", [128, D]); b_xs = Buf()
                sqj = sb2("sqj", [128, D]); b_sqj = Buf()
                ss = sb2("ss", [128, 1]); b_ss = Buf()
                rs = sb2("rs", [128, 1]); b_rs = Buf()
                for tt in range(NT):
                    xt, bx = xring.next()
                    S.dma("sp", xt[:], x_d[tt * 128:(tt + 1) * 128, :], writes=[bx])
                    S.op("act", lambda e: e.activation(out=sqj[:], in_=xt[:], func=AF.Square, accum_out=ss[:]),
                         reads=[bx], writes=[b_sqj, b_ss])
                    S.op("dve", lambda e: e.tensor_scalar(out=rs[:], in0=ss[:], scalar1=1.0 / D, scalar2=EPS, op0=ALU.mult, op1=ALU.add),
                         reads=[b_ss], writes=[b_rs])
                    S.op("act", lambda e: e.activation(out=rs[:], in_=rs[:], func=AF.Sqrt), reads=[b_rs], writes=[b_rs])
                    S.op("dve", lambda e: e.reciprocal(out=rs[:], in_=rs[:]), reads=[b_rs], writes=[b_rs])
                    S.op("dve", lambda e: e.tensor_scalar(out=xs[:], in0=xt[:], scalar1=rs[:, 0:1], scalar2=None, op0=ALU.mult),
                         reads=[bx, b_rs], writes=[b_xs])
                    for g in range(4):
                        pt, bp = P[4 + g % 4]
                        for j in range(4):
                            dc = g * 4 + j
                            S.op("pe", lambda e: e.transpose(out=pt[:, j * 128:(j + 1) * 128], in_=xs[:, dc * 128:(dc + 1) * 128], identity=ident[:]),
                                 reads=[b_xs, b_ident], writes=[bp])
                        for j in range(4):
                            dc = g * 4 + j
                            eng = "dve" if j % 2 == 0 else "pool"
                            if eng == "pool":
                                S.op("act", lambda e: e.activation(out=hT[:, dc, tt * 128:(tt + 1) * 128], in_=pt[:, j * 128:(j + 1) * 128],
                                                                   func=AF.Identity, scale=A1c[:, dc:dc + 1], bias=sh1[:, dc:dc + 1]),
                                     reads=[bp, b_A1c, b_sh1], writes=[b_hT[tt]])
                            else:
                                S.op("dve", lambda e: e.tensor_scalar(out=hT[:, dc, tt * 128:(tt + 1) * 128], in0=pt[:, j * 128:(j + 1) * 128],
                                                                      scalar1=A1c[:, dc:dc + 1], scalar2=sh1[:, dc:dc + 1], op0=ALU.mult, op1=ALU.add),
                                     reads=[bp, b_A1c, b_sh1], writes=[b_hT[tt]])
                S.barrier()
                tmp.close()
                stg = Ring([(sb(f"stgA{i}", [128, 512]), Buf()) for i in range(4)])
                srcs = [wsrc(wA_d, c) for c in range(16)] + [wsrc(wB_d, c) for c in range(3)]
                ws = WStream(mkwring(sb), srcs)
                pi = 0
                for c in range(16):
                    wt, bw = ws.get(c)
                    for ft in range(2):
                        rowtile = c * 2 + ft
                        for tc in range(4):
                            pt, bp = P[pi % 4]; pi += 1
                            for kt in range(16):
                                S.op("pe", lambda e: e.matmul(out=pt[:], lhsT=wt[:, kt, ft * 128:(ft + 1) * 128], rhs=hT[:, kt, tc * 512:(tc + 1) * 512],
                                                              start=(kt == 0), stop=(kt == 15)),
                                     reads=[bw] + b_hT[tc * 4:(tc + 1) * 4], writes=[bp])
                            st_, bs = stg.next()
                            if pi % 2 == 0:
                                S.op("act", lambda e: e.copy(out=st_[:], in_=pt[:]), reads=[bp], writes=[bs])
                            else:
                                S.op("dve", lambda e: e.tensor_copy(out=st_[:], in_=pt[:]), reads=[bp], writes=[bs])
                            S.dma("pool", zT_d[rowtile * 128:(rowtile + 1) * 128, tc * 512:(tc + 1) * 512], st_[:], reads=[bs], writes=[b_zT[rowtile]])
                for cb_ in range(3):
                    wt, bw = ws.get(16 + cb_)
                    for tt in range(NT):
                        pt, bp = P[pi % 4]; pi += 1
                        for kt in range(16):
                            S.op("pe", lambda e: e.matmul(out=pt[:, 0:WCOL], lhsT=hT[:, kt, tt * 128:(tt + 1) * 128], rhs=wt[:, kt, :],
                                                          start=(kt == 0), stop=(kt == 15)),
                                 reads=[bw, b_hT[tt]], writes=[bp])
                        st_, bs = stg.next()
                        S.op("act", lambda e: e.copy(out=st_[:, 0:WCOL], in_=pt[:, 0:WCOL]), reads=[bp], writes=[bs])
                        S.dma("pool", ztm_d[tt * 128:(tt + 1) * 128, cb_ * WCOL:(cb_ + 1) * WCOL], st_[:, 0:WCOL], reads=[bs], writes=[b_ztm[tt]])
            if lvl == 1:
                final_bufs += b_zT + b_ztm

        if lvl >= 2:
            with phase() as sb:

                def ld(name, shp, src, dt=F32):
                    t = sb(name, shp, dt); b = Buf()
                    S.dma("sp", t[:], src, writes=[b])
                    return t, b
                cw, b_cw = ld("cw", [128, 8, 4], cw_d[:, :, :])
                cb, b_cb = ld("cb", [128, 8], cb_d[:, :])
                wrg, b_wrg = ld("wrg", [128, 8, 128], r32(wrg_d[:, :, :]), F32R)
                wig, b_wig = ld("wig", [128, 8, 128], r32(wig_d[:, :, :]), F32R)
                brg, b_brg = ld("brg", [128, 8], brg_d[:, :])
                big, b_big = ld("big", [128, 8], big_d[:, :])
                lam, b_lam = ld("lam", [128, 8], lam_d[:, :])
                gol, b_gol = ld("gol", [128, 8], gol_d[:, :])
                ones_r, b_ones = ld("ones_r", [128, 128], r32(ones_d[:, :]), F32R)
                clam = sb("clam", [128, 8]); b_clam = Buf()
                clam2 = sb("clam2", [128, 8]); b_clam2 = Buf()
                S.op("act", lambda e: e.activation(out=clam[:], in_=lam[:], func=AF.Sigmoid), reads=[b_lam], writes=[b_clam])
                S.op("act", lambda e: e.activation(out=clam[:], in_=clam[:], func=AF.Ln), reads=[b_clam], writes=[b_clam])
                S.op("dve", lambda e: e.tensor_scalar(out=clam2[:], in0=clam[:], scalar1=16.0, scalar2=None, op0=ALU.mult), reads=[b_clam], writes=[b_clam2])
                S.op("dve", lambda e: e.tensor_scalar(out=clam[:], in0=clam[:], scalar1=8.0, scalar2=None, op0=ALU.mult), reads=[b_clam, b_clam2], writes=[b_clam])
                olru = sb("olru", [128, 8, S_LEN]); b_olru = [Buf() for _ in range(8)]
                xrp_r = Ring([(sb(f"xrp{i}", [128, 3 + S_LEN]), Buf()) for i in range(2)])
                xg_r = Ring([(sb(f"xg{i}", [128, S_LEN]), Buf()) for i in range(2)])
                xc = sb("xc", [128, S_LEN], F32R); b_xc = Buf()
                xcacc = sb("xcacc", [128, S_LEN])
                rt = sb("rt", [128, S_LEN]); b_rt = Buf()
                it = sb("it", [128, S_LEN]); b_it = Buf()
                at = sb("at", [128, S_LEN]); b_at = Buf()
                ut = sb("ut", [128, S_LEN]); b_ut = Buf()
                ht = sb("ht", [128, S_LEN]); b_ht = Buf()
                gt = sb("gt", [128, S_LEN]); b_gt = Buf()
                osq = sb("osq", [128, S_LEN], F32R); b_osq = Buf()
                for ct in range(8):
                    xrp, bxr = xrp_r.next()
                    xg, bxg = xg_r.next()
                    S.op("pool", lambda e: e.memset(xrp[:, 0:3], 0.0), writes=[bxr])
                    S.dma("sp", xrp[:, 3:3 + S_LEN], zT_d[2048 + ct * 128:2048 + (ct + 1) * 128, :], reads=[b_zT[16 + ct]], writes=[bxr])
                    S.dma("sp", xg[:], zT_d[3072 + ct * 128:3072 + (ct + 1) * 128, :], reads=[b_zT[24 + ct]], writes=[bxg])
                    xcf = xcacc[:]
                    S.op("dve", lambda e: e.tensor_scalar(out=xcf, in0=xrp[:, 0:S_LEN], scalar1=cw[:, ct, 0:1], scalar2=cb[:, ct:ct + 1], op0=ALU.mult, op1=ALU.add),
                         reads=[bxr, b_cw, b_cb], writes=[b_xc])
                    for k in range(1, 4):
                        outap = xc[:] if k == 3 else xcf
                        S.op("dve", lambda e: e.scalar_tensor_tensor(out=outap, in0=xrp[:, k:k + S_LEN], scalar=cw[:, ct, k:k + 1], in1=xcf, op0=ALU.mult, op1=ALU.add),
                             reads=[bxr, b_cw, b_xc], writes=[b_xc])
                    xcf = f32(xc[:])
                    for (wg, bwg, bg, bbg, dst, bdst) in ((wrg, b_wrg, brg, b_brg, rt, b_rt), (wig, b_wig, big, b_big, it, b_it)):
                        for tc in range(4):
                            pt, bp = P[tc % 2]
                            S.op("pe", lambda e: e.matmul(out=pt[:], lhsT=wg[:, ct, :], rhs=xc[:, tc * 512:(tc + 1) * 512], start=True, stop=True),
                                 reads=[bwg, b_xc], writes=[bp])
                            S.op("act", lambda e: e.activation(out=dst[:, tc * 512:(tc + 1) * 512], in_=pt[:], func=AF.Sigmoid, bias=bg[:, ct:ct + 1]),
                                 reads=[bp, bbg], writes=[bdst])
                    S.op("act", lambda e: e.activation(out=at[:], in_=rt[:], func=AF.Exp, scale=clam[:, ct:ct + 1]), reads=[b_rt, b_clam], writes=[b_at])
                    S.op("act", lambda e: e.activation(out=ut[:], in_=rt[:], func=AF.Exp, scale=clam2[:, ct:ct + 1]), reads=[b_rt, b_clam2], writes=[b_ut])
                    S.op("act", lambda e: e.activation(out=ut[:], in_=ut[:], func=AF.Sqrt, scale=-1.0, bias=1.0), reads=[b_ut], writes=[b_ut])
                    S.op("dve", lambda e: e.tensor_tensor(out=ut[:], in0=ut[:], in1=it[:], op=ALU.mult), reads=[b_ut, b_it], writes=[b_ut])
                    S.op("dve", lambda e: e.tensor_tensor(out=ut[:], in0=ut[:], in1=xcf, op=ALU.mult), reads=[b_ut, b_xc], writes=[b_ut])
                    S.op("dve", lambda e: e.tensor_tensor_scan(out=ht[:], data0=at[:], data1=ut[:], initial=0.0, op0=ALU.mult, op1=ALU.add),
                         reads=[b_at, b_ut], writes=[b_ht])
                    S.op("act", lambda e: e.activation(out=gt[:], in_=xg[:], func=AF.Square), reads=[bxg], writes=[b_gt])
                    S.op("dve", lambda e: e.tensor_scalar(out=gt[:], in0=gt[:], scalar1=0.044715, scalar2=1.0, op0=ALU.mult, op1=ALU.add), reads=[b_gt], writes=[b_gt])
                    S.op("dve", lambda e: e.tensor_tensor(out=gt[:], in0=gt[:], in1=xg[:], op=ALU.mult), reads=[b_gt, bxg], writes=[b_gt])
                    S.op("act", lambda e: e.activation(out=gt[:], in_=gt[:], func=AF.Sigmoid, scale=1.5957691216057308), reads=[b_gt], writes=[b_gt])
                    S.op("dve", lambda e: e.tensor_tensor(out=gt[:], in0=gt[:], in1=xg[:], op=ALU.mult), reads=[b_gt, bxg], writes=[b_gt])
                    S.op("dve", lambda e: e.tensor_tensor(out=olru[:, ct, :], in0=gt[:], in1=ht[:], op=ALU.mult), reads=[b_gt, b_ht], writes=[b_olru[ct]])
                    S.op("act", lambda e: e.activation(out=osq[:], in_=olru[:, ct, :], func=AF.Square), reads=[b_olru[ct]], writes=[b_osq])
                    for tc in range(4):
                        pt, bp = P[4 + tc]
                        S.op("pe", lambda e: e.matmul(out=pt[:], lhsT=ones_r[:], rhs=osq[:, tc * 512:(tc + 1) * 512], start=(ct == 0), stop=(ct == 7)),
                             reads=[b_ones, b_osq], writes=[bp])
                rb = rt; b_rb = b_rt
                for tc in range(4):
                    pt, bp = P[4 + tc]
                    S.op("dve", lambda e: e.tensor_scalar(out=rb[:, tc * 512:(tc + 1) * 512], in0=pt[:], scalar1=1.0 / 1024, scalar2=EPS, op0=ALU.mult, op1=ALU.add),
                         reads=[bp], writes=[b_rb])
                S.op("act", lambda e: e.activation(out=rb[:], in_=rb[:], func=AF.Sqrt), reads=[b_rb], writes=[b_rb])
                S.op("dve", lambda e: e.reciprocal(out=rb[:], in_=rb[:]), reads=[b_rb], writes=[b_rb])
                ostg = Ring([(at, b_at), (ut, b_ut)])
                for ct in range(8):
                    st_, bs = ostg.next()
                    S.op("dve", lambda e: e.scalar_tensor_tensor(out=st_[:], in0=olru[:, ct, :], scalar=gol[:, ct:ct + 1], in1=rb[:], op0=ALU.mult, op1=ALU.mult),
                         reads=[b_olru[ct], b_gol, b_rb], writes=[bs])
                    S.dma("pool", onT_d[1024 + ct * 128:1024 + (ct + 1) * 128, :], st_[:], reads=[bs], writes=[b_onT[8 + ct]])
            if lvl == 2:
                final_bufs += b_onT[8:]

        if lvl >= 3:
            with phase() as sb:
                def ld(name, shp, src, dt=F32, reads=()):
                    t = sb(name, shp, dt); b = Buf()
                    S.dma("sp", t[:], src, reads=list(reads), writes=[b])
                    return t, b
                bones, b_bones = ld("bones", [128, 128], r32(bones_d[:, :]), F32R)
                cmask, b_cmask = ld("cmask", [128, 4, 512], r32(cmask_d[:, :, :]), F32R)
                wmask, b_wmask = ld("wmask", [128, 4, 512], r32(wmask_d[:, :, :]), F32R)
                ccm, b_ccm = ld("ccm", [128, 4, 512], r32(ccm_d[:, :, :]), F32R)
                esel, b_esel = ld("esel", [32, 16, 128], r32(esel_d[:, :, :]), F32R)
                selv, b_selv = ld("selv", [128, 16, 32], selv_d[:, :, :])
                self_, b_self = ld("self", [128, 16, 32], self_d[:, :, :])
                qg, b_qg = ld("qg", [128, 1], qg_d[:, :])
                kg, b_kg = ld("kg", [128, 3], kg_d[:, :])
                ones_r, b_ones = ld("ones_r", [128, 128], r32(ones_d[:, :]), F32R)
                qgs = sb("qgs", [128, 1]); b_qgs = Buf()
                S.op("dve", lambda e: e.tensor_scalar(out=qgs[:], in0=qg[:], scalar1=0.125, scalar2=None, op0=ALU.mult), reads=[b_qg], writes=[b_qgs])
                gsig, b_gsig = ld("gsig", [128, NT, 48], ztm_d[:, 512:560].rearrange("(t p) c -> p t c", p=128), reads=b_ztm)
                S.op("act", lambda e: e.activation(out=gsig[:], in_=gsig[:], func=AF.Sigmoid), reads=[b_gsig], writes=[b_gsig])

                qn = sb("qn", [128, 4, S_LEN], F32R); b_qn = [Buf() for _ in range(4)]
                kns = sb("kns", [128, S_LEN], F32R); b_kns = Buf()
                knw = sb("knw", [128, S_LEN], F32R); b_knw = Buf()
                kcn = sb("kcn", [128, 128], F32R); b_kcn = Buf()
                vcaug = sb("vcaug", [128, 2, 98], F32R); b_vcaug = Buf()
                vs = sb("vs", [128, NT, 2, 66], F32R); b_vs = Buf()
                vw = sb("vw", [128, NT, 2, 66], F32R); b_vw = Buf()

                for pair in range(2):
                    g0 = 2 * pair
                    with phase() as sp_:
                        wck, b_wck = sp_("wck", [128, 32, 128], F32R), Buf()
                        S.dma("sp", wck[:], r32(wck_d[:, :, :]), writes=[b_wck])
                        wcv, b_wcv = sp_("wcv", [128, 32, 64], F32R), Buf()
                        S.dma("sp", wcv[:], r32(wcv_d[:, :, :]), writes=[b_wcv])
                        pek2, b_pek2 = sp_("pek2", [128, 32, 2], F32R), Buf()
                        S.dma("sp", pek2[:], r32(pek2_d[:, :, :]), writes=[b_pek2])
                        pevT, b_pevT = sp_("pevT", [128, 32], F32R), Buf()
                        S.dma("sp", pevT[:], r32(pevT_d[:, :]), writes=[b_pevT])
                        raw = sp_("raw", [128, S_LEN]); b_raw = Buf()
                        sqt = sp_("sqt", [128, S_LEN + 16], F32R); b_sqt = Buf()
                        rr = sp_("rr", [128, 512]); b_rr = Buf()
                        kcraw = sp_("kcraw", [128, 128]); b_kcraw = Buf()
                        biasc = sp_("biasc", [128, 1]); b_biasc = Buf()
                        biasv = sp_("biasv", [1, 64], F32R); b_biasv = Buf()

                        def rms_feat(rows, gcol_ap, b_gcol, dst_fn, b_dst):
                            for hf, r0 in enumerate(rows):
                                S.dma("sp", raw[hf * 64:(hf + 1) * 64, :], zT_d[r0:r0 + 64, :], reads=[b_zT[r0 // 128]], writes=[b_raw])
                            S.op("act", lambda e: e.activation(out=sqt[:, 0:S_LEN], in_=raw[:], func=AF.Square), reads=[b_raw], writes=[b_sqt])
                            for tc in range(4):
                                pt, bp = P[6 + tc % 2]
                                S.op("pe", lambda e: e.matmul(out=pt[:], lhsT=bones[:], rhs=sqt[:, tc * 512:(tc + 1) * 512], start=True, stop=True),
                                     reads=[b_bones, b_sqt], writes=[bp])
                                S.op("dve", lambda e: e.tensor_scalar(out=rr[:], in0=pt[:], scalar1=1.0 / 64, scalar2=EPS, op0=ALU.mult, op1=ALU.add), reads=[bp], writes=[b_rr])
                                S.op("act", lambda e: e.activation(out=rr[:], in_=rr[:], func=AF.Sqrt), reads=[b_rr], writes=[b_rr])
                                S.op("dve", lambda e: e.reciprocal(out=rr[:], in_=rr[:]), reads=[b_rr], writes=[b_rr])
                                S.op("dve", lambda e: e.scalar_tensor_tensor(out=dst_fn(tc), in0=raw[:, tc * 512:(tc + 1) * 512], scalar=gcol_ap, in1=rr[:], op0=ALU.mult, op1=ALU.mult),
                                     reads=[b_raw, b_gcol, b_rr], writes=[b_dst])

                        for i in range(4):
                            ha, hb = 4 * g0 + i, 4 * (g0 + 1) + i
                            rms_feat([ha * 64, hb * 64], qgs[:, 0:1], b_qgs, lambda tc: qn[:, i, tc * 512:(tc + 1) * 512], b_qn[i])
                        rms_feat([1536 + g0 * 64, 1536 + g0 * 64 + 64], kg[:, 1:2], b_kg, lambda tc: kns[:, tc * 512:(tc + 1) * 512], b_kns)
                        rms_feat([1792 + g0 * 64, 1792 + g0 * 64 + 64], kg[:, 2:3], b_kg, lambda tc: knw[:, tc * 512:(tc + 1) * 512], b_knw)

                        S.dma("sp", sqt[:, 0:S_LEN], r32(zT_d[1024 + g0 * 64:1024 + g0 * 64 + 128, :]), reads=[b_zT[8 + pair]], writes=[b_sqt])
                        S.dma("sp", sqt[:, S_LEN:S_LEN + 16], r32(vaug0_d[:, 0, 0, 0:16]), writes=[b_sqt])
                        pk, bpk = P[6]
                        pb, bpb = P[7]
                        for j in range(32):
                            S.op("pe", lambda e: e.matmul(out=pk[:, 0:128], lhsT=wck[:, j, :], rhs=sqt[:, j:j + 16 * 127 + 1:16],
                                                          start=(j == 0), stop=(j == 31)),
                                 reads=[b_wck, b_sqt], writes=[bpk])
                        for j in range(32):
                            S.op("pe", lambda e: e.matmul(out=pb[:, 0:2], lhsT=wck[:, j, :], rhs=pek2[:, j, :],
                                                          start=(j == 0), stop=(j == 31)),
                                 reads=[b_wck, b_pek2], writes=[bpb])
                        S.op("dve", lambda e: e.tensor_copy(out=biasc[:], in_=pb[:, 0:1]), reads=[bpb], writes=[b_biasc])
                        S.op("dve", lambda e: e.tensor_scalar(out=kcraw[:], in0=pk[:, 0:128], scalar1=biasc[:, 0:1], scalar2=None, op0=ALU.add),
                             reads=[bpk, b_biasc], writes=[b_kcraw])
                        sq128 = sp_("sq128", [128, 128], F32R); b_sq128 = Buf()
                        S.op("act", lambda e: e.activation(out=sq128[:], in_=kcraw[:], func=AF.Square), reads=[b_kcraw], writes=[b_sq128])
                        S.op("pe", lambda e: e.matmul(out=pk[:, 128:256], lhsT=bones[:], rhs=sq128[:], start=True, stop=True), reads=[b_bones, b_sq128], writes=[bpk])
                        S.op("dve", lambda e: e.tensor_scalar(out=rr[:, 0:128], in0=pk[:, 128:256], scalar1=1.0 / 64, scalar2=EPS, op0=ALU.mult, op1=ALU.add), reads=[bpk], writes=[b_rr])
                        S.op("act", lambda e: e.activation(out=rr[:, 0:128], in_=rr[:, 0:128], func=AF.Sqrt), reads=[b_rr], writes=[b_rr])
                        S.op("dve", lambda e: e.reciprocal(out=rr[:, 0:128], in_=rr[:, 0:128]), reads=[b_rr], writes=[b_rr])
                        S.op("dve", lambda e: e.scalar_tensor_tensor(out=kcn[:], in0=kcraw[:], scalar=kg[:, 0:1], in1=rr[:, 0:128], op0=ALU.mult, op1=ALU.mult),
                             reads=[b_kcraw, b_kg, b_rr], writes=[b_kcn])

                        S.dma("sp", sqt[:, 0:S_LEN], r32(zT_d[1280 + g0 * 64:1280 + g0 * 64 + 128, :]), reads=[b_zT[10 + pair]], writes=[b_sqt])
                        pbv, bpbv = P[7]
                        for j in range(32):
                            S.op("pe", lambda e: e.matmul(out=pbv[0:1, 0:64], lhsT=pevT[0:64, j:j + 1], rhs=wcv[0:64, j, :], start=(j == 0), stop=(j == 31)),
                                 reads=[b_pevT, b_wcv], writes=[bpbv])
                        S.op("dve", lambda e: e.tensor_copy(out=biasv[:], in_=pbv[0:1, 0:64]), reads=[bpbv], writes=[b_biasv])
                        for gi in range(2):
                            S.dma("sp", vcaug[:, gi, :], r32(vcaug0_d[:, :]), writes=[b_vcaug])
                        for hf in range(2):
                            hs = slice(hf * 64, (hf + 1) * 64)
                            pv, bpv = P[4 + hf]
                            for j in range(32):
                                S.op("pe", lambda e: e.matmul(out=pv[:, 0:64], lhsT=sqt[hs, j:j + 16 * 127 + 1:16], rhs=wcv[hs, j, :], start=(j == 0), stop=False),
                                     reads=[b_sqt, b_wcv], writes=[bpv])
                            S.op("pe", lambda e: e.matmul(out=pv[:, 0:64], lhsT=ones_r[0:1, :], rhs=biasv[0:1, :], start=False, stop=True),
                                 reads=[b_ones, b_biasv], writes=[bpv])
                            S.op("act", lambda e: e.copy(out=vcaug[:, hf, 0:64], in_=pv[:, 0:64]), reads=[bpv], writes=[b_vcaug])
                        for (vt, bvt, c0) in ((vs, b_vs, 0), (vw, b_vw, 256)):
                            S.dma("sp", vt[:], r32(vaug0_d[:, :, :, :]), writes=[bvt])
                            for gi in range(2):
                                S.dma("sp", vt[:, :, gi, 0:64],
                                      r32(ztm_d[:, c0 + (g0 + gi) * 64:c0 + (g0 + gi) * 64 + 64]).rearrange("(t p) d -> p t d", p=128),
                                      reads=b_ztm, writes=[bvt])

                    with phase() as sa:
                        oacc = sa("oacc", [128, NT, 512]); b_oacc = [Buf() for _ in range(NT)]
                        impacc = sa("impacc", [128, NT, 2, 32]); b_imp = [Buf() for _ in range(NT)]
                        negselT = sa("negselT", [32, 2, S_LEN], F32R); b_nst = [Buf() for _ in range(2)]
                        ptr_ = Ring([(sa(f"PT{i}", [128, 512], F32R), Buf()) for i in range(3)])
                        small = sa("small", [128, 8]); b_small = Buf()
                        coef = sa("coef", [128, 4]); b_coef = Buf()
                        top8 = sa("top8", [128, 8]); b_top8 = Buf()
                        impp = sa("impp", [128, 32]); b_impp = Buf()
                        negs = sa("negs", [128, 32]); b_negs = Buf()

                        def heads():
                            for i in range(4):
                                for hf in range(2):
                                    yield i, hf, 4 * (g0 + hf) + i, hf * 4 + i, slice(hf * 64, (hf + 1) * 64)

                        nb = 0
                        for (i, hf, h, hl, hs) in heads():
                            for qc in range(4):
                                pt, bp = P[nb % 2]
                                po, bpo = P[2 + nb % 2]
                                nb += 1
                                S.op("pe", lambda e: e.matmul(out=pt[:], lhsT=kcn[hs, :], rhs=qn[hs, i, qc * 512:(qc + 1) * 512], start=True, stop=False),
                                     reads=[b_kcn, b_qn[i]], writes=[bp])
                                S.op("pe", lambda e: e.matmul(out=pt[:], lhsT=identr[:], rhs=ccm[:, qc, :], start=False, stop=True),
                                     reads=[b_identr, b_ccm], writes=[bp])
                                PT, bPT = ptr_.next()
                                S.op("act", lambda e: e.activation(out=PT[:], in_=pt[:], func=AF.Exp), reads=[bp], writes=[bPT])
                                for j in range(4):
                                    S.op("pe", lambda e: e.matmul(out=po[:, j * 128:j * 128 + 98], lhsT=PT[:, j * 128:(j + 1) * 128], rhs=vcaug[:, hf, :], start=True, stop=True),
                                         reads=[bPT, b_vcaug], writes=[bpo])
                                pov = po[:].rearrange("p (j c) -> p j c", j=4)
                                S.op("dve", lambda e: e.tensor_scalar(out=small[:, 0:4], in0=pov[:, :, 64], scalar1=1e-30, scalar2=None, op0=ALU.add), reads=[bpo], writes=[b_small])
                                S.op("dve", lambda e: e.reciprocal(out=small[:, 0:4], in_=small[:, 0:4]), reads=[b_small], writes=[b_small])
                                S.op("dve", lambda e: e.tensor_tensor(out=coef[:], in0=small[:, 0:4], in1=gsig[:, qc * 4:(qc + 1) * 4, h * 3], op=ALU.mult),
                                     reads=[b_small, b_gsig], writes=[b_coef])
                                for j in range(4):
                                    tt = qc * 4 + j
                                    S.op("dve", lambda e: e.tensor_scalar(out=oacc[:, tt, hl * 64:(hl + 1) * 64], in0=po[:, j * 128:j * 128 + 64], scalar1=coef[:, j:j + 1], scalar2=None, op0=ALU.mult),
                                         reads=[bpo, b_coef], writes=[b_oacc[tt]])
                                    if i == 0:
                                        S.op("dve", lambda e: e.tensor_scalar(out=impacc[:, tt, hf, :], in0=po[:, j * 128 + 66:j * 128 + 98], scalar1=small[:, j:j + 1], scalar2=None, op0=ALU.mult),
                                             reads=[bpo, b_small], writes=[b_imp[tt]])
                                    else:
                                        S.op("dve", lambda e: e.scalar_tensor_tensor(out=impacc[:, tt, hf, :], in0=po[:, j * 128 + 66:j * 128 + 98], scalar=small[:, j:j + 1], in1=impacc[:, tt, hf, :], op0=ALU.mult, op1=ALU.add),
                                             reads=[bpo, b_small, b_imp[tt]], writes=[b_imp[tt]])
                        for hf in range(2):
                            for tt in range(NT):
                                S.op("dve", lambda e: e.tensor_tensor(out=impp[:], in0=impacc[:, tt, hf, :], in1=selv[:, tt, :], op=ALU.mult), reads=[b_imp[tt], b_selv], writes=[b_impp])
                                S.op("dve", lambda e: e.tensor_tensor(out=impp[:], in0=impp[:], in1=self_[:, tt, :], op=ALU.add), reads=[b_impp, b_self], writes=[b_impp])
                                S.op("dve", lambda e: e.max(out=top8[:], in_=impp[:]), reads=[b_impp], writes=[b_top8])
                                S.op("dve", lambda e: e.tensor_scalar(out=negs[:], in0=impp[:], scalar1=top8[:, 7:8], scalar2=NEG, op0=ALU.is_lt, op1=ALU.mult),
                                     reads=[b_impp, b_top8], writes=[b_negs])
                                pt, bp = P[6 + (tt // 4) % 2]
                                S.op("pe", lambda e: e.transpose(out=pt[0:32, (tt % 4) * 128:(tt % 4 + 1) * 128], in_=negs[:], identity=ident[:]),
                                     reads=[b_negs, b_ident], writes=[bp])
                                if tt % 4 == 3:
                                    S.op("act", lambda e: e.copy(out=negselT[:, hf, (tt // 4) * 512:(tt // 4 + 1) * 512], in_=pt[0:32, :]), reads=[bp], writes=[b_nst[hf]])
                        ns = 0
                        for (i, hf, h, hl, hs) in heads():
                            for qc in range(4):
                                for br in (1, 2):
                                    kn, b_kn, vt, bvt = (kns, b_kns, vs, b_vs) if br == 1 else (knw, b_knw, vw, b_vw)
                                    kts = list(range(0, 4 * qc + 4)) if br == 1 else list(range(max(0, 4 * qc - 4), 4 * qc + 4))
                                    for n, kt in enumerate(kts):
                                        pt, bp = P[ns % 2]
                                        ns += 1
                                        S.op("pe", lambda e: e.matmul(out=pt[:], lhsT=kn[hs, kt * 128:(kt + 1) * 128], rhs=qn[hs, i, qc * 512:(qc + 1) * 512], start=True, stop=False),
                                             reads=[b_kn, b_qn[i]], writes=[bp])
                                        if br == 1:
                                            diag = kt >= 4 * qc
                                            S.op("pe", lambda e: e.matmul(out=pt[:], lhsT=esel[:, kt, :], rhs=negselT[:, hf, qc * 512:(qc + 1) * 512], start=False, stop=not diag),
                                                 reads=[b_esel, b_nst[hf]], writes=[bp])
                                            if diag:
                                                S.op("pe", lambda e: e.matmul(out=pt[:], lhsT=identr[:], rhs=cmask[:, kt - 4 * qc, :], start=False, stop=True),
                                                     reads=[b_identr, b_cmask], writes=[bp])
                                        else:
                                            r = kt - (4 * qc - 4)
                                            mk_, bmk = (wmask[:, r, :], b_wmask) if r < 4 else (cmask[:, r - 4, :], b_cmask)
                                            S.op("pe", lambda e: e.matmul(out=pt[:], lhsT=identr[:], rhs=mk_, start=False, stop=True),
                                                 reads=[b_identr, bmk], writes=[bp])
                                        PT, bPT = ptr_.next()
                                        S.op("act", lambda e: e.activation(out=PT[:], in_=pt[:], func=AF.Exp), reads=[bp], writes=[bPT])
                                        for j in range(4):
                                            pa, bpa = P[2 + j]
                                            S.op("pe", lambda e: e.matmul(out=pa[:, 0:66], lhsT=PT[:, j * 128:(j + 1) * 128], rhs=vt[:, kt, hf, :],
                                                                          start=(n == 0), stop=(n == len(kts) - 1)),
                                                 reads=[bPT, bvt], writes=[bpa])
                                    for j in range(4):
                                        tt = qc * 4 + j
                                        pa, bpa = P[2 + j]
                                        S.op("dve", lambda e: e.reciprocal(out=small[:, 4 + j:5 + j], in_=pa[:, 64:65]), reads=[bpa], writes=[b_small])
                                        S.op("dve", lambda e: e.tensor_tensor(out=small[:, 4 + j:5 + j], in0=small[:, 4 + j:5 + j], in1=gsig[:, tt, h * 3 + br:h * 3 + br + 1], op=ALU.mult),
                                             reads=[b_small, b_gsig], writes=[b_small])
                                        S.op("dve", lambda e: e.scalar_tensor_tensor(out=oacc[:, tt, hl * 64:(hl + 1) * 64], in0=pa[:, 0:64], scalar=small[:, 4 + j:5 + j], in1=oacc[:, tt, hl * 64:(hl + 1) * 64], op0=ALU.mult, op1=ALU.add),
                                             reads=[bpa, b_small, b_oacc[tt]], writes=[b_oacc[tt]])
                        for tt in range(NT):
                            S.dma("pool", onsa_d[tt * 128:(tt + 1) * 128, pair * 512:(pair + 1) * 512], oacc[:, tt, :], reads=[b_oacc[tt]], writes=[b_onsa[tt][pair]])
            if lvl == 3:
                final_bufs += [b for bb in b_onsa for b in bb]

        if lvl >= 4:
            with phase() as sb:
                gonb = sb("gonb", [128, 1024]); b_gonb = Buf()
                S.dma("sp", gonb[:], gon_d[0:1, :].partition_broadcast(128), writes=[b_gonb])
                oring = Ring([(sb(f"ot{i}", [128, 1024]), Buf()) for i in range(2)])
                sqj = sb("sqjF", [128, 1024]); b_sqj = Buf()
                ss = sb("ssF", [128, 1]); b_ss = Buf()
                stgF = Ring([(sb(f"stgF{i}", [128, 8, 512]), Buf()) for i in range(2)])
                st_, bs = None, None
                for tt in range(NT):
                    ot, bo = oring.next()
                    S.dma("sp", ot[:], onsa_d[tt * 128:(tt + 1) * 128, :], reads=b_onsa[tt], writes=[bo])
                    S.op("act", lambda e: e.activation(out=sqj[:], in_=ot[:], func=AF.Square, accum_out=ss[:]), reads=[bo], writes=[b_sqj, b_ss])
                    S.op("dve", lambda e: e.tensor_scalar(out=ss[:], in0=ss[:], scalar1=1.0 / 1024, scalar2=EPS, op0=ALU.mult, op1=ALU.add), reads=[b_ss], writes=[b_ss])
                    S.op("act", lambda e: e.activation(out=ss[:], in_=ss[:], func=AF.Sqrt), reads=[b_ss], writes=[b_ss])
                    S.op("dve", lambda e: e.reciprocal(out=ss[:], in_=ss[:]), reads=[b_ss], writes=[b_ss])
                    S.op("dve", lambda e: e.scalar_tensor_tensor(out=ot[:], in0=ot[:], scalar=ss[:, 0:1], in1=gonb[:], op0=ALU.mult, op1=ALU.mult),
                         reads=[bo, b_ss, b_gonb], writes=[bo])
                    if tt % 4 == 0:
                        st_, bs = stgF.next()
                    for half in range(2):
                        pt, bp = P[(2 * tt + half) % 4]
                        for j in range(4):
                            ft = half * 4 + j
                            S.op("pe", lambda e: e.transpose(out=pt[:, j * 128:(j + 1) * 128], in_=ot[:, ft * 128:(ft + 1) * 128], identity=ident[:]),
                                 reads=[bo, b_ident], writes=[bp])
                        S.op("act", lambda e: e.copy(out=st_[:, half * 4:(half + 1) * 4, (tt % 4) * 128:(tt % 4 + 1) * 128], in_=pt[:].rearrange("p (a b) -> p a b", a=4)),
                             reads=[bp], writes=[bs])
                    if tt % 4 == 3:
                        tq = tt // 4
                        S.dma("pool", onT_d[0:1024, tq * 512:(tq + 1) * 512].rearrange("(f p) t -> p f t", p=128), st_[:], reads=[bs], writes=b_onT[0:8])
            if lvl == 4:
                final_bufs += b_onT

        if lvl >= 5:
            with phase() as sb:
                onT = sb("onT", [128, 16, S_LEN], F32R); b_onTs = Buf()
                for kt in range(16):
                    S.dma("sp", onT[:, kt, :], r32(onT_d[kt * 128:(kt + 1) * 128, :]), reads=[b_onT[kt]], writes=[b_onTs])
                ws = WStream(mkwring(sb), [wsrc(wout_d, c) for c in range(D // WCOL)])
                g1ring = Ring([(sb(f"g1b{i}", [128, WCOL]), Buf()) for i in range(2)])
                xcr = Ring([(sb(f"xc{i}", [128, WCOL]), Buf()) for i in range(3)])
                ocr = Ring([(sb(f"oc{i}", [128, WCOL]), Buf()) for i in range(3)])
                pi = 0
                for c in range(D // WCOL):
                    wt, bw = ws.get(c)
                    g1b, bg1 = g1ring.next()
                    S.dma("sp", g1b[:], mod_d[0:1, 2 * D + c * WCOL:2 * D + (c + 1) * WCOL].partition_broadcast(128), reads=[b_mod], writes=[bg1])
                    for tt in range(NT):
                        pt, bp = P[pi % 4]; pi += 1
                        for kt in range(16):
                            S.op("pe", lambda e: e.matmul(out=pt[:, 0:WCOL], lhsT=onT[:, kt, tt * 128:(tt + 1) * 128], rhs=wt[:, kt, :], start=(kt == 0), stop=(kt == 15)),
                                 reads=[b_onTs, bw], writes=[bp])
                        xt, bx = xcr.next()
                        S.dma("sp", xt[:], x_d[tt * 128:(tt + 1) * 128, c * WCOL:(c + 1) * WCOL], writes=[bx])
                        ot, bo = ocr.next()
                        S.op("dve", lambda e: e.tensor_tensor(out=ot[:], in0=pt[:, 0:WCOL], in1=g1b[:], op=ALU.mult), reads=[bp, bg1], writes=[bo])
                        S.op("dve", lambda e: e.tensor_tensor(out=ot[:], in0=ot[:], in1=xt[:], op=ALU.add), reads=[bo, bx], writes=[bo])
                        S.dma("pool", x1_d[tt * 128:(tt + 1) * 128, c * WCOL:(c + 1) * WCOL], ot[:], reads=[bo], writes=[b_x1[tt]])
            if lvl == 5:
                final_bufs += b_x1

        if lvl >= 6:
            with phase() as sbo:
                tokid = sbo("tokid", [128, NT, 2], I32); b_tokid = Buf()
                S.dma("sp", tokid[:], tokid_d[:, :, :], writes=[b_tokid])
                sidx = sbo("sidx", [128, NT, 4], I32); b_sidx = Buf()
                gk = sbo("gk", [128, NT, 4]); b_gk = Buf()
                with phase() as sb:
                    def ld(name, shp, src, dt=F32, reads=()):
                        t = sb(name, shp, dt); b = Buf()
                        S.dma("sp", t[:], src, reads=list(reads), writes=[b])
                        return t, b
                    maskall = sb("maskall", [128, NT, NE], F32R); b_mask = [Buf() for _ in range(NT)]
                    posall = sb("posall", [128, NT, NE]); b_pos = [Buf() for _ in range(NT)]
                    oobt = sb("oobt", [128, 2 * NE * CAP // 128], I32); b_oobt = Buf()
                    S.dma("sp", oobt[:], oobidx_d[:, :], writes=[b_oobt])
                    S.dma("sp", tokidx_d.rearrange("(p j) o -> p (j o)", p=128), oobt[:], reads=[b_oobt], writes=[b_tokidx])
                    A2b, b_A2b = ld("A2b", [128, D], mod_d[0:1, 4 * D:5 * D].partition_broadcast(128), reads=[b_mod])
                    g2b, b_g2b = ld("g2b", [128, D], g2_d[0:1, :].partition_broadcast(128))
                    B2b, b_B2b = ld("B2b", [128, D], mod_d[0:1, 3 * D:4 * D].partition_broadcast(128), reads=[b_mod])
                    S.op("dve", lambda e: e.scalar_tensor_tensor(out=A2b[:], in0=A2b[:], scalar=1.0, in1=g2b[:], op0=ALU.add, op1=ALU.mult),
                         reads=[b_A2b, b_g2b], writes=[b_A2b])
                    wr, b_wr = ld("wr", [128, 16, NE], wr_d[:, :, :])
                    brb, b_brb = ld("brb", [128, NE], br_d[0:1, :].partition_broadcast(128))
                    triu, b_triu = ld("triu", [128, 128], r32(triu_d[:, :]), F32R)
                    ones_r, b_ones = ld("ones_r", [128, 128], r32(ones_d[:, :]), F32R)
                    ebase, b_ebase = ld("ebase", [128, NE], ebase_d[:, :])
                    x1r = Ring([(sb(f"x1t{i}", [128, D]), Buf()) for i in range(2)])
                    sqj = sb("sqjH", [128, D]); b_sqj = Buf()
                    ss = sb("ssH", [128, 1]); b_ss = Buf()
                    h2f = sb("h2f", [128, D]); b_h2f = Buf()
                    h2T = sb("h2T", [128, 16, 128]); b_h2T = Buf()
                    lg = sb("lg", [128, NE]); b_lg = Buf()
                    t8 = sb("t8", [128, 8]); b_t8 = Buf()
                    ex = sb("ex", [128, NE]); b_ex = Buf()
                    nm = sb("nm", [128, 1]); b_nm = Buf()
                    gd = sb("gd", [128, 1]); b_gd = Buf()
                    e4 = sb("e4", [128, 4]); b_e4 = Buf()
                    slot = sb("slot", [128, NE]); b_slot = Buf()
                    oh = sb("oh", [128, NE]); b_oh = Buf()
                    sf = sb("sf", [128, 4]); b_sf = Buf()
                    rinfo = sb("rinfo", [128, NT, 8]); b_ri = Buf()
                    for tt in range(NT):
                        xt, bx = x1r.next()
                        S.dma("sp", xt[:], x1_d[tt * 128:(tt + 1) * 128, :], reads=[b_x1[tt]], writes=[bx])
                        S.op("act", lambda e: e.activation(out=sqj[:], in_=xt[:], func=AF.Square, accum_out=ss[:]), reads=[bx], writes=[b_sqj, b_ss])
                        S.op("dve", lambda e: e.tensor_scalar(out=ss[:], in0=ss[:], scalar1=1.0 / D, scalar2=EPS, op0=ALU.mult, op1=ALU.add), reads=[b_ss], writes=[b_ss])
                        S.op("act", lambda e: e.activation(out=ss[:], in_=ss[:], func=AF.Sqrt), reads=[b_ss], writes=[b_ss])
                        S.op("dve", lambda e: e.reciprocal(out=ss[:], in_=ss[:]), reads=[b_ss], writes=[b_ss])
                        S.op("dve", lambda e: e.scalar_tensor_tensor(out=h2f[:], in0=xt[:], scalar=ss[:, 0:1], in1=A2b[:], op0=ALU.mult, op1=ALU.mult),
                             reads=[bx, b_ss, b_A2b], writes=[b_h2f])
                        S.op("dve", lambda e: e.tensor_tensor(out=h2f[:], in0=h2f[:], in1=B2b[:], op=ALU.add), reads=[b_h2f, b_B2b], writes=[b_h2f])
                        S.dma("pool", h2_d[tt * 128:(tt + 1) * 128, :], h2f[:], reads=[b_h2f], writes=[b_h2d[tt]])
                        for g in range(4):
                            pt, bp = P[g]
                            for j in range(4):
                                dc = g * 4 + j
                                S.op("pe", lambda e: e.transpose(out=pt[:, j * 128:(j + 1) * 128], in_=h2f[:, dc * 128:(dc + 1) * 128], identity=ident[:]),
                                     reads=[b_h2f, b_ident], writes=[bp])
                            if g % 2 == 0:
                                S.op("act", lambda e: e.copy(out=h2T[:, g * 4:(g + 1) * 4, :], in_=pt[:].rearrange("p (a b) -> p a b", a=4)), reads=[bp], writes=[b_h2T])
                            else:
                                S.op("dve", lambda e: e.tensor_copy(out=h2T[:, g * 4:(g + 1) * 4, :], in_=pt[:].rearrange("p (a b) -> p a b", a=4)), reads=[bp], writes=[b_h2T])
                        pl, bpl = P[4]
                        for dc in range(16):
                            S.op("pe", lambda e: e.matmul(out=pl[:, 0:NE], lhsT=h2T[:, dc, :], rhs=wr[:, dc, :], start=(dc == 0), stop=(dc == 15)),
                                 reads=[b_h2T, b_wr], writes=[bpl])
                        S.op("dve", lambda e: e.tensor_tensor(out=lg[:], in0=pl[:, 0:NE], in1=brb[:], op=ALU.add), reads=[bpl, b_brb], writes=[b_lg])
                        S.op("dve", lambda e: e.max(out=t8[:], in_=lg[:]), reads=[b_lg], writes=[b_t8])
                        S.op("dve", lambda e: e.tensor_scalar(out=maskall[:, tt, :], in0=lg[:], scalar1=t8[:, 3:4], scalar2=None, op0=ALU.is_ge),
                             reads=[b_lg, b_t8], writes=[b_mask[tt]])
                        S.op("dve", lambda e: e.tensor_scalar(out=nm[:], in0=t8[:, 0:1], scalar1=-1.0, scalar2=None, op0=ALU.mult), reads=[b_t8], writes=[b_nm])
                        S.op("act", lambda e: e.activation(out=e4[:], in_=t8[:, 0:4], func=AF.Exp, bias=nm[:, 0:1], accum_out=gd[:]), reads=[b_t8, b_nm], writes=[b_e4, b_gd])
                        S.op("dve", lambda e: e.reciprocal(out=gd[:], in_=gd[:]), reads=[b_gd], writes=[b_gd])
                        S.op("dve", lambda e: e.tensor_scalar(out=gk[:, tt, :], in0=e4[:], scalar1=gd[:, 0:1], scalar2=None, op0=ALU.mult), reads=[b_e4, b_gd], writes=[b_gk])
                        pp, bpp = P[5 + tt % 2]
                        for t2 in range(tt):
                            S.op("pe", lambda e: e.matmul(out=pp[:, 0:NE], lhsT=ones_r[:], rhs=maskall[:, t2, :], start=(t2 == 0), stop=False),
                                 reads=[b_ones, b_mask[t2]], writes=[bpp])
                        S.op("pe", lambda e: e.matmul(out=pp[:, 0:NE], lhsT=triu[:], rhs=maskall[:, tt, :], start=(tt == 0), stop=True),
                             reads=[b_triu, b_mask[tt]], writes=[bpp])
                        S.op("act", lambda e: e.copy(out=posall[:, tt, :], in_=pp[:, 0:NE]), reads=[bpp], writes=[b_pos[tt]])
                        S.op("dve", lambda e: e.tensor_tensor(out=slot[:], in0=posall[:, tt, :], in1=ebase[:], op=ALU.add), reads=[b_pos[tt], b_ebase], writes=[b_slot])
                        for k in range(4):
                            S.op("dve", lambda e: e.tensor_scalar(out=oh[:], in0=lg[:], scalar1=t8[:, k:k + 1], scalar2=None, op0=ALU.is_equal), reads=[b_lg, b_t8], writes=[b_oh])
                            S.op("dve", lambda e: e.tensor_tensor(out=oh[:], in0=oh[:], in1=slot[:], op=ALU.mult), reads=[b_oh, b_slot], writes=[b_oh])
                            S.op("dve", lambda e: e.reduce_sum(out=sf[:, k:k + 1], in_=oh[:], axis=mybir.AxisListType.X), reads=[b_oh], writes=[b_sf])
                        S.op("dve", lambda e: e.tensor_copy(out=sidx[:, tt, :], in_=sf[:]), reads=[b_sf], writes=[b_sidx])
                        for k in range(4):
                            S.dma("pool", None, None, reads=[b_sidx, b_tokid], writes=[b_tokidx],
                                  fn=lambda e: e.indirect_dma_start(out=tokidx_d[:, :], out_offset=bass.IndirectOffsetOnAxis(ap=sidx[:, tt, k:k + 1], axis=0),
                                                                    in_=tokid[:, tt, :], in_offset=None,
                                                                    bounds_check=reg_slot, oob_is_err=False))
                        if dbg:
                            S.op("dve", lambda e: e.tensor_copy(out=rinfo[:, tt, 0:4], in_=sf[:]), reads=[b_sf], writes=[b_ri])
                            S.op("dve", lambda e: e.tensor_copy(out=rinfo[:, tt, 4:8], in_=gk[:, tt, :]), reads=[b_gk], writes=[b_ri])
                    if dbg:
                        S.dma("sp", rinfo_d.rearrange("(t p) c -> p t c", p=128), rinfo[:], reads=[b_ri], writes=[b_rinfo])
                if lvl == 6:
                    final_bufs += [b_rinfo]

                if lvl >= 7:
                    with phase() as sb:
                        b1r = Ring([(sb(f"b1c{i}", [128, 32]), Buf()) for i in range(2)])
                        ones_r = sb("ones_rE", [1, 128], F32R); b_ones = Buf()
                        S.dma("sp", ones_r[:], r32(ones_d[0:1, :]), writes=[b_ones])
                        xbT = sb("xbT", [128, 16, CAP], F32R); b_xbT = [Buf() for _ in range(NST)]
                        tact = sb("tact", [128, 16, CAP], F32R); b_tact = [Buf() for _ in range(16)]
                        ug = sb("ug", [128, 512]); b_ug = Buf()
                        sg = sb("sg", [128, 512]); b_sg = Buf()
                        idxr = Ring([(sb(f"idxe{i}", [128, NST], I32), Buf()) for i in range(2)])
                        xbr = Ring([(sb(f"xb{i}", [128, D]), Buf()) for i in range(2)])
                        for xt_, bx_ in xbr.items:
                            S.op("pool", lambda e: e.memset(xt_[:], 0.0), writes=[bx_])
                        g2ring = Ring([(sb(f"g2b{i}", [128, WCOL]), Buf()) for i in range(2)])
                        b2ring = Ring([(sb(f"b2r{i}", [1, WCOL], F32R), Buf()) for i in range(4)])
                        yring = Ring([(sb(f"yst{i}", [128, WCOL]), Buf()) for i in range(3)])
                        srcs = []
                        for e_ in range(NE):
                            srcs += [wsrc(we1_d[e_], c) for c in range(2 * D // WCOL)]
                            srcs += [wsrc(we2_d[e_], c) for c in range(D // WCOL)]
                        ws = WStream(mkwring(sb, 3), srcs, ahead=2)
                        wi = 0
                        pi = 0

                        def load_idx(e_):
                            idxe, bidx = idxr.next()
                            S.dma("sp", None, None, reads=[b_tokidx], writes=[bidx],
                                  fn=lambda e: e.dma_start(out=idxe[:], in_=tokidx_d[e_ * CAP:(e_ + 1) * CAP, 0:1].rearrange("(s p) o -> p (s o)", p=128),
                                                           allow_slow_non_contiguous=True))
                            return idxe, bidx

                        def gather(idxe, bidx, st):
                            xb, bxb = xbr.next()
                            S.dma("pool", None, None, reads=[bidx] + b_h2d, writes=[bxb],
                                  fn=lambda e: e.indirect_dma_start(out=xb[:, :], out_offset=None, in_=h2_d[:, :],
                                                                    in_offset=bass.IndirectOffsetOnAxis(ap=idxe[:, st:st + 1], axis=0),
                                                                    bounds_check=reg_tok, oob_is_err=False))
                            return xb, bxb

                        def transp(xb, bxb, st):
                            for g in range(4):
                                pt, bp = P[4 + g]
                                for j in range(4):
                                    dc = g * 4 + j
                                    S.op("pe", lambda e: e.transpose(out=pt[:, j * 128:(j + 1) * 128], in_=xb[:, dc * 128:(dc + 1) * 128], identity=ident[:]),
                                         reads=[bxb, b_ident], writes=[bp])
                                if g % 2 == 0:
                                    S.op("act", lambda e: e.copy(out=xbT[:, g * 4:(g + 1) * 4, st * 128:(st + 1) * 128], in_=pt[:].rearrange("p (a b) -> p a b", a=4)),
                                         reads=[bp], writes=[b_xbT[st]])
                                else:
                                    S.op("dve", lambda e: e.tensor_copy(out=xbT[:, g * 4:(g + 1) * 4, st * 128:(st + 1) * 128], in_=pt[:].rearrange("p (a b) -> p a b", a=4)),
                                         reads=[bp], writes=[b_xbT[st]])

                        idx0, bidx0 = load_idx(0)
                        for st in range(NST):
                            xb, bxb = gather(idx0, bidx0, st)
                            transp(xb, bxb, st)
                        for e_ in range(NE):
                            b1c, b_b1c = b1r.next()
                            S.dma("sp", b1c[:], b1c_d[:, e_, :], writes=[b_b1c])
                            for c in range(2 * D // WCOL):
                                wt, bw = ws.get(wi); wi += 1
                                for ftl in range(2):
                                    F = c * 2 + ftl
                                    for hv in range(2):
                                        cs = slice(hv * 512, (hv + 1) * 512)
                                        pt, bp = P[pi % 4]; pi += 1
                                        for kt in range(16):
                                            S.op("pe", lambda e: e.matmul(out=pt[:], lhsT=wt[:, kt, ftl * 128:(ftl + 1) * 128], rhs=xbT[:, kt, cs], start=(kt == 0), stop=(kt == 15)),
                                                 reads=[bw] + b_xbT[hv * 4:(hv + 1) * 4], writes=[bp])
                                        S.op("dve", lambda e: e.tensor_scalar(out=ug[:], in0=pt[:], scalar1=b1c[:, F:F + 1], scalar2=7.0, op0=ALU.add, op1=ALU.min),
                                             reads=[bp, b_b1c], writes=[b_ug])
                                        if F < 16:
                                            S.op("act", lambda e: e.activation(out=sg[:], in_=ug[:], func=AF.Sigmoid, scale=1.702), reads=[b_ug], writes=[b_sg])
                                            S.op("dve", lambda e: e.tensor_tensor(out=tact[:, F, cs], in0=ug[:], in1=sg[:], op=ALU.mult), reads=[b_ug, b_sg], writes=[b_tact[F]])
                                        else:
                                            Fl = F - 16
                                            S.op("dve", lambda e: e.tensor_scalar(out=ug[:], in0=ug[:], scalar1=-7.0, scalar2=1.0, op0=ALU.max, op1=ALU.add), reads=[b_ug], writes=[b_ug])
                                            S.op("dve", lambda e: e.tensor_tensor(out=tact[:, Fl, cs], in0=f32(tact[:, Fl, cs]), in1=ug[:], op=ALU.mult), reads=[b_ug, b_tact[Fl]], writes=[b_tact[Fl]])
                            if e_ + 1 < NE:
                                idxn, bidxn = load_idx(e_ + 1)
                            for c in range(D // WCOL):
                                wt, bw = ws.get(wi); wi += 1
                                if e_ + 1 < NE:
                                    xbn, bxbn = gather(idxn, bidxn, c)
                                g2b_, bg2 = g2ring.next()
                                S.dma("sp", g2b_[:], mod_d[0:1, 5 * D + c * WCOL:5 * D + (c + 1) * WCOL].partition_broadcast(128), reads=[b_mod], writes=[bg2])
                                b2r, bb2 = b2ring.next()
                                S.dma("sp", b2r[:], r32(be2_d[e_:e_ + 1, c * WCOL:(c + 1) * WCOL]), writes=[bb2])
                                for st in range(NST):
                                    pt, bp = P[pi % 4]; pi += 1
                                    for kt in range(16):
                                        S.op("pe", lambda e: e.matmul(out=pt[:, 0:WCOL], lhsT=tact[:, kt, st * 128:(st + 1) * 128], rhs=wt[:, kt, :], start=(kt == 0), stop=False),
                                             reads=[b_tact[kt], bw], writes=[bp])
                                    S.op("pe", lambda e: e.matmul(out=pt[:, 0:WCOL], lhsT=ones_r[0:1, :], rhs=b2r[0:1, :], start=False, stop=True),
                                         reads=[b_ones, bb2], writes=[bp])
                                    yt, by = yring.next()
                                    S.op("dve", lambda e: e.tensor_tensor(out=yt[:], in0=pt[:, 0:WCOL], in1=g2b_[:], op=ALU.mult), reads=[bp, bg2], writes=[by])
                                    r0 = e_ * CAP + st * 128
                                    S.dma("pool", Y_d[r0:r0 + 128, c * WCOL:(c + 1) * WCOL], yt[:], reads=[by], writes=[b_Y[e_][st]])
                                if e_ + 1 < NE:
                                    transp(xbn, bxbn, c)
                    if lvl == 7:
                        final_bufs += [b for bb in b_Y for b in bb]

                if lvl >= 8:
                    with phase() as sb:
                        x1r = Ring([(sb(f"x1c{i}", [128, D]), Buf()) for i in range(2)])
                        ykr = Ring([(sb(f"yk{i}", [128, D]), Buf()) for i in range(4)])
                        allY = [b for bb in b_Y for b in bb]
                        for tt in range(NT):
                            xt, bx = x1r.next()
                            S.dma("sp", xt[:], x1_d[tt * 128:(tt + 1) * 128, :], reads=[b_x1[tt]], writes=[bx])
                            for k in range(4):
                                yk, byk = ykr.next()
                                S.dma("pool", None, None, reads=allY + [b_sidx], writes=[byk],
                                      fn=lambda e: e.indirect_dma_start(out=yk[:, :], out_offset=None, in_=Y_d[:, :],
                                                                        in_offset=bass.IndirectOffsetOnAxis(ap=sidx[:, tt, k:k + 1], axis=0),
                                                                        bounds_check=reg_slot, oob_is_err=False))
                                S.op("dve", lambda e: e.scalar_tensor_tensor(out=xt[:], in0=yk[:], scalar=gk[:, tt, k:k + 1], in1=xt[:], op0=ALU.mult, op1=ALU.add),
                                     reads=[byk, b_gk, bx], writes=[bx])
                            S.dma("sp", out_d[tt * 128:(tt + 1) * 128, :], xt[:], reads=[bx], writes=[b_out[tt]])
                    final_bufs += b_out

        S.finish(final_bufs)
        S.barrier()
    return nc


def _consts():
    c = {}
    c["ident"] = np.eye(128, dtype=np.float32)
    bo = np.zeros((128, 128), np.float32); bo[:64, :64] = 1; bo[64:, 64:] = 1
    c["bones"] = bo
    c["ones"] = np.ones((128, 128), np.float32)
    p = np.arange(128)[:, None]; col = np.arange(512)[None, :]
    cm = np.zeros((128, 4, 512), np.float32); wm = np.zeros((128, 4, 512), np.float32); cc = np.zeros((128, 4, 512), np.float32)
    for r in range(4):
        cm[:, r, :] = np.where(r * 128 + p <= col, 0.0, NEG)
        wm[:, r, :] = np.where(p + r * 128 > col, 0.0, NEG)
        cc[:, r, :] = np.where(16 * p + 31 <= r * 512 + col, 0.0, NEG)
    c["cmask"], c["wmask"], c["ccm"] = cm, wm, cc
    es = np.zeros((32, 16, 128), np.float32)
    for kt in range(16):
        for pp in range(128):
            es[2 * kt + pp // 64, kt, pp] = 1.0
    c["esel"] = es
    t = np.arange(S_LEN); qb = t // 64; j = np.arange(32)
    valid = j[None, :] <= qb[:, None]
    forced = (j[None, :] == 0) | (j[None, :] == qb[:, None]) | (j[None, :] == qb[:, None] - 1)
    V = (valid & ~forced).astype(np.float32)
    Fm = np.where(forced, 1e30, np.where(valid, 0.0, -1e30)).astype(np.float32)
    c["selv"] = np.ascontiguousarray(V.reshape(16, 128, 32).transpose(1, 0, 2))
    c["self"] = np.ascontiguousarray(Fm.reshape(16, 128, 32).transpose(1, 0, 2))
    cs = np.arange(128) * 16; ss = np.arange(32) * 64
    ov = ((cs[:, None] < ss[None, :] + 64) & (cs[:, None] + 32 > ss[None, :])).astype(np.float32)
    vc0 = np.zeros((128, 98), np.float32); vc0[:, 64] = 1.0; vc0[:, 66:98] = ov
    c["vcaug0"] = vc0
    va0 = np.zeros((128, NT, 2, 66), np.float32); va0[..., 64] = 1.0
    c["vaug0"] = va0
    c["triu"] = np.triu(np.ones((128, 128), np.float32), k=1)
    tk = (np.arange(NT, dtype=np.int32)[None, :] * 128 + np.arange(128, dtype=np.int32)[:, None]).astype(np.int32)
    c["tokid"] = np.ascontiguousarray(np.stack([tk, tk], axis=-1))
    c["oobidx"] = np.full((128, 2 * NE * CAP // 128), 4095, np.int32)
    c["iotas"] = np.broadcast_to(np.arange(CAP, dtype=np.float32)[None, :], (128, CAP)).copy()
    c["ebase"] = np.broadcast_to((np.arange(NE, dtype=np.float32) * CAP)[None, :], (128, NE)).copy()
    return c


def _cols(v, n=None):
    v = np.asarray(v, np.float32).reshape(-1, 128)
    return np.ascontiguousarray(v.T)


def prep_shared(inp):
    f = lambda k: np.asarray(inp[k], np.float32)[0]
    sh = dict(_consts())
    w_in = f("w_in")
    sh["wA"] = np.ascontiguousarray(np.concatenate([w_in[:, 0:1024], w_in[:, 1024:1280], w_in[:, 1280:1536], w_in[:, 1536:1792],
                                                    w_in[:, 2048:2304], w_in[:, 2608:3632], w_in[:, 3632:4656]], axis=1))
    wB = np.zeros((D, 768), np.float32)
    wB[:, 0:256] = w_in[:, 1792:2048]; wB[:, 256:512] = w_in[:, 2304:2560]; wB[:, 512:560] = w_in[:, 2560:2608]
    sh["wB"] = wB
    sh["w_ada"] = f("w_ada"); sh["b_ada"] = f("b_ada").reshape(1, -1)
    sh["g1c"] = _cols(f("g_norm1"))
    wck = f("w_cmp_k").reshape(32, 64, 64).transpose(1, 0, 2)
    wcv = f("w_cmp_v").reshape(32, 64, 64).transpose(1, 0, 2)
    wbd = np.zeros((128, 32, 128), np.float32)
    wbd[0:64, :, 0:64] = wck; wbd[64:128, :, 64:128] = wck
    sh["wck"] = wbd
    sh["wcv"] = np.ascontiguousarray(np.concatenate([wcv, wcv], axis=0))
    pek = f("pe_cmp_k").T; pev = f("pe_cmp_v").T
    pek = np.concatenate([pek, pek], axis=0)
    sh["pek2"] = np.ascontiguousarray(np.stack([pek, pek], axis=-1))
    sh["pevT"] = np.ascontiguousarray(np.concatenate([pev, pev], axis=0))
    qg = f("q_gain"); sh["qg"] = np.concatenate([qg, qg]).reshape(128, 1).copy()
    kgn = f("k_gain").T; sh["kg"] = np.ascontiguousarray(np.concatenate([kgn, kgn], axis=0))
    sh["cw"] = np.ascontiguousarray(f("conv_w").reshape(4, 8, 128).transpose(2, 1, 0))
    sh["cb"] = _cols(f("conv_b"))
    for nm, key in (("wrg", "w_rg"), ("wig", "w_ig")):
        w = f(key)
        bd = np.zeros((128, 8, 128), np.float32)
        for ct in range(8):
            bd[0:64, ct, 0:64] = w[2 * ct]; bd[64:128, ct, 64:128] = w[2 * ct + 1]
        sh[nm] = bd
    sh["brg"] = _cols(f("b_rg").reshape(-1)); sh["big"] = _cols(f("b_ig").reshape(-1))
    sh["lam"] = _cols(f("lru_lambda"))
    sh["gon"] = f("g_out_nsa").reshape(1, -1); sh["gol"] = _cols(f("g_out_lru"))
    sh["w_out"] = f("w_out"); sh["g2"] = f("g_norm2").reshape(1, -1)
    sh["wr"] = np.ascontiguousarray(f("w_router").reshape(16, 128, NE).transpose(1, 0, 2))
    sh["br"] = f("b_router").reshape(1, -1)
    sh["w_e1"] = f("w_e1"); sh["w_e2"] = f("w_e2")
    sh["b1c"] = np.ascontiguousarray(f("b_e1").reshape(NE, 32, 128).transpose(2, 0, 1))
    sh["b_e2"] = f("b_e2")
    return sh


def core_inputs(inp, sh, b):
    m = dict(sh)
    m["x"] = np.ascontiguousarray(np.asarray(inp["x"], np.float32)[b])
    m["csil"] = _cols(np.asarray(inp["c"], np.float32)[b])
    return m


_NC_CACHE = {}


def kernel(**inputs):
    sh = prep_shared(inputs)
    if "nc" not in _NC_CACHE:
        _NC_CACHE["nc"] = build_nc("all", False)
    nc = _NC_CACHE["nc"]
    in_maps = [core_inputs(inputs, sh, b) for b in range(8)]
    res = run_bass_kernel_spmd(nc, in_maps, core_ids=list(range(8)))
    return np.stack([np.asarray(r["out"], np.float32) for r in res.results], axis=0)
```

```python
from contextlib import ExitStack, contextmanager

import numpy as np
import concourse.bass as bass
import concourse.mybir as mybir
from concourse.bass_utils import run_bass_kernel_spmd

F32 = mybir.dt.float32
F32R = mybir.dt.float32r
BF16 = mybir.dt.bfloat16
I32 = mybir.dt.int32
AF = mybir.ActivationFunctionType
ALU = mybir.AluOpType

S_LEN = 2048
D = 2048
NT = 16
NE = 32
CAP = 1024
NST = CAP // 128
EPS = 1e-6
NEG = -30000.0
WCOL = 256


class Buf:
    __slots__ = ("name", "w", "r")

    def __init__(self, name=""):
        self.name = name
        self.w = None
        self.r = {}


class Sched:
    def __init__(self, nc, stack, n_dma_slots=12):
        self.nc = nc
        self.eng = {"pe": nc.tensor, "act": nc.scalar, "dve": nc.vector, "pool": nc.gpsimd, "sp": nc.sync}
        self.sem = {k: stack.enter_context(nc.semaphore("s_" + k)) for k in self.eng}
        self.cnt = {k: 0 for k in self.eng}
        self.waited = {k: {} for k in self.eng}
        self.slots = {}
        for q in ("sp", "pool"):
            self.slots[q] = [[stack.enter_context(nc.semaphore(f"d_{q}{i}")), 0] for i in range(n_dma_slots)]
        self.slot_i = {q: 0 for q in self.slots}
        self.nwaits = 0

    def _wait(self, e, ev):
        if ev is None:
            return
        sem, val = ev
        if e == "pe" and sem is self.sem["pe"]:
            return
        key = id(sem)
        if self.waited[e].get(key, 0) >= val:
            return
        self.eng[e].wait_ge(sem, val)
        self.waited[e][key] = val
        self.nwaits += 1

    def _deps(self, e, reads, writes):
        for b in reads:
            self._wait(e, b.w)
        for b in writes:
            self._wait(e, b.w)
            for ev in b.r.values():
                self._wait(e, ev)

    def _mark(self, ev, reads, writes):
        sem, val = ev
        for b in reads:
            b.r[id(sem)] = ev
        for b in writes:
            b.w = ev
            b.r = {}

    def op(self, e, fn, reads=(), writes=()):
        self._deps(e, reads, writes)
        ins = fn(self.eng[e])
        self.cnt[e] += 1
        ins.then_inc(self.sem[e], 1)
        ev = (self.sem[e], self.cnt[e])
        self._mark(ev, reads, writes)
        return ev

    def dma(self, q, out, in_, reads=(), writes=(), fn=None):
        slots = self.slots[q]
        i = self.slot_i[q]
        self.slot_i[q] = (i + 1) % len(slots)
        sem, uses = slots[i]
        if uses:
            self._wait(q, (sem, 16 * uses))
        self._deps(q, reads, writes)
        if fn is None:
            ins = self.eng[q].dma_start(out=out, in_=in_)
        else:
            ins = fn(self.eng[q])
        ins.then_inc(sem, 16)
        slots[i][1] = uses + 1
        ev = (sem, 16 * (uses + 1))
        self._mark(ev, reads, writes)
        return ev

    def barrier(self):
        evs = [(self.sem[k], self.cnt[k]) for k in self.eng if self.cnt[k] > 0]
        for q in self.slots:
            for sem, uses in self.slots[q]:
                if uses:
                    evs.append((sem, 16 * uses))
        for e in self.eng:
            for ev in evs:
                self._wait(e, ev)

    def finish(self, bufs):
        for b in bufs:
            self._wait("sp", b.w)
            for ev in b.r.values():
                self._wait("sp", ev)


class Ring:
    def __init__(self, items):
        self.items = items
        self.i = 0

    def next(self):
        it = self.items[self.i]
        self.i = (self.i + 1) % len(self.items)
        return it


def r32(ap):
    return ap.bitcast(F32R)


def f32(ap):
    return ap.bitcast(F32)


def build_nc(upto="all", dbg=False):
    nc = bass.Bass("TRN2", target_bir_lowering=False)
    nc.dge_precook = False
    order = ["mod", "inproj", "lru", "nsa", "nsaout", "outproj", "router", "experts", "combine", "all"]
    lvl = order.index(upto)

    def din(n, shp, dt=F32):
        return nc.dram_tensor(n, list(shp), dt, kind="ExternalInput").ap()

    dbg_sets = {"mod": ["mod_s"], "inproj": ["zT_s", "ztm_s"], "lru": ["onT_s"], "nsa": ["onsa_s"], "nsaout": ["onT_s"],
                "outproj": ["x1_s"], "router": ["rinfo_s"], "experts": [], "combine": [], "all": []}

    def dscr(n, shp, dt=F32):
        ext = dbg and n in dbg_sets[upto]
        return nc.dram_tensor(n, list(shp), dt, kind=("ExternalOutput" if ext else "Internal")).ap()

    x_d = din("x", [S_LEN, D])
    csil_d = din("csil", [128, 16])
    wada_d = din("w_ada", [D, 6 * D])
    bada_d = din("b_ada", [1, 6 * D])
    g1c_d = din("g1c", [128, 16])
    wA_d = din("wA", [D, 4096])
    wB_d = din("wB", [D, 768])
    wck_d = din("wck", [128, 32, 128])
    wcv_d = din("wcv", [128, 32, 64])
    pek2_d = din("pek2", [128, 32, 2])
    pevT_d = din("pevT", [128, 32])
    qg_d = din("qg", [128, 1])
    kg_d = din("kg", [128, 3])
    cw_d = din("cw", [128, 8, 4])
    cb_d = din("cb", [128, 8])
    wrg_d = din("wrg", [128, 8, 128])
    wig_d = din("wig", [128, 8, 128])
    brg_d = din("brg", [128, 8])
    big_d = din("big", [128, 8])
    lam_d = din("lam", [128, 8])
    gon_d = din("gon", [1, 1024])
    gol_d = din("gol", [128, 8])
    wout_d = din("w_out", [D, D])
    g2_d = din("g2", [1, D])
    wr_d = din("wr", [128, 16, NE])
    br_d = din("br", [1, NE])
    we1_d = din("w_e1", [NE, D, 2 * D]) if lvl >= 7 else None
    b1c_d = din("b1c", [128, NE, 32])
    we2_d = din("w_e2", [NE, D, D]) if lvl >= 7 else None
    be2_d = din("b_e2", [NE, D])
    ident_d = din("ident", [128, 128])
    bones_d = din("bones", [128, 128])
    cmask_d = din("cmask", [128, 4, 512])
    wmask_d = din("wmask", [128, 4, 512])
    ccm_d = din("ccm", [128, 4, 512])
    esel_d = din("esel", [32, 16, 128])
    selv_d = din("selv", [128, 16, 32])
    self_d = din("self", [128, 16, 32])
    triu_d = din("triu", [128, 128])
    iotas_d = din("iotas", [128, CAP])
    ebase_d = din("ebase", [128, NE])
    ones_d = din("ones", [128, 128])
    tokid_d = din("tokid", [128, NT, 2], I32)
    oobidx_d = din("oobidx", [128, 2 * NE * CAP // 128], I32)
    vaug0_d = din("vaug0", [128, NT, 2, 66])
    vcaug0_d = din("vcaug0", [128, 98])

    out_d = nc.dram_tensor("out", [S_LEN, D], F32, kind="ExternalOutput").ap()
    mod_d = dscr("mod_s", [1, 6 * D])
    zT_d = dscr("zT_s", [4096, S_LEN])
    ztm_d = dscr("ztm_s", [S_LEN, 768])
    onsa_d = dscr("onsa_s", [S_LEN, 1024])
    onT_d = dscr("onT_s", [D, S_LEN])
    x1_d = dscr("x1_s", [S_LEN, D])
    Y_d = dscr("Y_s", [NE * CAP, D])
    rinfo_d = dscr("rinfo_s", [S_LEN, 8])
    h2_d = dscr("h2_s", [S_LEN, D])
    tokidx_d = dscr("tokidx_s", [NE * CAP, 2], I32)
    b_h2d = [Buf() for _ in range(NT)]
    b_tokidx = Buf("tokidx")

    b_mod = Buf("mod_d")
    b_zT = [Buf(f"zT{i}") for i in range(32)]
    b_ztm = [Buf(f"ztm{i}") for i in range(NT)]
    b_onsa = [[Buf() for _ in range(2)] for _ in range(NT)]
    b_onT = [Buf(f"onT{i}") for i in range(16)]
    b_x1 = [Buf(f"x1{i}") for i in range(NT)]
    b_Y = [[Buf() for _ in range(NST)] for _ in range(NE)]
    b_out = [Buf(f"out{i}") for i in range(NT)]
    b_rinfo = Buf("rinfo")
    final_bufs = []

    with ExitStack() as top:
        S = Sched(nc, top)
        reg_slot = nc.gpsimd.to_reg(NE * CAP - 1)
        reg_tok = nc.gpsimd.to_reg(S_LEN - 1)

        uniq = [0]

        def mk(stack):
            def sb(n, shp, dt=F32):
                uniq[0] += 1
                return stack.enter_context(nc.sbuf_tensor(f"sb{uniq[0]}_{n}", list(shp), dt))
            return sb

        sbt = mk(top)

        @contextmanager
        def phase():
            with ExitStack() as ph_:
                yield mk(ph_)
                S.barrier()
        P = [(top.enter_context(nc.psum_tensor(f"P{i}", [128, 512], F32)), Buf(f"P{i}")) for i in range(8)]

        ident = sbt("ident", [128, 128]); b_ident = Buf("ident")
        S.dma("sp", ident[:], ident_d[:, :], writes=[b_ident])
        identr = sbt("identr", [128, 128], F32R); b_identr = Buf("identr")
        S.dma("sp", identr[:], r32(ident_d[:, :]), writes=[b_identr])
        def mkwring(sb_, n=3):
            return Ring([(sb_(f"wch{i}", [128, 16, WCOL], F32R), Buf(f"wch{i}")) for i in range(n)])

        def wsrc(w2d, c):
            return r32(w2d[:, c * WCOL:(c + 1) * WCOL]).rearrange("(kt p) f -> p kt f", p=128)

        class WStream:
            def __init__(self, wring, srcs, ahead=2):
                self.wring = wring
                self.srcs = srcs
                self.loaded = []
                self.ahead = ahead

            def get(self, k):
                while len(self.loaded) <= min(k + self.ahead, len(self.srcs) - 1):
                    t, b = self.wring.next()
                    S.dma("sp", t[:], self.srcs[len(self.loaded)], writes=[b])
                    self.loaded.append((t, b))
                return self.loaded[k]

        with phase() as sb:
            cs = sb("cs", [128, 16]); b_cs = Buf()
            S.dma("sp", cs[:], csil_d[:, :], writes=[b_cs])
            scr = sb("scr", [128, 16], F32R); b_scr = Buf()
            S.op("act", lambda e: e.activation(out=scr[:], in_=cs[:], func=AF.Silu), reads=[b_cs], writes=[b_scr])
            barow = Ring([(sb(f"barow{i}", [1, WCOL]), Buf()) for i in range(3)])
            mrow = Ring([(sb(f"mrow{i}", [1, WCOL]), Buf()) for i in range(3)])
            ws = WStream(mkwring(sb), [wsrc(wada_d, c) for c in range(6 * D // WCOL)])
            for c in range(6 * D // WCOL):
                wt, bw = ws.get(c)
                bt, bb = barow.next()
                S.dma("sp", bt[:], bada_d[:, c * WCOL:(c + 1) * WCOL], writes=[bb])
                pt, bp = P[c % 2]
                for kt in range(16):
                    S.op("pe", lambda e: e.matmul(out=pt[0:1, 0:WCOL], lhsT=scr[:, kt:kt + 1], rhs=wt[:, kt, :],
                                                  start=(kt == 0), stop=(kt == 15)),
                         reads=[b_scr, bw], writes=[bp])
                mt, bm = mrow.next()
                S.op("dve", lambda e: e.tensor_tensor(out=mt[:], in0=pt[0:1, 0:WCOL], in1=bt[:], op=ALU.add),
                     reads=[bp, bb], writes=[bm])
                S.dma("sp", mod_d[:, c * WCOL:(c + 1) * WCOL], mt[:], reads=[bm], writes=[b_mod])
        if lvl == 0:
            final_bufs.append(b_mod)

        def mod_cols(stack_sb, name, off):
            t = stack_sb(name, [128, 16]); b = Buf()
            S.dma("sp", None, None, reads=[b_mod], writes=[b],
                  fn=lambda e: e.dma_start(out=t[:], in_=mod_d[:, off:off + D].rearrange("o (c p) -> p (o c)", p=128),
                                           allow_slow_non_contiguous=True))
            return t, b

        def bc_load(t_ap, row_ap, reads, b):
            S.dma("sp", t_ap, row_ap.partition_broadcast(128), reads=reads, writes=[b])

        if lvl >= 1:
            with phase() as sb:
                sh1, b_sh1 = mod_cols(sb, "sh1", 0)
                sc1, b_sc1 = mod_cols(sb, "sc1", D)
                g1c = sb("g1c", [128, 16]); b_g1c = Buf()
                S.dma("sp", g1c[:], g1c_d[:, :], writes=[b_g1c])
                A1c = sb("A1c", [128, 16]); b_A1c = Buf()
                S.op("dve", lambda e: e.scalar_tensor_tensor(out=A1c[:], in0=sc1[:], scalar=1.0, in1=g1c[:], op0=ALU.add, op1=ALU.mult),
                     reads=[b_sc1, b_g1c], writes=[b_A1c])
                hT = sb("hT", [128, 16, S_LEN], F32R)
                b_hT = [Buf(f"hT{i}") for i in range(NT)]
                tmp = ExitStack(); sb2 = mk(tmp)
                xring = Ring([(sb2(f"xin{i}", [128, D]), Buf()) for i in range(2)])
                xs = sb2("xs", [128, D]); b_xs = Buf()
                sqj = sb2("sqj", [128, D]); b_sqj = Buf()
                ss = sb2("ss", [128, 1]); b_ss = Buf()
                rs = sb2("rs", [128, 1]); b_rs = Buf()
                for tt in range(NT):
                    xt, bx = xring.next()
                    S.dma("sp", xt[:], x_d[tt * 128:(tt + 1) * 128, :], writes=[bx])
                    S.op("act", lambda e: e.activation(out=sqj[:], in_=xt[:], func=AF.Square, accum_out=ss[:]),
                         reads=[bx], writes=[b_sqj, b_ss])
                    S.op("dve", lambda e: e.tensor_scalar(out=rs[:], in0=ss[:], scalar1=1.0 / D, scalar2=EPS, op0=ALU.mult, op1=ALU.add),
                         reads=[b_ss], writes=[b_rs])
                    S.op("act", lambda e: e.activation(out=rs[:], in_=rs[:], func=AF.Sqrt), reads=[b_rs], writes=[b_rs])
                    S.op("dve", lambda e: e.reciprocal(out=rs[:], in_=rs[:]), reads=[b_rs], writes=[b_rs])
                    S.op("dve", lambda e: e.tensor_scalar(out=xs[:], in0=xt[:], scalar1=rs[:, 0:1], scalar2=None, op0=ALU.mult),
                         reads=[bx, b_rs], writes=[b_xs])
                    for g in range(4):
                        pt, bp = P[4 + g % 4]
                        for j in range(4):
                            dc = g * 4 + j
                            S.op("pe", lambda e: e.transpose(out=pt[:, j * 128:(j + 1) * 128], in_=xs[:, dc * 128:(dc + 1) * 128], identity=ident[:]),
                                 reads=[b_xs, b_ident], writes=[bp])
                        for j in range(4):
                            dc = g * 4 + j
                            eng = "dve" if j % 2 == 0 else "pool"
                            if eng == "pool":
                                S.op("act", lambda e: e.activation(out=hT[:, dc, tt * 128:(tt + 1) * 128], in_=pt[:, j * 128:(j + 1) * 128],
                                                                   func=AF.Identity, scale=A1c[:, dc:dc + 1], bias=sh1[:, dc:dc + 1]),
                                     reads=[bp, b_A1c, b_sh1], writes=[b_hT[tt]])
                            else:
                                S.op("dve", lambda e: e.tensor_scalar(out=hT[:, dc, tt * 128:(tt + 1) * 128], in0=pt[:, j * 128:(j + 1) * 128],
                                                                      scalar1=A1c[:, dc:dc + 1], scalar2=sh1[:, dc:dc + 1], op0=ALU.mult, op1=ALU.add),
                                     reads=[bp, b_A1c, b_sh1], writes=[b_hT[tt]])
                S.barrier()
                tmp.close()
                stg = Ring([(sb(f"stgA{i}", [128, 512]), Buf()) for i in range(4)])
                srcs = [wsrc(wA_d, c) for c in range(16)] + [wsrc(wB_d, c) for c in range(3)]
                ws = WStream(mkwring(sb), srcs)
                pi = 0
                for c in range(16):
                    wt, bw = ws.get(c)
                    for ft in range(2):
                        rowtile = c * 2 + ft
                        for tc in range(4):
                            pt, bp = P[pi % 4]; pi += 1
                            for kt in range(16):
                                S.op("pe", lambda e: e.matmul(out=pt[:], lhsT=wt[:, kt, ft * 128:(ft + 1) * 128], rhs=hT[:, kt, tc * 512:(tc + 1) * 512],
                                                              start=(kt == 0), stop=(kt == 15)),
                                     reads=[bw] + b_hT[tc * 4:(tc + 1) * 4], writes=[bp])
                            st_, bs = stg.next()
                            if pi % 2 == 0:
                                S.op("act", lambda e: e.copy(out=st_[:], in_=pt[:]), reads=[bp], writes=[bs])
                            else:
                                S.op("dve", lambda e: e.tensor_copy(out=st_[:], in_=pt[:]), reads=[bp], writes=[bs])
                            S.dma("pool", zT_d[rowtile * 128:(rowtile + 1) * 128, tc * 512:(tc + 1) * 512], st_[:], reads=[bs], writes=[b_zT[rowtile]])
                for cb_ in range(3):
                    wt, bw = ws.get(16 + cb_)
                    for tt in range(NT):
                        pt, bp = P[pi % 4]; pi += 1
                        for kt in range(16):
                            S.op("pe", lambda e: e.matmul(out=pt[:, 0:WCOL], lhsT=hT[:, kt, tt * 128:(tt + 1) * 128], rhs=wt[:, kt, :],
                                                          start=(kt == 0), stop=(kt == 15)),
                                 reads=[bw, b_hT[tt]], writes=[bp])
                        st_, bs = stg.next()
                        S.op("act", lambda e: e.copy(out=st_[:, 0:WCOL], in_=pt[:, 0:WCOL]), reads=[bp], writes=[bs])
                        S.dma("pool", ztm_d[tt * 128:(tt + 1) * 128, cb_ * WCOL:(cb_ + 1) * WCOL], st_[:, 0:WCOL], reads=[bs], writes=[b_ztm[tt]])
            if lvl == 1:
                final_bufs += b_zT + b_ztm

        if lvl >= 2:
            with phase() as sb:

                def ld(name, shp, src, dt=F32):
                    t = sb(name, shp, dt); b = Buf()
                    S.dma("sp", t[:], src, writes=[b])
                    return t, b
                cw, b_cw = ld("cw", [128, 8, 4], cw_d[:, :, :])
                cb, b_cb = ld("cb", [128, 8], cb_d[:, :])
                wrg, b_wrg = ld("wrg", [128, 8, 128], r32(wrg_d[:, :, :]), F32R)
                wig, b_wig = ld("wig", [128, 8, 128], r32(wig_d[:, :, :]), F32R)
                brg, b_brg = ld("brg", [128, 8], brg_d[:, :])
                big, b_big = ld("big", [128, 8], big_d[:, :])
                lam, b_lam = ld("lam", [128, 8], lam_d[:, :])
                gol, b_gol = ld("gol", [128, 8], gol_d[:, :])
                ones_r, b_ones = ld("ones_r", [128, 128], r32(ones_d[:, :]), F32R)
                clam = sb("clam", [128, 8]); b_clam = Buf()
                clam2 = sb("clam2", [128, 8]); b_clam2 = Buf()
                S.op("act", lambda e: e.activation(out=clam[:], in_=lam[:], func=AF.Sigmoid), reads=[b_lam], writes=[b_clam])
                S.op("act", lambda e: e.activation(out=clam[:], in_=clam[:], func=AF.Ln), reads=[b_clam], writes=[b_clam])
                S.op("dve", lambda e: e.tensor_scalar(out=clam2[:], in0=clam[:], scalar1=16.0, scalar2=None, op0=ALU.mult), reads=[b_clam], writes=[b_clam2])
                S.op("dve", lambda e: e.tensor_scalar(out=clam[:], in0=clam[:], scalar1=8.0, scalar2=None, op0=ALU.mult), reads=[b_clam, b_clam2], writes=[b_clam])
                olru = sb("olru", [128, 8, S_LEN]); b_olru = [Buf() for _ in range(8)]
                xrp_r = Ring([(sb(f"xrp{i}", [128, 3 + S_LEN]), Buf()) for i in range(2)])
                xg_r = Ring([(sb(f"xg{i}", [128, S_LEN]), Buf()) for i in range(2)])
                xc = sb("xc", [128, S_LEN], F32R); b_xc = Buf()
                xcacc = sb("xcacc", [128, S_LEN])
                rt = sb("rt", [128, S_LEN]); b_rt = Buf()
                it = sb("it", [128, S_LEN]); b_it = Buf()
                at = sb("at", [128, S_LEN]); b_at = Buf()
                ut = sb("ut", [128, S_LEN]); b_ut = Buf()
                ht = sb("ht", [128, S_LEN]); b_ht = Buf()
                gt = sb("gt", [128, S_LEN]); b_gt = Buf()
                osq = sb("osq", [128, S_LEN], F32R); b_osq = Buf()
                for ct in range(8):
                    xrp, bxr = xrp_r.next()
                    xg, bxg = xg_r.next()
                    S.op("pool", lambda e: e.memset(xrp[:, 0:3], 0.0), writes=[bxr])
                    S.dma("sp", xrp[:, 3:3 + S_LEN], zT_d[2048 + ct * 128:2048 + (ct + 1) * 128, :], reads=[b_zT[16 + ct]], writes=[bxr])
                    S.dma("sp", xg[:], zT_d[3072 + ct * 128:3072 + (ct + 1) * 128, :], reads=[b_zT[24 + ct]], writes=[bxg])
                    xcf = xcacc[:]
                    S.op("dve", lambda e: e.tensor_scalar(out=xcf, in0=xrp[:, 0:S_LEN], scalar1=cw[:, ct, 0:1], scalar2=cb[:, ct:ct + 1], op0=ALU.mult, op1=ALU.add),
                         reads=[bxr, b_cw, b_cb], writes=[b_xc])
                    for k in range(1, 4):
                        outap = xc[:] if k == 3 else xcf
                        S.op("dve", lambda e: e.scalar_tensor_tensor(out=outap, in0=xrp[:, k:k + S_LEN], scalar=cw[:, ct, k:k + 1], in1=xcf, op0=ALU.mult, op1=ALU.add),
                             reads=[bxr, b_cw, b_xc], writes=[b_xc])
                    xcf = f32(xc[:])
                    for (wg, bwg, bg, bbg, dst, bdst) in ((wrg, b_wrg, brg, b_brg, rt, b_rt), (wig, b_wig, big, b_big, it, b_it)):
                        for tc in range(4):
                            pt, bp = P[tc % 2]
                            S.op("pe", lambda e: e.matmul(out=pt[:], lhsT=wg[:, ct, :], rhs=xc[:, tc * 512:(tc + 1) * 512], start=True, stop=True),
                                 reads=[bwg, b_xc], writes=[bp])
                            S.op("act", lambda e: e.activation(out=dst[:, tc * 512:(tc + 1) * 512], in_=pt[:], func=AF.Sigmoid, bias=bg[:, ct:ct + 1]),
                                 reads=[bp, bbg], writes=[bdst])
                    S.op("act", lambda e: e.activation(out=at[:], in_=rt[:], func=AF.Exp, scale=clam[:, ct:ct + 1]), reads=[b_rt, b_clam], writes=[b_at])
                    S.op("act", lambda e: e.activation(out=ut[:], in_=rt[:], func=AF.Exp, scale=clam2[:, ct:ct + 1]), reads=[b_rt, b_clam2], writes=[b_ut])
                    S.op("act", lambda e: e.activation(out=ut[:], in_=ut[:], func=AF.Sqrt, scale=-1.0, bias=1.0), reads=[b_ut], writes=[b_ut])
                    S.op("dve", lambda e: e.tensor_tensor(out=ut[:], in0=ut[:], in1=it[:], op=ALU.mult), reads=[b_ut, b_it], writes=[b_ut])
                    S.op("dve", lambda e: e.tensor_tensor(out=ut[:], in0=ut[:], in1=xcf, op=ALU.mult), reads=[b_ut, b_xc], writes=[b_ut])
                    S.op("dve", lambda e: e.tensor_tensor_scan(out=ht[:], data0=at[:], data1=ut[:], initial=0.0, op0=ALU.mult, op1=ALU.add),
                         reads=[b_at, b_ut], writes=[b_ht])
                    S.op("act", lambda e: e.activation(out=gt[:], in_=xg[:], func=AF.Square), reads=[bxg], writes=[b_gt])
                    S.op("dve", lambda e: e.tensor_scalar(out=gt[:], in0=gt[:], scalar1=0.044715, scalar2=1.0, op0=ALU.mult, op1=ALU.add), reads=[b_gt], writes=[b_gt])
                    S.op("dve", lambda e: e.tensor_tensor(out=gt[:], in0=gt[:], in1=xg[:], op=ALU.mult), reads=[b_gt, bxg], writes=[b_gt])
                    S.op("act", lambda e: e.activation(out=gt[:], in_=gt[:], func=AF.Sigmoid, scale=1.5957691216057308), reads=[b_gt], writes=[b_gt])
                    S.op("dve", lambda e: e.tensor_tensor(out=gt[:], in0=gt[:], in1=xg[:], op=ALU.mult), reads=[b_gt, bxg], writes=[b_gt])
                    S.op("dve", lambda e: e.tensor_tensor(out=olru[:, ct, :], in0=gt[:], in1=ht[:], op=ALU.mult), reads=[b_gt, b_ht], writes=[b_olru[ct]])
                    S.op("act", lambda e: e.activation(out=osq[:], in_=olru[:, ct, :], func=AF.Square), reads=[b_olru[ct]], writes=[b_osq])
                    for tc in range(4):
                        pt, bp = P[4 + tc]
                        S.op("pe", lambda e: e.matmul(out=pt[:], lhsT=ones_r[:], rhs=osq[:, tc * 512:(tc + 1) * 512], start=(ct == 0), stop=(ct == 7)),
                             reads=[b_ones, b_osq], writes=[bp])
                rb = rt; b_rb = b_rt
                for tc in range(4):
                    pt, bp = P[4 + tc]
                    S.op("dve", lambda e: e.tensor_scalar(out=rb[:, tc * 512:(tc + 1) * 512], in0=pt[:], scalar1=1.0 / 1024, scalar2=EPS, op0=ALU.mult, op1=ALU.add),
                         reads=[bp], writes=[b_rb])
                S.op("act", lambda e: e.activation(out=rb[:], in_=rb[:], func=AF.Sqrt), reads=[b_rb], writes=[b_rb])
                S.op("dve", lambda e: e.reciprocal(out=rb[:], in_=rb[:]), reads=[b_rb], writes=[b_rb])
                ostg = Ring([(at, b_at), (ut, b_ut)])
                for ct in range(8):
                    st_, bs = ostg.next()
                    S.op("dve", lambda e: e.scalar_tensor_tensor(out=st_[:], in0=olru[:, ct, :], scalar=gol[:, ct:ct + 1], in1=rb[:], op0=ALU.mult, op1=ALU.mult),
                         reads=[b_olru[ct], b_gol, b_rb], writes=[bs])
                    S.dma("pool", onT_d[1024 + ct * 128:1024 + (ct + 1) * 128, :], st_[:], reads=[bs], writes=[b_onT[8 + ct]])
            if lvl == 2:
                final_bufs += b_onT[8:]

        if lvl >= 3:
            with phase() as sb:
                def ld(name, shp, src, dt=F32, reads=()):
                    t = sb(name, shp, dt); b = Buf()
                    S.dma("sp", t[:], src, reads=list(reads), writes=[b])
                    return t, b
                bones, b_bones = ld("bones", [128, 128], r32(bones_d[:, :]), F32R)
                cmask, b_cmask = ld("cmask", [128, 4, 512], r32(cmask_d[:, :, :]), F32R)
                wmask, b_wmask = ld("wmask", [128, 4, 512], r32(wmask_d[:, :, :]), F32R)
                ccm, b_ccm = ld("ccm", [128, 4, 512], r32(ccm_d[:, :, :]), F32R)
                esel, b_esel = ld("esel", [32, 16, 128], r32(esel_d[:, :, :]), F32R)
                selv, b_selv = ld("selv", [128, 16, 32], selv_d[:, :, :])
                self_, b_self = ld("self", [128, 16, 32], self_d[:, :, :])
                qg, b_qg = ld("qg", [128, 1], qg_d[:, :])
                kg, b_kg = ld("kg", [128, 3], kg_d[:, :])
                ones_r, b_ones = ld("ones_r", [128, 128], r32(ones_d[:, :]), F32R)
                qgs = sb("qgs", [128, 1]); b_qgs = Buf()
                S.op("dve", lambda e: e.tensor_scalar(out=qgs[:], in0=qg[:], scalar1=0.125, scalar2=None, op0=ALU.mult), reads=[b_qg], writes=[b_qgs])
                gsig, b_gsig = ld("gsig", [128, NT, 48], ztm_d[:, 512:560].rearrange("(t p) c -> p t c", p=128), reads=b_ztm)
                S.op("act", lambda e: e.activation(out=gsig[:], in_=gsig[:], func=AF.Sigmoid), reads=[b_gsig], writes=[b_gsig])

                qn = sb("qn", [128, 4, S_LEN], F32R); b_qn = [Buf() for _ in range(4)]
                kns = sb("kns", [128, S_LEN], F32R); b_kns = Buf()
                knw = sb("knw", [128, S_LEN], F32R); b_knw = Buf()
                kcn = sb("kcn", [128, 128], F32R); b_kcn = Buf()
                vcaug = sb("vcaug", [128, 2, 98], F32R); b_vcaug = Buf()
                vs = sb("vs", [128, NT, 2, 66], F32R); b_vs = Buf()
                vw = sb("vw", [128, NT, 2, 66], F32R); b_vw = Buf()

                for pair in range(2):
                    g0 = 2 * pair
                    with phase() as sp_:
                        wck, b_wck = sp_("wck", [128, 32, 128], F32R), Buf()
                        S.dma("sp", wck[:], r32(wck_d[:, :, :]), writes=[b_wck])
                        wcv, b_wcv = sp_("wcv", [128, 32, 64], F32R), Buf()
                        S.dma("sp", wcv[:], r32(wcv_d[:, :, :]), writes=[b_wcv])
                        pek2, b_pek2 = sp_("pek2", [128, 32, 2], F32R), Buf()
                        S.dma("sp", pek2[:], r32(pek2_d[:, :, :]), writes=[b_pek2])
                        pevT, b_pevT = sp_("pevT", [128, 32], F32R), Buf()
                        S.dma("sp", pevT[:], r32(pevT_d[:, :]), writes=[b_pevT])
                        raw = sp_("raw", [128, S_LEN]); b_raw = Buf()
                        sqt = sp_("sqt", [128, S_LEN + 16], F32R); b_sqt = Buf()
                        rr = sp_("rr", [128, 512]); b_rr = Buf()
                        kcraw = sp_("kcraw", [128, 128]); b_kcraw = Buf()
                        biasc = sp_("biasc", [128, 1]); b_biasc = Buf()
                        biasv = sp_("biasv", [1, 64], F32R); b_biasv = Buf()

                        def rms_feat(rows, gcol_ap, b_gcol, dst_fn, b_dst):
                            for hf, r0 in enumerate(rows):
                                S.dma("sp", raw[hf * 64:(hf + 1) * 64, :], zT_d[r0:r0 + 64, :], reads=[b_zT[r0 // 128]], writes=[b_raw])
                            S.op("act", lambda e: e.activation(out=sqt[:, 0:S_LEN], in_=raw[:], func=AF.Square), reads=[b_raw], writes=[b_sqt])
                            for tc in range(4):
                                pt, bp = P[6 + tc % 2]
                                S.op("pe", lambda e: e.matmul(out=pt[:], lhsT=bones[:], rhs=sqt[:, tc * 512:(tc + 1) * 512], start=True, stop=True),
                                     reads=[b_bones, b_sqt], writes=[bp])
                                S.op("dve", lambda e: e.tensor_scalar(out=rr[:], in0=pt[:], scalar1=1.0 / 64, scalar2=EPS, op0=ALU.mult, op1=ALU.add), reads=[bp], writes=[b_rr])
                                S.op("act", lambda e: e.activation(out=rr[:], in_=rr[:], func=AF.Sqrt), reads=[b_rr], writes=[b_rr])
                                S.op("dve", lambda e: e.reciprocal(out=rr[:], in_=rr[:]), reads=[b_rr], writes=[b_rr])
                                S.op("dve", lambda e: e.scalar_tensor_tensor(out=dst_fn(tc), in0=raw[:, tc * 512:(tc + 1) * 512], scalar=gcol_ap, in1=rr[:], op0=ALU.mult, op1=ALU.mult),
                                     reads=[b_raw, b_gcol, b_rr], writes=[b_dst])

                        for i in range(4):
                            ha, hb = 4 * g0 + i, 4 * (g0 + 1) + i
                            rms_feat([ha * 64, hb * 64], qgs[:, 0:1], b_qgs, lambda tc: qn[:, i, tc * 512:(tc + 1) * 512], b_qn[i])
                        rms_feat([1536 + g0 * 64, 1536 + g0 * 64 + 64], kg[:, 1:2], b_kg, lambda tc: kns[:, tc * 512:(tc + 1) * 512], b_kns)
                        rms_feat([1792 + g0 * 64, 1792 + g0 * 64 + 64], kg[:, 2:3], b_kg, lambda tc: knw[:, tc * 512:(tc + 1) * 512], b_knw)

                        S.dma("sp", sqt[:, 0:S_LEN], r32(zT_d[1024 + g0 * 64:1024 + g0 * 64 + 128, :]), reads=[b_zT[8 + pair]], writes=[b_sqt])
                        S.dma("sp", sqt[:, S_LEN:S_LEN + 16], r32(vaug0_d[:, 0, 0, 0:16]), writes=[b_sqt])
                        pk, bpk = P[6]
                        pb, bpb = P[7]
                        for j in range(32):
                            S.op("pe", lambda e: e.matmul(out=pk[:, 0:128], lhsT=wck[:, j, :], rhs=sqt[:, j:j + 16 * 127 + 1:16],
                                                          start=(j == 0), stop=(j == 31)),
                                 reads=[b_wck, b_sqt], writes=[bpk])
                        for j in range(32):
                            S.op("pe", lambda e: e.matmul(out=pb[:, 0:2], lhsT=wck[:, j, :], rhs=pek2[:, j, :],
                                                          start=(j == 0), stop=(j == 31)),
                                 reads=[b_wck, b_pek2], writes=[bpb])
                        S.op("dve", lambda e: e.tensor_copy(out=biasc[:], in_=pb[:, 0:1]), reads=[bpb], writes=[b_biasc])
                        S.op("dve", lambda e: e.tensor_scalar(out=kcraw[:], in0=pk[:, 0:128], scalar1=biasc[:, 0:1], scalar2=None, op0=ALU.add),
                             reads=[bpk, b_biasc], writes=[b_kcraw])
                        sq128 = sp_("sq128", [128, 128], F32R); b_sq128 = Buf()
                        S.op("act", lambda e: e.activation(out=sq128[:], in_=kcraw[:], func=AF.Square), reads=[b_kcraw], writes=[b_sq128])
                        S.op("pe", lambda e: e.matmul(out=pk[:, 128:256], lhsT=bones[:], rhs=sq128[:], start=True, stop=True), reads=[b_bones, b_sq128], writes=[bpk])
                        S.op("dve", lambda e: e.tensor_scalar(out=rr[:, 0:128], in0=pk[:, 128:256], scalar1=1.0 / 64, scalar2=EPS, op0=ALU.mult, op1=ALU.add), reads=[bpk], writes=[b_rr])
                        S.op("act", lambda e: e.activation(out=rr[:, 0:128], in_=rr[:, 0:128], func=AF.Sqrt), reads=[b_rr], writes=[b_rr])
                        S.op("dve", lambda e: e.reciprocal(out=rr[:, 0:128], in_=rr[:, 0:128]), reads=[b_rr], writes=[b_rr])
                        S.op("dve", lambda e: e.scalar_tensor_tensor(out=kcn[:], in0=kcraw[:], scalar=kg[:, 0:1], in1=rr[:, 0:128], op0=ALU.mult, op1=ALU.mult),
                             reads=[b_kcraw, b_kg, b_rr], writes=[b_kcn])

                        S.dma("sp", sqt[:, 0:S_LEN], r32(zT_d[1280 + g0 * 64:1280 + g0 * 64 + 128, :]), reads=[b_zT[10 + pair]], writes=[b_sqt])
                        pbv, bpbv = P[7]
                        for j in range(32):
                            S.op("pe", lambda e: e.matmul(out=pbv[0:1, 0:64], lhsT=pevT[0:64, j:j + 1], rhs=wcv[0:64, j, :], start=(j == 0), stop=(j == 31)),
                                 reads=[b_pevT, b_wcv], writes=[bpbv])
                        S.op("dve", lambda e: e.tensor_copy(out=biasv[:], in_=pbv[0:1, 0:64]), reads=[bpbv], writes=[b_biasv])
                        for gi in range(2):
                            S.dma("sp", vcaug[:, gi, :], r32(vcaug0_d[:, :]), writes=[b_vcaug])
                        for hf in range(2):
                            hs = slice(hf * 64, (hf + 1) * 64)
                            pv, bpv = P[4 + hf]
                            for j in range(32):
                                S.op("pe", lambda e: e.matmul(out=pv[:, 0:64], lhsT=sqt[hs, j:j + 16 * 127 + 1:16], rhs=wcv[hs, j, :], start=(j == 0), stop=False),
                                     reads=[b_sqt, b_wcv], writes=[bpv])
                            S.op("pe", lambda e: e.matmul(out=pv[:, 0:64], lhsT=ones_r[0:1, :], rhs=biasv[0:1, :], start=False, stop=True),
                                 reads=[b_ones, b_biasv], writes=[bpv])
                            S.op("act", lambda e: e.copy(out=vcaug[:, hf, 0:64], in_=pv[:, 0:64]), reads=[bpv], writes=[b_vcaug])
                        for (vt, bvt, c0) in ((vs, b_vs, 0), (vw, b_vw, 256)):
                            S.dma("sp", vt[:], r32(vaug0_d[:, :, :, :]), writes=[bvt])
                            for gi in range(2):
                                S.dma("sp", vt[:, :, gi, 0:64],
                                      r32(ztm_d[:, c0 + (g0 + gi) * 64:c0 + (g0 + gi) * 64 + 64]).rearrange("(t p) d -> p t d", p=128),
                                      reads=b_ztm, writes=[bvt])

                    with phase() as sa:
                        oacc = sa("oacc", [128, NT, 512]); b_oacc = [Buf() for _ in range(NT)]
                        impacc = sa("impacc", [128, NT, 2, 32]); b_imp = [Buf() for _ in range(NT)]
                        negselT = sa("negselT", [32, 2, S_LEN], F32R); b_nst = [Buf() for _ in range(2)]
                        ptr_ = Ring([(sa(f"PT{i}", [128, 512], F32R), Buf()) for i in range(3)])
                        small = sa("small", [128, 8]); b_small = Buf()
                        coef = sa("coef", [128, 4]); b_coef = Buf()
                        top8 = sa("top8", [128, 8]); b_top8 = Buf()
                        impp = sa("impp", [128, 32]); b_impp = Buf()
                        negs = sa("negs", [128, 32]); b_negs = Buf()

                        def heads():
                            for i in range(4):
                                for hf in range(2):
                                    yield i, hf, 4 * (g0 + hf) + i, hf * 4 + i, slice(hf * 64, (hf + 1) * 64)

                        nb = 0
                        for (i, hf, h, hl, hs) in heads():
                            for qc in range(4):
                                pt, bp = P[nb % 2]
                                po, bpo = P[2 + nb % 2]
                                nb += 1
                                S.op("pe", lambda e: e.matmul(out=pt[:], lhsT=kcn[hs, :], rhs=qn[hs, i, qc * 512:(qc + 1) * 512], start=True, stop=False),
                                     reads=[b_kcn, b_qn[i]], writes=[bp])
                                S.op("pe", lambda e: e.matmul(out=pt[:], lhsT=identr[:], rhs=ccm[:, qc, :], start=False, stop=True),
                                     reads=[b_identr, b_ccm], writes=[bp])
                                PT, bPT = ptr_.next()
                                S.op("act", lambda e: e.activation(out=PT[:], in_=pt[:], func=AF.Exp), reads=[bp], writes=[bPT])
                                for j in range(4):
                                    S.op("pe", lambda e: e.matmul(out=po[:, j * 128:j * 128 + 98], lhsT=PT[:, j * 128:(j + 1) * 128], rhs=vcaug[:, hf, :], start=True, stop=True),
                                         reads=[bPT, b_vcaug], writes=[bpo])
                                pov = po[:].rearrange("p (j c) -> p j c", j=4)
                                S.op("dve", lambda e: e.tensor_scalar(out=small[:, 0:4], in0=pov[:, :, 64], scalar1=1e-30, scalar2=None, op0=ALU.add), reads=[bpo], writes=[b_small])
                                S.op("dve", lambda e: e.reciprocal(out=small[:, 0:4], in_=small[:, 0:4]), reads=[b_small], writes=[b_small])
                                S.op("dve", lambda e: e.tensor_tensor(out=coef[:], in0=small[:, 0:4], in1=gsig[:, qc * 4:(qc + 1) * 4, h * 3], op=ALU.mult),
                                     reads=[b_small, b_gsig], writes=[b_coef])
                                for j in range(4):
                                    tt = qc * 4 + j
                                    S.op("dve", lambda e: e.tensor_scalar(out=oacc[:, tt, hl * 64:(hl + 1) * 64], in0=po[:, j * 128:j * 128 + 64], scalar1=coef[:, j:j + 1], scalar2=None, op0=ALU.mult),
                                         reads=[bpo, b_coef], writes=[b_oacc[tt]])
                                    if i == 0:
                                        S.op("dve", lambda e: e.tensor_scalar(out=impacc[:, tt, hf, :], in0=po[:, j * 128 + 66:j * 128 + 98], scalar1=small[:, j:j + 1], scalar2=None, op0=ALU.mult),
                                             reads=[bpo, b_small], writes=[b_imp[tt]])
                                    else:
                                        S.op("dve", lambda e: e.scalar_tensor_tensor(out=impacc[:, tt, hf, :], in0=po[:, j * 128 + 66:j * 128 + 98], scalar=small[:, j:j + 1], in1=impacc[:, tt, hf, :], op0=ALU.mult, op1=ALU.add),
                                             reads=[bpo, b_small, b_imp[tt]], writes=[b_imp[tt]])
                        for hf in range(2):
                            for tt in range(NT):
                                S.op("dve", lambda e: e.tensor_tensor(out=impp[:], in0=impacc[:, tt, hf, :], in1=selv[:, tt, :], op=ALU.mult), reads=[b_imp[tt], b_selv], writes=[b_impp])
                                S.op("dve", lambda e: e.tensor_tensor(out=impp[:], in0=impp[:], in1=self_[:, tt, :], op=ALU.add), reads=[b_impp, b_self], writes=[b_impp])
                                S.op("dve", lambda e: e.max(out=top8[:], in_=impp[:]), reads=[b_impp], writes=[b_top8])
                                S.op("dve", lambda e: e.tensor_scalar(out=negs[:], in0=impp[:], scalar1=top8[:, 7:8], scalar2=NEG, op0=ALU.is_lt, op1=ALU.mult),
                                     reads=[b_impp, b_top8], writes=[b_negs])
                                pt, bp = P[6 + (tt // 4) % 2]
                                S.op("pe", lambda e: e.transpose(out=pt[0:32, (tt % 4) * 128:(tt % 4 + 1) * 128], in_=negs[:], identity=ident[:]),
                                     reads=[b_negs, b_ident], writes=[bp])
                                if tt % 4 == 3:
                                    S.op("act", lambda e: e.copy(out=negselT[:, hf, (tt // 4) * 512:(tt // 4 + 1) * 512], in_=pt[0:32, :]), reads=[bp], writes=[b_nst[hf]])
                        units = []
                        gi_ = 0
                        for (i, hf, h, hl, hs) in heads():
                            for qc in range(4):
                                for br in (1, 2):
                                    kts = list(range(0, 4 * qc + 4)) if br == 1 else list(range(max(0, 4 * qc - 4), 4 * qc + 4))
                                    for n, kt in enumerate(kts):
                                        units.append((i, hf, h, hl, hs, qc, br, n, kt, len(kts), gi_))
                                    gi_ += 1
                        sbank = {}
                        oTr = Ring([(sa(f"oT{i}", [66, 512]), Buf()) for i in range(2)])

                        def emit_S(ui):
                            (i, hf, h, hl, hs, qc, br, n, kt, nk, gix) = units[ui]
                            kn, b_kn = (kns, b_kns) if br == 1 else (knw, b_knw)
                            pt, bp = P[ui % 2]
                            sbank[ui] = (pt, bp)
                            S.op("pe", lambda e: e.matmul(out=pt[:], lhsT=kn[hs, kt * 128:(kt + 1) * 128], rhs=qn[hs, i, qc * 512:(qc + 1) * 512], start=True, stop=False),
                                 reads=[b_kn, b_qn[i]], writes=[bp])
                            if br == 1:
                                diag = kt >= 4 * qc
                                S.op("pe", lambda e: e.matmul(out=pt[:], lhsT=esel[:, kt, :], rhs=negselT[:, hf, qc * 512:(qc + 1) * 512], start=False, stop=not diag),
                                     reads=[b_esel, b_nst[hf]], writes=[bp])
                                if diag:
                                    S.op("pe", lambda e: e.matmul(out=pt[:], lhsT=identr[:], rhs=cmask[:, kt - 4 * qc, :], start=False, stop=True),
                                         reads=[b_identr, b_cmask], writes=[bp])
                            else:
                                r = kt - (4 * qc - 4)
                                mk_, bmk = (wmask[:, r, :], b_wmask) if r < 4 else (cmask[:, r - 4, :], b_cmask)
                                S.op("pe", lambda e: e.matmul(out=pt[:], lhsT=identr[:], rhs=mk_, start=False, stop=True),
                                     reads=[b_identr, bmk], writes=[bp])

                        def emit_rest(ui):
                            (i, hf, h, hl, hs, qc, br, n, kt, nk, gix) = units[ui]
                            vt, bvt = (vs, b_vs) if br == 1 else (vw, b_vw)
                            pt, bp = sbank.pop(ui)
                            PT, bPT = ptr_.next()
                            S.op("act", lambda e: e.activation(out=PT[:], in_=pt[:], func=AF.Exp), reads=[bp], writes=[bPT])
                            pacc, bpacc = P[2 + gix % 2]
                            S.op("pe", lambda e: e.matmul(out=pacc[0:66, :], lhsT=vt[:, kt, hf, :], rhs=PT[:], start=(n == 0), stop=(n == nk - 1)),
                                 reads=[bPT, bvt], writes=[bpacc])
                            if n == nk - 1:
                                oT, boT = oTr.next()
                                S.op("dve", lambda e: e.tensor_copy(out=oT[:], in_=pacc[0:66, :]), reads=[bpacc], writes=[boT])
                                ptp, bptp = P[4 + gix % 2]
                                for j in range(4):
                                    S.op("pe", lambda e: e.transpose(out=ptp[:, j * 128:j * 128 + 66], in_=oT[:, j * 128:(j + 1) * 128], identity=ident[0:66, 0:66]),
                                         reads=[boT, b_ident], writes=[bptp])
                                for j in range(4):
                                    tt = qc * 4 + j
                                    pa, bpa = ptp[:, j * 128:(j + 1) * 128], bptp
                                    S.op("dve", lambda e: e.reciprocal(out=small[:, 4 + j:5 + j], in_=pa[:, 64:65]), reads=[bpa], writes=[b_small])
                                    S.op("dve", lambda e: e.tensor_tensor(out=small[:, 4 + j:5 + j], in0=small[:, 4 + j:5 + j], in1=gsig[:, tt, h * 3 + br:h * 3 + br + 1], op=ALU.mult),
                                         reads=[b_small, b_gsig], writes=[b_small])
                                    S.op("dve", lambda e: e.scalar_tensor_tensor(out=oacc[:, tt, hl * 64:(hl + 1) * 64], in0=pa[:, 0:64], scalar=small[:, 4 + j:5 + j], in1=oacc[:, tt, hl * 64:(hl + 1) * 64], op0=ALU.mult, op1=ALU.add),
                                         reads=[bpa, b_small, b_oacc[tt]], writes=[b_oacc[tt]])

                        emit_S(0)
                        for ui in range(len(units)):
                            if ui + 1 < len(units):
                                emit_S(ui + 1)
                            emit_rest(ui)
                        for tt in range(NT):
                            S.dma("pool", onsa_d[tt * 128:(tt + 1) * 128, pair * 512:(pair + 1) * 512], oacc[:, tt, :], reads=[b_oacc[tt]], writes=[b_onsa[tt][pair]])
            if lvl == 3:
                final_bufs += [b for bb in b_onsa for b in bb]

        if lvl >= 4:
            with phase() as sb:
                gonb = sb("gonb", [128, 1024]); b_gonb = Buf()
                S.dma("sp", gonb[:], gon_d[0:1, :].partition_broadcast(128), writes=[b_gonb])
                oring = Ring([(sb(f"ot{i}", [128, 1024]), Buf()) for i in range(2)])
                sqj = sb("sqjF", [128, 1024]); b_sqj = Buf()
                ss = sb("ssF", [128, 1]); b_ss = Buf()
                stgF = Ring([(sb(f"stgF{i}", [128, 8, 512]), Buf()) for i in range(2)])
                st_, bs = None, None
                for tt in range(NT):
                    ot, bo = oring.next()
                    S.dma("sp", ot[:], onsa_d[tt * 128:(tt + 1) * 128, :], reads=b_onsa[tt], writes=[bo])
                    S.op("act", lambda e: e.activation(out=sqj[:], in_=ot[:], func=AF.Square, accum_out=ss[:]), reads=[bo], writes=[b_sqj, b_ss])
                    S.op("dve", lambda e: e.tensor_scalar(out=ss[:], in0=ss[:], scalar1=1.0 / 1024, scalar2=EPS, op0=ALU.mult, op1=ALU.add), reads=[b_ss], writes=[b_ss])
                    S.op("act", lambda e: e.activation(out=ss[:], in_=ss[:], func=AF.Sqrt), reads=[b_ss], writes=[b_ss])
                    S.op("dve", lambda e: e.reciprocal(out=ss[:], in_=ss[:]), reads=[b_ss], writes=[b_ss])
                    S.op("dve", lambda e: e.scalar_tensor_tensor(out=ot[:], in0=ot[:], scalar=ss[:, 0:1], in1=gonb[:], op0=ALU.mult, op1=ALU.mult),
                         reads=[bo, b_ss, b_gonb], writes=[bo])
                    if tt % 4 == 0:
                        st_, bs = stgF.next()
                    for half in range(2):
                        pt, bp = P[(2 * tt + half) % 4]
                        for j in range(4):
                            ft = half * 4 + j
                            S.op("pe", lambda e: e.transpose(out=pt[:, j * 128:(j + 1) * 128], in_=ot[:, ft * 128:(ft + 1) * 128], identity=ident[:]),
                                 reads=[bo, b_ident], writes=[bp])
                        S.op("act", lambda e: e.copy(out=st_[:, half * 4:(half + 1) * 4, (tt % 4) * 128:(tt % 4 + 1) * 128], in_=pt[:].rearrange("p (a b) -> p a b", a=4)),
                             reads=[bp], writes=[bs])
                    if tt % 4 == 3:
                        tq = tt // 4
                        S.dma("pool", onT_d[0:1024, tq * 512:(tq + 1) * 512].rearrange("(f p) t -> p f t", p=128), st_[:], reads=[bs], writes=b_onT[0:8])
            if lvl == 4:
                final_bufs += b_onT

        if lvl >= 5:
            with phase() as sb:
                onT = sb("onT", [128, 16, S_LEN], F32R); b_onTs = Buf()
                for kt in range(16):
                    S.dma("sp", onT[:, kt, :], r32(onT_d[kt * 128:(kt + 1) * 128, :]), reads=[b_onT[kt]], writes=[b_onTs])
                ws = WStream(mkwring(sb), [wsrc(wout_d, c) for c in range(D // WCOL)])
                g1ring = Ring([(sb(f"g1b{i}", [128, WCOL]), Buf()) for i in range(2)])
                xcr = Ring([(sb(f"xc{i}", [128, WCOL]), Buf()) for i in range(3)])
                ocr = Ring([(sb(f"oc{i}", [128, WCOL]), Buf()) for i in range(3)])
                pi = 0
                for c in range(D // WCOL):
                    wt, bw = ws.get(c)
                    g1b, bg1 = g1ring.next()
                    S.dma("sp", g1b[:], mod_d[0:1, 2 * D + c * WCOL:2 * D + (c + 1) * WCOL].partition_broadcast(128), reads=[b_mod], writes=[bg1])
                    for tt in range(NT):
                        pt, bp = P[pi % 4]; pi += 1
                        for kt in range(16):
                            S.op("pe", lambda e: e.matmul(out=pt[:, 0:WCOL], lhsT=onT[:, kt, tt * 128:(tt + 1) * 128], rhs=wt[:, kt, :], start=(kt == 0), stop=(kt == 15)),
                                 reads=[b_onTs, bw], writes=[bp])
                        xt, bx = xcr.next()
                        S.dma("sp", xt[:], x_d[tt * 128:(tt + 1) * 128, c * WCOL:(c + 1) * WCOL], writes=[bx])
                        ot, bo = ocr.next()
                        S.op("dve", lambda e: e.tensor_tensor(out=ot[:], in0=pt[:, 0:WCOL], in1=g1b[:], op=ALU.mult), reads=[bp, bg1], writes=[bo])
                        S.op("dve", lambda e: e.tensor_tensor(out=ot[:], in0=ot[:], in1=xt[:], op=ALU.add), reads=[bo, bx], writes=[bo])
                        S.dma("pool", x1_d[tt * 128:(tt + 1) * 128, c * WCOL:(c + 1) * WCOL], ot[:], reads=[bo], writes=[b_x1[tt]])
            if lvl == 5:
                final_bufs += b_x1

        if lvl >= 6:
            with phase() as sbo:
                tokid = sbo("tokid", [128, NT, 2], I32); b_tokid = Buf()
                S.dma("sp", tokid[:], tokid_d[:, :, :], writes=[b_tokid])
                sidx = sbo("sidx", [128, NT, 4], I32); b_sidx = Buf()
                gk = sbo("gk", [128, NT, 4]); b_gk = Buf()
                with phase() as sb:
                    def ld(name, shp, src, dt=F32, reads=()):
                        t = sb(name, shp, dt); b = Buf()
                        S.dma("sp", t[:], src, reads=list(reads), writes=[b])
                        return t, b
                    maskall = sb("maskall", [128, NT, NE], F32R); b_mask = [Buf() for _ in range(NT)]
                    posall = sb("posall", [128, NT, NE]); b_pos = [Buf() for _ in range(NT)]
                    oobt = sb("oobt", [128, 2 * NE * CAP // 128], I32); b_oobt = Buf()
                    S.dma("sp", oobt[:], oobidx_d[:, :], writes=[b_oobt])
                    S.dma("sp", tokidx_d.rearrange("(p j) o -> p (j o)", p=128), oobt[:], reads=[b_oobt], writes=[b_tokidx])
                    A2b, b_A2b = ld("A2b", [128, D], mod_d[0:1, 4 * D:5 * D].partition_broadcast(128), reads=[b_mod])
                    g2b, b_g2b = ld("g2b", [128, D], g2_d[0:1, :].partition_broadcast(128))
                    B2b, b_B2b = ld("B2b", [128, D], mod_d[0:1, 3 * D:4 * D].partition_broadcast(128), reads=[b_mod])
                    S.op("dve", lambda e: e.scalar_tensor_tensor(out=A2b[:], in0=A2b[:], scalar=1.0, in1=g2b[:], op0=ALU.add, op1=ALU.mult),
                         reads=[b_A2b, b_g2b], writes=[b_A2b])
                    wr, b_wr = ld("wr", [128, 16, NE], wr_d[:, :, :])
                    brb, b_brb = ld("brb", [128, NE], br_d[0:1, :].partition_broadcast(128))
                    triu, b_triu = ld("triu", [128, 128], r32(triu_d[:, :]), F32R)
                    ones_r, b_ones = ld("ones_r", [128, 128], r32(ones_d[:, :]), F32R)
                    ebase, b_ebase = ld("ebase", [128, NE], ebase_d[:, :])
                    x1r = Ring([(sb(f"x1t{i}", [128, D]), Buf()) for i in range(2)])
                    sqj = sb("sqjH", [128, D]); b_sqj = Buf()
                    ss = sb("ssH", [128, 1]); b_ss = Buf()
                    h2f = sb("h2f", [128, D]); b_h2f = Buf()
                    h2T = sb("h2T", [128, 16, 128]); b_h2T = Buf()
                    lg = sb("lg", [128, NE]); b_lg = Buf()
                    t8 = sb("t8", [128, 8]); b_t8 = Buf()
                    ex = sb("ex", [128, NE]); b_ex = Buf()
                    nm = sb("nm", [128, 1]); b_nm = Buf()
                    gd = sb("gd", [128, 1]); b_gd = Buf()
                    e4 = sb("e4", [128, 4]); b_e4 = Buf()
                    slot = sb("slot", [128, NE]); b_slot = Buf()
                    oh = sb("oh", [128, NE]); b_oh = Buf()
                    sf = sb("sf", [128, 4]); b_sf = Buf()
                    rinfo = sb("rinfo", [128, NT, 8]); b_ri = Buf()
                    for tt in range(NT):
                        xt, bx = x1r.next()
                        S.dma("sp", xt[:], x1_d[tt * 128:(tt + 1) * 128, :], reads=[b_x1[tt]], writes=[bx])
                        S.op("act", lambda e: e.activation(out=sqj[:], in_=xt[:], func=AF.Square, accum_out=ss[:]), reads=[bx], writes=[b_sqj, b_ss])
                        S.op("dve", lambda e: e.tensor_scalar(out=ss[:], in0=ss[:], scalar1=1.0 / D, scalar2=EPS, op0=ALU.mult, op1=ALU.add), reads=[b_ss], writes=[b_ss])
                        S.op("act", lambda e: e.activation(out=ss[:], in_=ss[:], func=AF.Sqrt), reads=[b_ss], writes=[b_ss])
                        S.op("dve", lambda e: e.reciprocal(out=ss[:], in_=ss[:]), reads=[b_ss], writes=[b_ss])
                        S.op("dve", lambda e: e.scalar_tensor_tensor(out=h2f[:], in0=xt[:], scalar=ss[:, 0:1], in1=A2b[:], op0=ALU.mult, op1=ALU.mult),
                             reads=[bx, b_ss, b_A2b], writes=[b_h2f])
                        S.op("dve", lambda e: e.tensor_tensor(out=h2f[:], in0=h2f[:], in1=B2b[:], op=ALU.add), reads=[b_h2f, b_B2b], writes=[b_h2f])
                        S.dma("pool", h2_d[tt * 128:(tt + 1) * 128, :], h2f[:], reads=[b_h2f], writes=[b_h2d[tt]])
                        for g in range(4):
                            pt, bp = P[g]
                            for j in range(4):
                                dc = g * 4 + j
                                S.op("pe", lambda e: e.transpose(out=pt[:, j * 128:(j + 1) * 128], in_=h2f[:, dc * 128:(dc + 1) * 128], identity=ident[:]),
                                     reads=[b_h2f, b_ident], writes=[bp])
                            if g % 2 == 0:
                                S.op("act", lambda e: e.copy(out=h2T[:, g * 4:(g + 1) * 4, :], in_=pt[:].rearrange("p (a b) -> p a b", a=4)), reads=[bp], writes=[b_h2T])
                            else:
                                S.op("dve", lambda e: e.tensor_copy(out=h2T[:, g * 4:(g + 1) * 4, :], in_=pt[:].rearrange("p (a b) -> p a b", a=4)), reads=[bp], writes=[b_h2T])
                        pl, bpl = P[4]
                        for dc in range(16):
                            S.op("pe", lambda e: e.matmul(out=pl[:, 0:NE], lhsT=h2T[:, dc, :], rhs=wr[:, dc, :], start=(dc == 0), stop=(dc == 15)),
                                 reads=[b_h2T, b_wr], writes=[bpl])
                        S.op("dve", lambda e: e.tensor_tensor(out=lg[:], in0=pl[:, 0:NE], in1=brb[:], op=ALU.add), reads=[bpl, b_brb], writes=[b_lg])
                        S.op("dve", lambda e: e.max(out=t8[:], in_=lg[:]), reads=[b_lg], writes=[b_t8])
                        S.op("dve", lambda e: e.tensor_scalar(out=maskall[:, tt, :], in0=lg[:], scalar1=t8[:, 3:4], scalar2=None, op0=ALU.is_ge),
                             reads=[b_lg, b_t8], writes=[b_mask[tt]])
                        S.op("dve", lambda e: e.tensor_scalar(out=nm[:], in0=t8[:, 0:1], scalar1=-1.0, scalar2=None, op0=ALU.mult), reads=[b_t8], writes=[b_nm])
                        S.op("act", lambda e: e.activation(out=e4[:], in_=t8[:, 0:4], func=AF.Exp, bias=nm[:, 0:1], accum_out=gd[:]), reads=[b_t8, b_nm], writes=[b_e4, b_gd])
                        S.op("dve", lambda e: e.reciprocal(out=gd[:], in_=gd[:]), reads=[b_gd], writes=[b_gd])
                        S.op("dve", lambda e: e.tensor_scalar(out=gk[:, tt, :], in0=e4[:], scalar1=gd[:, 0:1], scalar2=None, op0=ALU.mult), reads=[b_e4, b_gd], writes=[b_gk])
                        pp, bpp = P[5 + tt % 2]
                        for t2 in range(tt):
                            S.op("pe", lambda e: e.matmul(out=pp[:, 0:NE], lhsT=ones_r[:], rhs=maskall[:, t2, :], start=(t2 == 0), stop=False),
                                 reads=[b_ones, b_mask[t2]], writes=[bpp])
                        S.op("pe", lambda e: e.matmul(out=pp[:, 0:NE], lhsT=triu[:], rhs=maskall[:, tt, :], start=(tt == 0), stop=True),
                             reads=[b_triu, b_mask[tt]], writes=[bpp])
                        S.op("act", lambda e: e.copy(out=posall[:, tt, :], in_=pp[:, 0:NE]), reads=[bpp], writes=[b_pos[tt]])
                        S.op("dve", lambda e: e.tensor_tensor(out=slot[:], in0=posall[:, tt, :], in1=ebase[:], op=ALU.add), reads=[b_pos[tt], b_ebase], writes=[b_slot])
                        for k in range(4):
                            S.op("dve", lambda e: e.tensor_scalar(out=oh[:], in0=lg[:], scalar1=t8[:, k:k + 1], scalar2=None, op0=ALU.is_equal), reads=[b_lg, b_t8], writes=[b_oh])
                            S.op("dve", lambda e: e.tensor_tensor(out=oh[:], in0=oh[:], in1=slot[:], op=ALU.mult), reads=[b_oh, b_slot], writes=[b_oh])
                            S.op("dve", lambda e: e.reduce_sum(out=sf[:, k:k + 1], in_=oh[:], axis=mybir.AxisListType.X), reads=[b_oh], writes=[b_sf])
                        S.op("dve", lambda e: e.tensor_copy(out=sidx[:, tt, :], in_=sf[:]), reads=[b_sf], writes=[b_sidx])
                        for k in range(4):
                            S.dma("pool", None, None, reads=[b_sidx, b_tokid], writes=[b_tokidx],
                                  fn=lambda e: e.indirect_dma_start(out=tokidx_d[:, :], out_offset=bass.IndirectOffsetOnAxis(ap=sidx[:, tt, k:k + 1], axis=0),
                                                                    in_=tokid[:, tt, :], in_offset=None,
                                                                    bounds_check=reg_slot, oob_is_err=False))
                        if dbg:
                            S.op("dve", lambda e: e.tensor_copy(out=rinfo[:, tt, 0:4], in_=sf[:]), reads=[b_sf], writes=[b_ri])
                            S.op("dve", lambda e: e.tensor_copy(out=rinfo[:, tt, 4:8], in_=gk[:, tt, :]), reads=[b_gk], writes=[b_ri])
                    if dbg:
                        S.dma("sp", rinfo_d.rearrange("(t p) c -> p t c", p=128), rinfo[:], reads=[b_ri], writes=[b_rinfo])
                if lvl == 6:
                    final_bufs += [b_rinfo]

                if lvl >= 7:
                    with phase() as sb:
                        b1r = Ring([(sb(f"b1c{i}", [128, 32]), Buf()) for i in range(2)])
                        ones_r = sb("ones_rE", [1, 128], F32R); b_ones = Buf()
                        S.dma("sp", ones_r[:], r32(ones_d[0:1, :]), writes=[b_ones])
                        xbT = sb("xbT", [128, 16, CAP], F32R); b_xbT = [Buf() for _ in range(NST)]
                        tact = sb("tact", [128, 16, CAP], F32R); b_tact = [Buf() for _ in range(16)]
                        ug = sb("ug", [128, 512]); b_ug = Buf()
                        sg = sb("sg", [128, 512]); b_sg = Buf()
                        idxr = Ring([(sb(f"idxe{i}", [128, NST], I32), Buf()) for i in range(2)])
                        xbr = Ring([(sb(f"xb{i}", [128, D]), Buf()) for i in range(2)])
                        for xt_, bx_ in xbr.items:
                            S.op("pool", lambda e: e.memset(xt_[:], 0.0), writes=[bx_])
                        g2ring = Ring([(sb(f"g2b{i}", [128, WCOL]), Buf()) for i in range(2)])
                        b2ring = Ring([(sb(f"b2r{i}", [1, WCOL], F32R), Buf()) for i in range(4)])
                        yring = Ring([(sb(f"yst{i}", [128, WCOL]), Buf()) for i in range(3)])
                        srcs = []
                        for e_ in range(NE):
                            srcs += [wsrc(we1_d[e_], c) for c in range(2 * D // WCOL)]
                            srcs += [wsrc(we2_d[e_], c) for c in range(D // WCOL)]
                        ws = WStream(mkwring(sb, 3), srcs, ahead=2)
                        wi = 0
                        pi = 0

                        def load_idx(e_):
                            idxe, bidx = idxr.next()
                            S.dma("sp", None, None, reads=[b_tokidx], writes=[bidx],
                                  fn=lambda e: e.dma_start(out=idxe[:], in_=tokidx_d[e_ * CAP:(e_ + 1) * CAP, 0:1].rearrange("(s p) o -> p (s o)", p=128),
                                                           allow_slow_non_contiguous=True))
                            return idxe, bidx

                        def gather(idxe, bidx, st):
                            xb, bxb = xbr.next()
                            S.dma("pool", None, None, reads=[bidx] + b_h2d, writes=[bxb],
                                  fn=lambda e: e.indirect_dma_start(out=xb[:, :], out_offset=None, in_=h2_d[:, :],
                                                                    in_offset=bass.IndirectOffsetOnAxis(ap=idxe[:, st:st + 1], axis=0),
                                                                    bounds_check=reg_tok, oob_is_err=False))
                            return xb, bxb

                        def transp(xb, bxb, st):
                            for g in range(4):
                                pt, bp = P[4 + g]
                                for j in range(4):
                                    dc = g * 4 + j
                                    S.op("pe", lambda e: e.transpose(out=pt[:, j * 128:(j + 1) * 128], in_=xb[:, dc * 128:(dc + 1) * 128], identity=ident[:]),
                                         reads=[bxb, b_ident], writes=[bp])
                                if g % 2 == 0:
                                    S.op("act", lambda e: e.copy(out=xbT[:, g * 4:(g + 1) * 4, st * 128:(st + 1) * 128], in_=pt[:].rearrange("p (a b) -> p a b", a=4)),
                                         reads=[bp], writes=[b_xbT[st]])
                                else:
                                    S.op("dve", lambda e: e.tensor_copy(out=xbT[:, g * 4:(g + 1) * 4, st * 128:(st + 1) * 128], in_=pt[:].rearrange("p (a b) -> p a b", a=4)),
                                         reads=[bp], writes=[b_xbT[st]])

                        idx0, bidx0 = load_idx(0)
                        for st in range(NST):
                            xb, bxb = gather(idx0, bidx0, st)
                            transp(xb, bxb, st)
                        for e_ in range(NE):
                            b1c, b_b1c = b1r.next()
                            S.dma("sp", b1c[:], b1c_d[:, e_, :], writes=[b_b1c])
                            for c in range(2 * D // WCOL):
                                wt, bw = ws.get(wi); wi += 1
                                for ftl in range(2):
                                    F = c * 2 + ftl
                                    for hv in range(2):
                                        cs = slice(hv * 512, (hv + 1) * 512)
                                        pt, bp = P[pi % 4]; pi += 1
                                        for kt in range(16):
                                            S.op("pe", lambda e: e.matmul(out=pt[:], lhsT=wt[:, kt, ftl * 128:(ftl + 1) * 128], rhs=xbT[:, kt, cs], start=(kt == 0), stop=(kt == 15)),
                                                 reads=[bw] + b_xbT[hv * 4:(hv + 1) * 4], writes=[bp])
                                        S.op("dve", lambda e: e.tensor_scalar(out=ug[:], in0=pt[:], scalar1=b1c[:, F:F + 1], scalar2=7.0, op0=ALU.add, op1=ALU.min),
                                             reads=[bp, b_b1c], writes=[b_ug])
                                        if F < 16:
                                            S.op("act", lambda e: e.activation(out=sg[:], in_=ug[:], func=AF.Sigmoid, scale=1.702), reads=[b_ug], writes=[b_sg])
                                            S.op("dve", lambda e: e.tensor_tensor(out=tact[:, F, cs], in0=ug[:], in1=sg[:], op=ALU.mult), reads=[b_ug, b_sg], writes=[b_tact[F]])
                                        else:
                                            Fl = F - 16
                                            S.op("dve", lambda e: e.tensor_scalar(out=ug[:], in0=ug[:], scalar1=-7.0, scalar2=1.0, op0=ALU.max, op1=ALU.add), reads=[b_ug], writes=[b_ug])
                                            S.op("dve", lambda e: e.tensor_tensor(out=tact[:, Fl, cs], in0=f32(tact[:, Fl, cs]), in1=ug[:], op=ALU.mult), reads=[b_ug, b_tact[Fl]], writes=[b_tact[Fl]])
                            if e_ + 1 < NE:
                                idxn, bidxn = load_idx(e_ + 1)
                            for c in range(D // WCOL):
                                wt, bw = ws.get(wi); wi += 1
                                if e_ + 1 < NE:
                                    xbn, bxbn = gather(idxn, bidxn, c)
                                g2b_, bg2 = g2ring.next()
                                S.dma("sp", g2b_[:], mod_d[0:1, 5 * D + c * WCOL:5 * D + (c + 1) * WCOL].partition_broadcast(128), reads=[b_mod], writes=[bg2])
                                b2r, bb2 = b2ring.next()
                                S.dma("sp", b2r[:], r32(be2_d[e_:e_ + 1, c * WCOL:(c + 1) * WCOL]), writes=[bb2])
                                for st in range(NST):
                                    pt, bp = P[pi % 4]; pi += 1
                                    for kt in range(16):
                                        S.op("pe", lambda e: e.matmul(out=pt[:, 0:WCOL], lhsT=tact[:, kt, st * 128:(st + 1) * 128], rhs=wt[:, kt, :], start=(kt == 0), stop=False),
                                             reads=[b_tact[kt], bw], writes=[bp])
                                    S.op("pe", lambda e: e.matmul(out=pt[:, 0:WCOL], lhsT=ones_r[0:1, :], rhs=b2r[0:1, :], start=False, stop=True),
                                         reads=[b_ones, bb2], writes=[bp])
                                    yt, by = yring.next()
                                    S.op("dve", lambda e: e.tensor_tensor(out=yt[:], in0=pt[:, 0:WCOL], in1=g2b_[:], op=ALU.mult), reads=[bp, bg2], writes=[by])
                                    r0 = e_ * CAP + st * 128
                                    S.dma("pool", Y_d[r0:r0 + 128, c * WCOL:(c + 1) * WCOL], yt[:], reads=[by], writes=[b_Y[e_][st]])
                                if e_ + 1 < NE:
                                    transp(xbn, bxbn, c)
                    if lvl == 7:
                        final_bufs += [b for bb in b_Y for b in bb]

                if lvl >= 8:
                    with phase() as sb:
                        x1r = Ring([(sb(f"x1c{i}", [128, D]), Buf()) for i in range(2)])
                        ykr = Ring([(sb(f"yk{i}", [128, D]), Buf()) for i in range(4)])
                        allY = [b for bb in b_Y for b in bb]
                        for tt in range(NT):
                            xt, bx = x1r.next()
                            S.dma("sp", xt[:], x1_d[tt * 128:(tt + 1) * 128, :], reads=[b_x1[tt]], writes=[bx])
                            for k in range(4):
                                yk, byk = ykr.next()
                                S.dma("pool", None, None, reads=allY + [b_sidx], writes=[byk],
                                      fn=lambda e: e.indirect_dma_start(out=yk[:, :], out_offset=None, in_=Y_d[:, :],
                                                                        in_offset=bass.IndirectOffsetOnAxis(ap=sidx[:, tt, k:k + 1], axis=0),
                                                                        bounds_check=reg_slot, oob_is_err=False))
                                S.op("dve", lambda e: e.scalar_tensor_tensor(out=xt[:], in0=yk[:], scalar=gk[:, tt, k:k + 1], in1=xt[:], op0=ALU.mult, op1=ALU.add),
                                     reads=[byk, b_gk, bx], writes=[bx])
                            S.dma("sp", out_d[tt * 128:(tt + 1) * 128, :], xt[:], reads=[bx], writes=[b_out[tt]])
                    final_bufs += b_out

        S.finish(final_bufs)
        S.barrier()
    return nc


def _consts():
    c = {}
    c["ident"] = np.eye(128, dtype=np.float32)
    bo = np.zeros((128, 128), np.float32); bo[:64, :64] = 1; bo[64:, 64:] = 1
    c["bones"] = bo
    c["ones"] = np.ones((128, 128), np.float32)
    p = np.arange(128)[:, None]; col = np.arange(512)[None, :]
    cm = np.zeros((128, 4, 512), np.float32); wm = np.zeros((128, 4, 512), np.float32); cc = np.zeros((128, 4, 512), np.float32)
    for r in range(4):
        cm[:, r, :] = np.where(r * 128 + p <= col, 0.0, NEG)
        wm[:, r, :] = np.where(p + r * 128 > col, 0.0, NEG)
        cc[:, r, :] = np.where(16 * p + 31 <= r * 512 + col, 0.0, NEG)
    c["cmask"], c["wmask"], c["ccm"] = cm, wm, cc
    es = np.zeros((32, 16, 128), np.float32)
    for kt in range(16):
        for pp in range(128):
            es[2 * kt + pp // 64, kt, pp] = 1.0
    c["esel"] = es
    t = np.arange(S_LEN); qb = t // 64; j = np.arange(32)
    valid = j[None, :] <= qb[:, None]
    forced = (j[None, :] == 0) | (j[None, :] == qb[:, None]) | (j[None, :] == qb[:, None] - 1)
    V = (valid & ~forced).astype(np.float32)
    Fm = np.where(forced, 1e30, np.where(valid, 0.0, -1e30)).astype(np.float32)
    c["selv"] = np.ascontiguousarray(V.reshape(16, 128, 32).transpose(1, 0, 2))
    c["self"] = np.ascontiguousarray(Fm.reshape(16, 128, 32).transpose(1, 0, 2))
    cs = np.arange(128) * 16; ss = np.arange(32) * 64
    ov = ((cs[:, None] < ss[None, :] + 64) & (cs[:, None] + 32 > ss[None, :])).astype(np.float32)
    vc0 = np.zeros((128, 98), np.float32); vc0[:, 64] = 1.0; vc0[:, 66:98] = ov
    c["vcaug0"] = vc0
    va0 = np.zeros((128, NT, 2, 66), np.float32); va0[..., 64] = 1.0
    c["vaug0"] = va0
    c["triu"] = np.triu(np.ones((128, 128), np.float32), k=1)
    tk = (np.arange(NT, dtype=np.int32)[None, :] * 128 + np.arange(128, dtype=np.int32)[:, None]).astype(np.int32)
    c["tokid"] = np.ascontiguousarray(np.stack([tk, tk], axis=-1))
    c["oobidx"] = np.full((128, 2 * NE * CAP // 128), 4095, np.int32)
    c["iotas"] = np.broadcast_to(np.arange(CAP, dtype=np.float32)[None, :], (128, CAP)).copy()
    c["ebase"] = np.broadcast_to((np.arange(NE, dtype=np.float32) * CAP)[None, :], (128, NE)).copy()
    return c


def _cols(v, n=None):
    v = np.asarray(v, np.float32).reshape(-1, 128)
    return np.ascontiguousarray(v.T)


def prep_shared(inp):
    f = lambda k: np.asarray(inp[k], np.float32)[0]
    sh = dict(_consts())
    w_in = f("w_in")
    sh["wA"] = np.ascontiguousarray(np.concatenate([w_in[:, 0:1024], w_in[:, 1024:1280], w_in[:, 1280:1536], w_in[:, 1536:1792],
                                                    w_in[:, 2048:2304], w_in[:, 2608:3632], w_in[:, 3632:4656]], axis=1))
    wB = np.zeros((D, 768), np.float32)
    wB[:, 0:256] = w_in[:, 1792:2048]; wB[:, 256:512] = w_in[:, 2304:2560]; wB[:, 512:560] = w_in[:, 2560:2608]
    sh["wB"] = wB
    sh["w_ada"] = f("w_ada"); sh["b_ada"] = f("b_ada").reshape(1, -1)
    sh["g1c"] = _cols(f("g_norm1"))
    wck = f("w_cmp_k").reshape(32, 64, 64).transpose(1, 0, 2)
    wcv = f("w_cmp_v").reshape(32, 64, 64).transpose(1, 0, 2)
    wbd = np.zeros((128, 32, 128), np.float32)
    wbd[0:64, :, 0:64] = wck; wbd[64:128, :, 64:128] = wck
    sh["wck"] = wbd
    sh["wcv"] = np.ascontiguousarray(np.concatenate([wcv, wcv], axis=0))
    pek = f("pe_cmp_k").T; pev = f("pe_cmp_v").T
    pek = np.concatenate([pek, pek], axis=0)
    sh["pek2"] = np.ascontiguousarray(np.stack([pek, pek], axis=-1))
    sh["pevT"] = np.ascontiguousarray(np.concatenate([pev, pev], axis=0))
    qg = f("q_gain"); sh["qg"] = np.concatenate([qg, qg]).reshape(128, 1).copy()
    kgn = f("k_gain").T; sh["kg"] = np.ascontiguousarray(np.concatenate([kgn, kgn], axis=0))
    sh["cw"] = np.ascontiguousarray(f("conv_w").reshape(4, 8, 128).transpose(2, 1, 0))
    sh["cb"] = _cols(f("conv_b"))
    for nm, key in (("wrg", "w_rg"), ("wig", "w_ig")):
        w = f(key)
        bd = np.zeros((128, 8, 128), np.float32)
        for ct in range(8):
            bd[0:64, ct, 0:64] = w[2 * ct]; bd[64:128, ct, 64:128] = w[2 * ct + 1]
        sh[nm] = bd
    sh["brg"] = _cols(f("b_rg").reshape(-1)); sh["big"] = _cols(f("b_ig").reshape(-1))
    sh["lam"] = _cols(f("lru_lambda"))
    sh["gon"] = f("g_out_nsa").reshape(1, -1); sh["gol"] = _cols(f("g_out_lru"))
    sh["w_out"] = f("w_out"); sh["g2"] = f("g_norm2").reshape(1, -1)
    sh["wr"] = np.ascontiguousarray(f("w_router").reshape(16, 128, NE).transpose(1, 0, 2))
    sh["br"] = f("b_router").reshape(1, -1)
    sh["w_e1"] = f("w_e1"); sh["w_e2"] = f("w_e2")
    sh["b1c"] = np.ascontiguousarray(f("b_e1").reshape(NE, 32, 128).transpose(2, 0, 1))
    sh["b_e2"] = f("b_e2")
    return sh


def core_inputs(inp, sh, b):
    m = dict(sh)
    m["x"] = np.ascontiguousarray(np.asarray(inp["x"], np.float32)[b])
    m["csil"] = _cols(np.asarray(inp["c"], np.float32)[b])
    return m


_NC_CACHE = {}


def kernel(**inputs):
    sh = prep_shared(inputs)
    if "nc" not in _NC_CACHE:
        _NC_CACHE["nc"] = build_nc("all", False)
    nc = _NC_CACHE["nc"]
    in_maps = [core_inputs(inputs, sh, b) for b in range(8)]
    res = run_bass_kernel_spmd(nc, in_maps, core_ids=list(range(8)))
    return np.stack([np.asarray(r["out"], np.float32) for r in res.results], axis=0)
```

```python
from contextlib import ExitStack, contextmanager

import numpy as np
import concourse.bass as bass
import concourse.mybir as mybir
from concourse.bass_utils import run_bass_kernel_spmd

F32 = mybir.dt.float32
F32R = mybir.dt.float32r
BF16 = mybir.dt.bfloat16
I32 = mybir.dt.int32
AF = mybir.ActivationFunctionType
ALU = mybir.AluOpType

S_LEN = 2048
D = 2048
NT = 16
NE = 32
CAP = 1024
NST = CAP // 128
EPS = 1e-6
NEG = -30000.0
WCOL = 256


class Buf:
    __slots__ = ("name", "w", "r")

    def __init__(self, name=""):
        self.name = name
        self.w = None
        self.r = {}


class Sched:
    def __init__(self, nc, stack, n_dma_slots=12):
        self.nc = nc
        self.eng = {"pe": nc.tensor, "act": nc.scalar, "dve": nc.vector, "pool": nc.gpsimd, "sp": nc.sync}
        self.sem = {k: stack.enter_context(nc.semaphore("s_" + k)) for k in self.eng}
        self.cnt = {k: 0 for k in self.eng}
        self.waited = {k: {} for k in self.eng}
        self.slots = {}
        for q in ("sp", "pool"):
            self.slots[q] = [[stack.enter_context(nc.semaphore(f"d_{q}{i}")), 0] for i in range(n_dma_slots)]
        self.slot_i = {q: 0 for q in self.slots}
        self.nwaits = 0

    def _wait(self, e, ev):
        if ev is None:
            return
        sem, val = ev
        if e == "pe" and sem is self.sem["pe"]:
            return
        key = id(sem)
        if self.waited[e].get(key, 0) >= val:
            return
        self.eng[e].wait_ge(sem, val)
        self.waited[e][key] = val
        self.nwaits += 1

    def _deps(self, e, reads, writes):
        for b in reads:
            self._wait(e, b.w)
        for b in writes:
            self._wait(e, b.w)
            for ev in b.r.values():
                self._wait(e, ev)

    def _mark(self, ev, reads, writes):
        sem, val = ev
        for b in reads:
            b.r[id(sem)] = ev
        for b in writes:
            b.w = ev
            b.r = {}

    def op(self, e, fn, reads=(), writes=()):
        self._deps(e, reads, writes)
        ins = fn(self.eng[e])
        self.cnt[e] += 1
        ins.then_inc(self.sem[e], 1)
        ev = (self.sem[e], self.cnt[e])
        self._mark(ev, reads, writes)
        return ev

    def dma(self, q, out, in_, reads=(), writes=(), fn=None):
        slots = self.slots[q]
        i = self.slot_i[q]
        self.slot_i[q] = (i + 1) % len(slots)
        sem, uses = slots[i]
        if uses:
            self._wait(q, (sem, 16 * uses))
        self._deps(q, reads, writes)
        if fn is None:
            ins = self.eng[q].dma_start(out=out, in_=in_)
        else:
            ins = fn(self.eng[q])
        ins.then_inc(sem, 16)
        slots[i][1] = uses + 1
        ev = (sem, 16 * (uses + 1))
        self._mark(ev, reads, writes)
        return ev

    def barrier(self):
        evs = [(self.sem[k], self.cnt[k]) for k in self.eng if self.cnt[k] > 0]
        for q in self.slots:
            for sem, uses in self.slots[q]:
                if uses:
                    evs.append((sem, 16 * uses))
        for e in self.eng:
            for ev in evs:
                self._wait(e, ev)

    def finish(self, bufs):
        for b in bufs:
            self._wait("sp", b.w)
            for ev in b.r.values():
                self._wait("sp", ev)


class Ring:
    def __init__(self, items):
        self.items = items
        self.i = 0

    def next(self):
        it = self.items[self.i]
        self.i = (self.i + 1) % len(self.items)
        return it


def r32(ap):
    return ap.bitcast(F32R)


def f32(ap):
    return ap.bitcast(F32)


def build_nc(upto="all", dbg=False):
    nc = bass.Bass("TRN2", target_bir_lowering=False)
    nc.dge_precook = False
    order = ["mod", "inproj", "lru", "nsa", "nsaout", "outproj", "router", "experts", "combine", "all"]
    lvl = order.index(upto)

    def din(n, shp, dt=F32):
        return nc.dram_tensor(n, list(shp), dt, kind="ExternalInput").ap()

    dbg_sets = {"mod": ["mod_s"], "inproj": ["zT_s", "ztm_s"], "lru": ["onT_s"], "nsa": ["onsa_s"], "nsaout": ["onT_s"],
                "outproj": ["x1_s"], "router": ["rinfo_s"], "experts": [], "combine": [], "all": []}

    def dscr(n, shp, dt=F32):
        ext = dbg and n in dbg_sets[upto]
        return nc.dram_tensor(n, list(shp), dt, kind=("ExternalOutput" if ext else "Internal")).ap()

    x_d = din("x", [S_LEN, D])
    csil_d = din("csil", [128, 16])
    wada_d = din("w_ada", [D, 6 * D])
    bada_d = din("b_ada", [1, 6 * D])
    g1c_d = din("g1c", [128, 16])
    wA_d = din("wA", [D, 4096])
    wB_d = din("wB", [D, 768])
    wck_d = din("wck", [128, 32, 128])
    wcv_d = din("wcv", [128, 32, 64])
    pek2_d = din("pek2", [128, 32, 2])
    pevT_d = din("pevT", [128, 32])
    qg_d = din("qg", [128, 1])
    kg_d = din("kg", [128, 3])
    cw_d = din("cw", [128, 8, 4])
    cb_d = din("cb", [128, 8])
    wrg_d = din("wrg", [128, 8, 128])
    wig_d = din("wig", [128, 8, 128])
    brg_d = din("brg", [128, 8])
    big_d = din("big", [128, 8])
    lam_d = din("lam", [128, 8])
    gon_d = din("gon", [1, 1024])
    gol_d = din("gol", [128, 8])
    wout_d = din("w_out", [D, D])
    g2_d = din("g2", [1, D])
    wr_d = din("wr", [128, 16, NE])
    br_d = din("br", [1, NE])
    we1_d = din("w_e1", [NE, D, 2 * D]) if lvl >= 7 else None
    b1c_d = din("b1c", [128, NE, 32])
    we2_d = din("w_e2", [NE, D, D]) if lvl >= 7 else None
    be2_d = din("b_e2", [NE, D])
    ident_d = din("ident", [128, 128])
    bones_d = din("bones", [128, 128])
    cmask_d = din("cmask", [128, 4, 512])
    wmask_d = din("wmask", [128, 4, 512])
    ccm_d = din("ccm", [128, 4, 512])
    esel_d = din("esel", [128, 16, 128])
    zeros_d = din("zeros", [128, 2 * S_LEN])
    selv_d = din("selv", [128, 16, 32])
    self_d = din("self", [128, 16, 32])
    triu_d = din("triu", [128, 128])
    iotas_d = din("iotas", [128, CAP])
    ebase_d = din("ebase", [128, NE])
    ones_d = din("ones", [128, 128])
    tokid_d = din("tokid", [128, NT, 2], I32)
    oobidx_d = din("oobidx", [128, 2 * NE * CAP // 128], I32)
    vaug0_d = din("vaug0", [128, NT, 2, 66])
    vcaug0_d = din("vcaug0", [128, 98])

    out_d = nc.dram_tensor("out", [S_LEN, D], F32, kind="ExternalOutput").ap()
    mod_d = dscr("mod_s", [1, 6 * D])
    zT_d = dscr("zT_s", [4096, S_LEN])
    ztm_d = dscr("ztm_s", [S_LEN, 768])
    onsa_d = dscr("onsa_s", [S_LEN, 1024])
    onT_d = dscr("onT_s", [D, S_LEN])
    x1_d = dscr("x1_s", [S_LEN, D])
    Y_d = dscr("Y_s", [NE * CAP, D])
    rinfo_d = dscr("rinfo_s", [S_LEN, 8])
    h2_d = dscr("h2_s", [S_LEN, D])
    tokidx_d = dscr("tokidx_s", [NE * CAP, 2], I32)
    b_h2d = [Buf() for _ in range(NT)]
    b_tokidx = Buf("tokidx")

    b_mod = Buf("mod_d")
    b_zT = [Buf(f"zT{i}") for i in range(32)]
    b_ztm = [Buf(f"ztm{i}") for i in range(NT)]
    b_onsa = [[Buf() for _ in range(2)] for _ in range(NT)]
    b_onT = [Buf(f"onT{i}") for i in range(16)]
    b_x1 = [Buf(f"x1{i}") for i in range(NT)]
    b_Y = [[Buf() for _ in range(NST)] for _ in range(NE)]
    b_out = [Buf(f"out{i}") for i in range(NT)]
    b_rinfo = Buf("rinfo")
    final_bufs = []

    with ExitStack() as top:
        S = Sched(nc, top)
        reg_slot = nc.gpsimd.to_reg(NE * CAP - 1)
        reg_tok = nc.gpsimd.to_reg(S_LEN - 1)

        uniq = [0]

        def mk(stack):
            def sb(n, shp, dt=F32):
                uniq[0] += 1
                return stack.enter_context(nc.sbuf_tensor(f"sb{uniq[0]}_{n}", list(shp), dt))
            return sb

        sbt = mk(top)

        @contextmanager
        def phase():
            with ExitStack() as ph_:
                yield mk(ph_)
                S.barrier()
        P = [(top.enter_context(nc.psum_tensor(f"P{i}", [128, 512], F32)), Buf(f"P{i}")) for i in range(8)]

        ident = sbt("ident", [128, 128]); b_ident = Buf("ident")
        S.dma("sp", ident[:], ident_d[:, :], writes=[b_ident])
        identr = sbt("identr", [128, 128], F32R); b_identr = Buf("identr")
        S.dma("sp", identr[:], r32(ident_d[:, :]), writes=[b_identr])
        def mkwring(sb_, n=3):
            return Ring([(sb_(f"wch{i}", [128, 16, WCOL], F32R), Buf(f"wch{i}")) for i in range(n)])

        def wsrc(w2d, c):
            return r32(w2d[:, c * WCOL:(c + 1) * WCOL]).rearrange("(kt p) f -> p kt f", p=128)

        class WStream:
            def __init__(self, wring, srcs, ahead=2):
                self.wring = wring
                self.srcs = srcs
                self.loaded = []
                self.ahead = ahead

            def get(self, k):
                while len(self.loaded) <= min(k + self.ahead, len(self.srcs) - 1):
                    t, b = self.wring.next()
                    S.dma("sp", t[:], self.srcs[len(self.loaded)], writes=[b])
                    self.loaded.append((t, b))
                return self.loaded[k]

        with phase() as sb:
            cs = sb("cs", [128, 16]); b_cs = Buf()
            S.dma("sp", cs[:], csil_d[:, :], writes=[b_cs])
            scr = sb("scr", [128, 16], F32R); b_scr = Buf()
            S.op("act", lambda e: e.activation(out=scr[:], in_=cs[:], func=AF.Silu), reads=[b_cs], writes=[b_scr])
            barow = Ring([(sb(f"barow{i}", [1, WCOL]), Buf()) for i in range(3)])
            mrow = Ring([(sb(f"mrow{i}", [1, WCOL]), Buf()) for i in range(3)])
            ws = WStream(mkwring(sb), [wsrc(wada_d, c) for c in range(6 * D // WCOL)])
            for c in range(6 * D // WCOL):
                wt, bw = ws.get(c)
                bt, bb = barow.next()
                S.dma("sp", bt[:], bada_d[:, c * WCOL:(c + 1) * WCOL], writes=[bb])
                pt, bp = P[c % 2]
                for kt in range(16):
                    S.op("pe", lambda e: e.matmul(out=pt[0:1, 0:WCOL], lhsT=scr[:, kt:kt + 1], rhs=wt[:, kt, :],
                                                  start=(kt == 0), stop=(kt == 15)),
                         reads=[b_scr, bw], writes=[bp])
                mt, bm = mrow.next()
                S.op("dve", lambda e: e.tensor_tensor(out=mt[:], in0=pt[0:1, 0:WCOL], in1=bt[:], op=ALU.add),
                     reads=[bp, bb], writes=[bm])
                S.dma("sp", mod_d[:, c * WCOL:(c + 1) * WCOL], mt[:], reads=[bm], writes=[b_mod])
        if lvl == 0:
            final_bufs.append(b_mod)

        def mod_cols(stack_sb, name, off):
            t = stack_sb(name, [128, 16]); b = Buf()
            S.dma("sp", None, None, reads=[b_mod], writes=[b],
                  fn=lambda e: e.dma_start(out=t[:], in_=mod_d[:, off:off + D].rearrange("o (c p) -> p (o c)", p=128),
                                           allow_slow_non_contiguous=True))
            return t, b

        def bc_load(t_ap, row_ap, reads, b):
            S.dma("sp", t_ap, row_ap.partition_broadcast(128), reads=reads, writes=[b])

        if lvl >= 1:
            with phase() as sb:
                sh1, b_sh1 = mod_cols(sb, "sh1", 0)
                sc1, b_sc1 = mod_cols(sb, "sc1", D)
                g1c = sb("g1c", [128, 16]); b_g1c = Buf()
                S.dma("sp", g1c[:], g1c_d[:, :], writes=[b_g1c])
                A1c = sb("A1c", [128, 16]); b_A1c = Buf()
                S.op("dve", lambda e: e.scalar_tensor_tensor(out=A1c[:], in0=sc1[:], scalar=1.0, in1=g1c[:], op0=ALU.add, op1=ALU.mult),
                     reads=[b_sc1, b_g1c], writes=[b_A1c])
                hT = sb("hT", [128, 16, S_LEN], F32R)
                b_hT = [Buf(f"hT{i}") for i in range(NT)]
                tmp = ExitStack(); sb2 = mk(tmp)
                xring = Ring([(sb2(f"xin{i}", [128, D]), Buf()) for i in range(2)])
                xs = sb2("xs", [128, D]); b_xs = Buf()
                sqj = sb2("sqj", [128, D]); b_sqj = Buf()
                ss = sb2("ss", [128, 1]); b_ss = Buf()
                rs = sb2("rs", [128, 1]); b_rs = Buf()
                for tt in range(NT):
                    xt, bx = xring.next()
                    S.dma("sp", xt[:], x_d[tt * 128:(tt + 1) * 128, :], writes=[bx])
                    S.op("act", lambda e: e.activation(out=sqj[:], in_=xt[:], func=AF.Square, accum_out=ss[:]),
                         reads=[bx], writes=[b_sqj, b_ss])
                    S.op("dve", lambda e: e.tensor_scalar(out=rs[:], in0=ss[:], scalar1=1.0 / D, scalar2=EPS, op0=ALU.mult, op1=ALU.add),
                         reads=[b_ss], writes=[b_rs])
                    S.op("act", lambda e: e.activation(out=rs[:], in_=rs[:], func=AF.Sqrt), reads=[b_rs], writes=[b_rs])
                    S.op("dve", lambda e: e.reciprocal(out=rs[:], in_=rs[:]), reads=[b_rs], writes=[b_rs])
                    S.op("dve", lambda e: e.tensor_scalar(out=xs[:], in0=xt[:], scalar1=rs[:, 0:1], scalar2=None, op0=ALU.mult),
                         reads=[bx, b_rs], writes=[b_xs])
                    for g in range(4):
                        pt, bp = P[4 + g % 4]
                        for j in range(4):
                            dc = g * 4 + j
                            S.op("pe", lambda e: e.transpose(out=pt[:, j * 128:(j + 1) * 128], in_=xs[:, dc * 128:(dc + 1) * 128], identity=ident[:]),
                                 reads=[b_xs, b_ident], writes=[bp])
                        for j in range(4):
                            dc = g * 4 + j
                            eng = "dve" if j % 2 == 0 else "pool"
                            if eng == "pool":
                                S.op("act", lambda e: e.activation(out=hT[:, dc, tt * 128:(tt + 1) * 128], in_=pt[:, j * 128:(j + 1) * 128],
                                                                   func=AF.Identity, scale=A1c[:, dc:dc + 1], bias=sh1[:, dc:dc + 1]),
                                     reads=[bp, b_A1c, b_sh1], writes=[b_hT[tt]])
                            else:
                                S.op("dve", lambda e: e.tensor_scalar(out=hT[:, dc, tt * 128:(tt + 1) * 128], in0=pt[:, j * 128:(j + 1) * 128],
                                                                      scalar1=A1c[:, dc:dc + 1], scalar2=sh1[:, dc:dc + 1], op0=ALU.mult, op1=ALU.add),
                                     reads=[bp, b_A1c, b_sh1], writes=[b_hT[tt]])
                S.barrier()
                tmp.close()
                stg = Ring([(sb(f"stgA{i}", [128, 512]), Buf()) for i in range(4)])
                srcs = [wsrc(wA_d, c) for c in range(16)] + [wsrc(wB_d, c) for c in range(3)]
                ws = WStream(mkwring(sb), srcs)
                pi = 0
                for c in range(16):
                    wt, bw = ws.get(c)
                    for ft in range(2):
                        rowtile = c * 2 + ft
                        for tc in range(4):
                            pt, bp = P[pi % 4]; pi += 1
                            for kt in range(16):
                                S.op("pe", lambda e: e.matmul(out=pt[:], lhsT=wt[:, kt, ft * 128:(ft + 1) * 128], rhs=hT[:, kt, tc * 512:(tc + 1) * 512],
                                                              start=(kt == 0), stop=(kt == 15)),
                                     reads=[bw] + b_hT[tc * 4:(tc + 1) * 4], writes=[bp])
                            st_, bs = stg.next()
                            if pi % 2 == 0:
                                S.op("act", lambda e: e.copy(out=st_[:], in_=pt[:]), reads=[bp], writes=[bs])
                            else:
                                S.op("dve", lambda e: e.tensor_copy(out=st_[:], in_=pt[:]), reads=[bp], writes=[bs])
                            S.dma("pool", zT_d[rowtile * 128:(rowtile + 1) * 128, tc * 512:(tc + 1) * 512], st_[:], reads=[bs], writes=[b_zT[rowtile]])
                for cb_ in range(3):
                    wt, bw = ws.get(16 + cb_)
                    for tt in range(NT):
                        pt, bp = P[pi % 4]; pi += 1
                        for kt in range(16):
                            S.op("pe", lambda e: e.matmul(out=pt[:, 0:WCOL], lhsT=hT[:, kt, tt * 128:(tt + 1) * 128], rhs=wt[:, kt, :],
                                                          start=(kt == 0), stop=(kt == 15)),
                                 reads=[bw, b_hT[tt]], writes=[bp])
                        st_, bs = stg.next()
                        S.op("act", lambda e: e.copy(out=st_[:, 0:WCOL], in_=pt[:, 0:WCOL]), reads=[bp], writes=[bs])
                        S.dma("pool", ztm_d[tt * 128:(tt + 1) * 128, cb_ * WCOL:(cb_ + 1) * WCOL], st_[:, 0:WCOL], reads=[bs], writes=[b_ztm[tt]])
            if lvl == 1:
                final_bufs += b_zT + b_ztm

        if lvl >= 2:
            with phase() as sb:

                def ld(name, shp, src, dt=F32):
                    t = sb(name, shp, dt); b = Buf()
                    S.dma("sp", t[:], src, writes=[b])
                    return t, b
                cw, b_cw = ld("cw", [128, 8, 4], cw_d[:, :, :])
                cb, b_cb = ld("cb", [128, 8], cb_d[:, :])
                wrg, b_wrg = ld("wrg", [128, 8, 128], r32(wrg_d[:, :, :]), F32R)
                wig, b_wig = ld("wig", [128, 8, 128], r32(wig_d[:, :, :]), F32R)
                brg, b_brg = ld("brg", [128, 8], brg_d[:, :])
                big, b_big = ld("big", [128, 8], big_d[:, :])
                lam, b_lam = ld("lam", [128, 8], lam_d[:, :])
                gol, b_gol = ld("gol", [128, 8], gol_d[:, :])
                ones_r, b_ones = ld("ones_r", [128, 128], r32(ones_d[:, :]), F32R)
                clam = sb("clam", [128, 8]); b_clam = Buf()
                clam2 = sb("clam2", [128, 8]); b_clam2 = Buf()
                S.op("act", lambda e: e.activation(out=clam[:], in_=lam[:], func=AF.Sigmoid), reads=[b_lam], writes=[b_clam])
                S.op("act", lambda e: e.activation(out=clam[:], in_=clam[:], func=AF.Ln), reads=[b_clam], writes=[b_clam])
                S.op("dve", lambda e: e.tensor_scalar(out=clam2[:], in0=clam[:], scalar1=16.0, scalar2=None, op0=ALU.mult), reads=[b_clam], writes=[b_clam2])
                S.op("dve", lambda e: e.tensor_scalar(out=clam[:], in0=clam[:], scalar1=8.0, scalar2=None, op0=ALU.mult), reads=[b_clam, b_clam2], writes=[b_clam])
                olru = sb("olru", [128, 8, S_LEN]); b_olru = [Buf() for _ in range(8)]
                xrp_r = Ring([(sb(f"xrp{i}", [128, 3 + S_LEN]), Buf()) for i in range(2)])
                xg_r = Ring([(sb(f"xg{i}", [128, S_LEN]), Buf()) for i in range(2)])
                xc = sb("xc", [128, S_LEN], F32R); b_xc = Buf()
                xcacc = sb("xcacc", [128, S_LEN])
                rt = sb("rt", [128, S_LEN]); b_rt = Buf()
                it = sb("it", [128, S_LEN]); b_it = Buf()
                at = sb("at", [128, S_LEN]); b_at = Buf()
                ut = sb("ut", [128, S_LEN]); b_ut = Buf()
                ht = sb("ht", [128, S_LEN]); b_ht = Buf()
                gt = sb("gt", [128, S_LEN]); b_gt = Buf()
                osq = sb("osq", [128, S_LEN], F32R); b_osq = Buf()
                for ct in range(8):
                    xrp, bxr = xrp_r.next()
                    xg, bxg = xg_r.next()
                    S.op("pool", lambda e: e.memset(xrp[:, 0:3], 0.0), writes=[bxr])
                    S.dma("sp", xrp[:, 3:3 + S_LEN], zT_d[2048 + ct * 128:2048 + (ct + 1) * 128, :], reads=[b_zT[16 + ct]], writes=[bxr])
                    S.dma("sp", xg[:], zT_d[3072 + ct * 128:3072 + (ct + 1) * 128, :], reads=[b_zT[24 + ct]], writes=[bxg])
                    xcf = xcacc[:]
                    S.op("dve", lambda e: e.tensor_scalar(out=xcf, in0=xrp[:, 0:S_LEN], scalar1=cw[:, ct, 0:1], scalar2=cb[:, ct:ct + 1], op0=ALU.mult, op1=ALU.add),
                         reads=[bxr, b_cw, b_cb], writes=[b_xc])
                    for k in range(1, 4):
                        outap = xc[:] if k == 3 else xcf
                        S.op("dve", lambda e: e.scalar_tensor_tensor(out=outap, in0=xrp[:, k:k + S_LEN], scalar=cw[:, ct, k:k + 1], in1=xcf, op0=ALU.mult, op1=ALU.add),
                             reads=[bxr, b_cw, b_xc], writes=[b_xc])
                    xcf = f32(xc[:])
                    for (wg, bwg, bg, bbg, dst, bdst) in ((wrg, b_wrg, brg, b_brg, rt, b_rt), (wig, b_wig, big, b_big, it, b_it)):
                        for tc in range(4):
                            pt, bp = P[tc % 2]
                            S.op("pe", lambda e: e.matmul(out=pt[:], lhsT=wg[:, ct, :], rhs=xc[:, tc * 512:(tc + 1) * 512], start=True, stop=True),
                                 reads=[bwg, b_xc], writes=[bp])
                            S.op("act", lambda e: e.activation(out=dst[:, tc * 512:(tc + 1) * 512], in_=pt[:], func=AF.Sigmoid, bias=bg[:, ct:ct + 1]),
                                 reads=[bp, bbg], writes=[bdst])
                    S.op("act", lambda e: e.activation(out=at[:], in_=rt[:], func=AF.Exp, scale=clam[:, ct:ct + 1]), reads=[b_rt, b_clam], writes=[b_at])
                    S.op("act", lambda e: e.activation(out=ut[:], in_=rt[:], func=AF.Exp, scale=clam2[:, ct:ct + 1]), reads=[b_rt, b_clam2], writes=[b_ut])
                    S.op("act", lambda e: e.activation(out=ut[:], in_=ut[:], func=AF.Sqrt, scale=-1.0, bias=1.0), reads=[b_ut], writes=[b_ut])
                    S.op("dve", lambda e: e.tensor_tensor(out=ut[:], in0=ut[:], in1=it[:], op=ALU.mult), reads=[b_ut, b_it], writes=[b_ut])
                    S.op("dve", lambda e: e.tensor_tensor(out=ut[:], in0=ut[:], in1=xcf, op=ALU.mult), reads=[b_ut, b_xc], writes=[b_ut])
                    S.op("dve", lambda e: e.tensor_tensor_scan(out=ht[:], data0=at[:], data1=ut[:], initial=0.0, op0=ALU.mult, op1=ALU.add),
                         reads=[b_at, b_ut], writes=[b_ht])
                    S.op("act", lambda e: e.activation(out=gt[:], in_=xg[:], func=AF.Square), reads=[bxg], writes=[b_gt])
                    S.op("dve", lambda e: e.tensor_scalar(out=gt[:], in0=gt[:], scalar1=0.044715, scalar2=1.0, op0=ALU.mult, op1=ALU.add), reads=[b_gt], writes=[b_gt])
                    S.op("dve", lambda e: e.tensor_tensor(out=gt[:], in0=gt[:], in1=xg[:], op=ALU.mult), reads=[b_gt, bxg], writes=[b_gt])
                    S.op("act", lambda e: e.activation(out=gt[:], in_=gt[:], func=AF.Sigmoid, scale=1.5957691216057308), reads=[b_gt], writes=[b_gt])
                    S.op("dve", lambda e: e.tensor_tensor(out=gt[:], in0=gt[:], in1=xg[:], op=ALU.mult), reads=[b_gt, bxg], writes=[b_gt])
                    S.op("dve", lambda e: e.tensor_tensor(out=olru[:, ct, :], in0=gt[:], in1=ht[:], op=ALU.mult), reads=[b_gt, b_ht], writes=[b_olru[ct]])
                    S.op("act", lambda e: e.activation(out=osq[:], in_=olru[:, ct, :], func=AF.Square), reads=[b_olru[ct]], writes=[b_osq])
                    for tc in range(4):
                        pt, bp = P[4 + tc]
                        S.op("pe", lambda e: e.matmul(out=pt[:], lhsT=ones_r[:], rhs=osq[:, tc * 512:(tc + 1) * 512], start=(ct == 0), stop=(ct == 7)),
                             reads=[b_ones, b_osq], writes=[bp])
                rb = rt; b_rb = b_rt
                for tc in range(4):
                    pt, bp = P[4 + tc]
                    S.op("dve", lambda e: e.tensor_scalar(out=rb[:, tc * 512:(tc + 1) * 512], in0=pt[:], scalar1=1.0 / 1024, scalar2=EPS, op0=ALU.mult, op1=ALU.add),
                         reads=[bp], writes=[b_rb])
                S.op("act", lambda e: e.activation(out=rb[:], in_=rb[:], func=AF.Sqrt), reads=[b_rb], writes=[b_rb])
                S.op("dve", lambda e: e.reciprocal(out=rb[:], in_=rb[:]), reads=[b_rb], writes=[b_rb])
                ostg = Ring([(at, b_at), (ut, b_ut)])
                for ct in range(8):
                    st_, bs = ostg.next()
                    S.op("dve", lambda e: e.scalar_tensor_tensor(out=st_[:], in0=olru[:, ct, :], scalar=gol[:, ct:ct + 1], in1=rb[:], op0=ALU.mult, op1=ALU.mult),
                         reads=[b_olru[ct], b_gol, b_rb], writes=[bs])
                    S.dma("pool", onT_d[1024 + ct * 128:1024 + (ct + 1) * 128, :], st_[:], reads=[bs], writes=[b_onT[8 + ct]])
            if lvl == 2:
                final_bufs += b_onT[8:]

        if lvl >= 3:
            with phase() as sb:
                def ld(name, shp, src, dt=F32, reads=()):
                    t = sb(name, shp, dt); b = Buf()
                    S.dma("sp", t[:], src, reads=list(reads), writes=[b])
                    return t, b
                bones, b_bones = ld("bones", [128, 128], r32(bones_d[:, :]), F32R)
                cmask, b_cmask = ld("cmask", [128, 4, 512], r32(cmask_d[:, :, :]), F32R)
                wmask, b_wmask = ld("wmask", [128, 4, 512], r32(wmask_d[:, :, :]), F32R)
                ccm, b_ccm = ld("ccm", [128, 4, 512], r32(ccm_d[:, :, :]), F32R)
                esel, b_esel = ld("esel", [128, 16, 128], r32(esel_d[:, :, :]), F32R)
                selv, b_selv = ld("selv", [128, 16, 32], selv_d[:, :, :])
                self_, b_self = ld("self", [128, 16, 32], self_d[:, :, :])
                qg, b_qg = ld("qg", [128, 1], qg_d[:, :])
                kg, b_kg = ld("kg", [128, 3], kg_d[:, :])
                ones_r, b_ones = ld("ones_r", [128, 128], r32(ones_d[:, :]), F32R)
                qgs = sb("qgs", [128, 1]); b_qgs = Buf()
                S.op("dve", lambda e: e.tensor_scalar(out=qgs[:], in0=qg[:], scalar1=0.125, scalar2=None, op0=ALU.mult), reads=[b_qg], writes=[b_qgs])
                gsig, b_gsig = ld("gsig", [128, NT, 48], ztm_d[:, 512:560].rearrange("(t p) c -> p t c", p=128), reads=b_ztm)
                S.op("act", lambda e: e.activation(out=gsig[:], in_=gsig[:], func=AF.Sigmoid), reads=[b_gsig], writes=[b_gsig])

                qn = sb("qn", [128, 4, S_LEN], F32R); b_qn = [Buf() for _ in range(4)]
                kns = sb("kns", [128, 2, S_LEN], F32R); b_kns = Buf()
                knw = sb("knw", [128, 2, S_LEN], F32R); b_knw = Buf()
                S.dma("sp", kns[:].rearrange("p a b -> p (a b)"), r32(zeros_d[:, :]), writes=[b_kns])
                S.dma("sp", knw[:].rearrange("p a b -> p (a b)"), r32(zeros_d[:, :]), writes=[b_knw])
                kcn = sb("kcn", [128, 128], F32R); b_kcn = Buf()
                vcaug = sb("vcaug", [128, 2, 98], F32R); b_vcaug = Buf()
                vs = sb("vs", [128, NT, 2, 66], F32R); b_vs = Buf()
                vw = sb("vw", [128, NT, 2, 66], F32R); b_vw = Buf()

                for pair in range(2):
                    g0 = 2 * pair
                    with phase() as sp_:
                        wck, b_wck = sp_("wck", [128, 32, 128], F32R), Buf()
                        S.dma("sp", wck[:], r32(wck_d[:, :, :]), writes=[b_wck])
                        wcv, b_wcv = sp_("wcv", [128, 32, 64], F32R), Buf()
                        S.dma("sp", wcv[:], r32(wcv_d[:, :, :]), writes=[b_wcv])
                        pek2, b_pek2 = sp_("pek2", [128, 32, 2], F32R), Buf()
                        S.dma("sp", pek2[:], r32(pek2_d[:, :, :]), writes=[b_pek2])
                        pevT, b_pevT = sp_("pevT", [128, 32], F32R), Buf()
                        S.dma("sp", pevT[:], r32(pevT_d[:, :]), writes=[b_pevT])
                        raw = sp_("raw", [128, S_LEN]); b_raw = Buf()
                        sqt = sp_("sqt", [128, S_LEN + 16], F32R); b_sqt = Buf()
                        rr = sp_("rr", [128, 512]); b_rr = Buf()
                        kcraw = sp_("kcraw", [128, 128]); b_kcraw = Buf()
                        biasc = sp_("biasc", [128, 1]); b_biasc = Buf()
                        biasv = sp_("biasv", [1, 64], F32R); b_biasv = Buf()

                        def rms_feat(rows, gcol_ap, b_gcol, dst_fn, b_dst):
                            for hf, r0 in enumerate(rows):
                                S.dma("sp", raw[hf * 64:(hf + 1) * 64, :], zT_d[r0:r0 + 64, :], reads=[b_zT[r0 // 128]], writes=[b_raw])
                            S.op("act", lambda e: e.activation(out=sqt[:, 0:S_LEN], in_=raw[:], func=AF.Square), reads=[b_raw], writes=[b_sqt])
                            for tc in range(4):
                                pt, bp = P[6 + tc % 2]
                                S.op("pe", lambda e: e.matmul(out=pt[:], lhsT=bones[:], rhs=sqt[:, tc * 512:(tc + 1) * 512], start=True, stop=True),
                                     reads=[b_bones, b_sqt], writes=[bp])
                                S.op("dve", lambda e: e.tensor_scalar(out=rr[:], in0=pt[:], scalar1=1.0 / 64, scalar2=EPS, op0=ALU.mult, op1=ALU.add), reads=[bp], writes=[b_rr])
                                S.op("act", lambda e: e.activation(out=rr[:], in_=rr[:], func=AF.Sqrt), reads=[b_rr], writes=[b_rr])
                                S.op("dve", lambda e: e.reciprocal(out=rr[:], in_=rr[:]), reads=[b_rr], writes=[b_rr])
                                for (dst_ap, psl) in dst_fn(tc):
                                    S.op("dve", lambda e: e.scalar_tensor_tensor(out=dst_ap, in0=raw[psl, tc * 512:(tc + 1) * 512], scalar=gcol_ap[psl, :], in1=rr[psl, :], op0=ALU.mult, op1=ALU.mult),
                                         reads=[b_raw, b_gcol, b_rr], writes=[b_dst])

                        for i in range(4):
                            ha, hb = 4 * g0 + i, 4 * (g0 + 1) + i
                            rms_feat([ha * 64, hb * 64], qgs[:, 0:1], b_qgs, lambda tc: [(qn[:, i, tc * 512:(tc + 1) * 512], slice(0, 128))], b_qn[i])
                        rms_feat([1536 + g0 * 64, 1536 + g0 * 64 + 64], kg[:, 1:2], b_kg, lambda tc: [(kns[0:64, 0, tc * 512:(tc + 1) * 512], slice(0, 64)), (kns[64:128, 1, tc * 512:(tc + 1) * 512], slice(64, 128))], b_kns)
                        rms_feat([1792 + g0 * 64, 1792 + g0 * 64 + 64], kg[:, 2:3], b_kg, lambda tc: [(knw[0:64, 0, tc * 512:(tc + 1) * 512], slice(0, 64)), (knw[64:128, 1, tc * 512:(tc + 1) * 512], slice(64, 128))], b_knw)

                        S.dma("sp", sqt[:, 0:S_LEN], r32(zT_d[1024 + g0 * 64:1024 + g0 * 64 + 128, :]), reads=[b_zT[8 + pair]], writes=[b_sqt])
                        S.dma("sp", sqt[:, S_LEN:S_LEN + 16], r32(vaug0_d[:, 0, 0, 0:16]), writes=[b_sqt])
                        pk, bpk = P[6]
                        pb, bpb = P[7]
                        for j in range(32):
                            S.op("pe", lambda e: e.matmul(out=pk[:, 0:128], lhsT=wck[:, j, :], rhs=sqt[:, j:j + 16 * 127 + 1:16],
                                                          start=(j == 0), stop=(j == 31)),
                                 reads=[b_wck, b_sqt], writes=[bpk])
                        for j in range(32):
                            S.op("pe", lambda e: e.matmul(out=pb[:, 0:2], lhsT=wck[:, j, :], rhs=pek2[:, j, :],
                                                          start=(j == 0), stop=(j == 31)),
                                 reads=[b_wck, b_pek2], writes=[bpb])
                        S.op("dve", lambda e: e.tensor_copy(out=biasc[:], in_=pb[:, 0:1]), reads=[bpb], writes=[b_biasc])
                        S.op("dve", lambda e: e.tensor_scalar(out=kcraw[:], in0=pk[:, 0:128], scalar1=biasc[:, 0:1], scalar2=None, op0=ALU.add),
                             reads=[bpk, b_biasc], writes=[b_kcraw])
                        sq128 = sp_("sq128", [128, 128], F32R); b_sq128 = Buf()
                        S.op("act", lambda e: e.activation(out=sq128[:], in_=kcraw[:], func=AF.Square), reads=[b_kcraw], writes=[b_sq128])
                        S.op("pe", lambda e: e.matmul(out=pk[:, 128:256], lhsT=bones[:], rhs=sq128[:], start=True, stop=True), reads=[b_bones, b_sq128], writes=[bpk])
                        S.op("dve", lambda e: e.tensor_scalar(out=rr[:, 0:128], in0=pk[:, 128:256], scalar1=1.0 / 64, scalar2=EPS, op0=ALU.mult, op1=ALU.add), reads=[bpk], writes=[b_rr])
                        S.op("act", lambda e: e.activation(out=rr[:, 0:128], in_=rr[:, 0:128], func=AF.Sqrt), reads=[b_rr], writes=[b_rr])
                        S.op("dve", lambda e: e.reciprocal(out=rr[:, 0:128], in_=rr[:, 0:128]), reads=[b_rr], writes=[b_rr])
                        S.op("dve", lambda e: e.scalar_tensor_tensor(out=kcn[:], in0=kcraw[:], scalar=kg[:, 0:1], in1=rr[:, 0:128], op0=ALU.mult, op1=ALU.mult),
                             reads=[b_kcraw, b_kg, b_rr], writes=[b_kcn])

                        S.dma("sp", sqt[:, 0:S_LEN], r32(zT_d[1280 + g0 * 64:1280 + g0 * 64 + 128, :]), reads=[b_zT[10 + pair]], writes=[b_sqt])
                        pbv, bpbv = P[7]
                        for j in range(32):
                            S.op("pe", lambda e: e.matmul(out=pbv[0:1, 0:64], lhsT=pevT[0:64, j:j + 1], rhs=wcv[0:64, j, :], start=(j == 0), stop=(j == 31)),
                                 reads=[b_pevT, b_wcv], writes=[bpbv])
                        S.op("dve", lambda e: e.tensor_copy(out=biasv[:], in_=pbv[0:1, 0:64]), reads=[bpbv], writes=[b_biasv])
                        for gi in range(2):
                            S.dma("sp", vcaug[:, gi, :], r32(vcaug0_d[:, :]), writes=[b_vcaug])
                        for hf in range(2):
                            hs = slice(hf * 64, (hf + 1) * 64)
                            pv, bpv = P[4 + hf]
                            for j in range(32):
                                S.op("pe", lambda e: e.matmul(out=pv[:, 0:64], lhsT=sqt[hs, j:j + 16 * 127 + 1:16], rhs=wcv[hs, j, :], start=(j == 0), stop=False),
                                     reads=[b_sqt, b_wcv], writes=[bpv])
                            S.op("pe", lambda e: e.matmul(out=pv[:, 0:64], lhsT=ones_r[0:1, :], rhs=biasv[0:1, :], start=False, stop=True),
                                 reads=[b_ones, b_biasv], writes=[bpv])
                            S.op("act", lambda e: e.copy(out=vcaug[:, hf, 0:64], in_=pv[:, 0:64]), reads=[bpv], writes=[b_vcaug])
                        for (vt, bvt, c0) in ((vs, b_vs, 0), (vw, b_vw, 256)):
                            S.dma("sp", vt[:], r32(vaug0_d[:, :, :, :]), writes=[bvt])
                            for gi in range(2):
                                S.dma("sp", vt[:, :, gi, 0:64],
                                      r32(ztm_d[:, c0 + (g0 + gi) * 64:c0 + (g0 + gi) * 64 + 64]).rearrange("(t p) d -> p t d", p=128),
                                      reads=b_ztm, writes=[bvt])

                    with phase() as sa:
                        oacc = sa("oacc", [128, NT, 512]); b_oacc = [Buf() for _ in range(NT)]
                        impacc = sa("impacc", [128, NT, 2, 32]); b_imp = [Buf() for _ in range(NT)]
                        negselT = sa("negselT", [128, 2, S_LEN], F32R); b_nst = [Buf() for _ in range(2)]
                        S.dma("sp", negselT[:].rearrange("p a b -> p (a b)"), r32(zeros_d[:, :]), writes=b_nst)
                        ptr_ = Ring([(sa(f"PT{i}", [128, 512], F32R), Buf()) for i in range(3)])
                        small = sa("small", [128, 8]); b_small = Buf()
                        coef = sa("coef", [128, 4]); b_coef = Buf()
                        top8 = sa("top8", [128, 8]); b_top8 = Buf()
                        impp = sa("impp", [128, 32]); b_impp = Buf()
                        negs = sa("negs", [128, 32]); b_negs = Buf()

                        def heads():
                            for i in range(4):
                                for hf in range(2):
                                    yield i, hf, 4 * (g0 + hf) + i, hf * 4 + i, slice(hf * 64, (hf + 1) * 64)

                        nb = 0
                        for (i, hf, h, hl, hs) in heads():
                            for qc in range(4):
                                pt, bp = P[nb % 2]
                                po, bpo = P[2 + nb % 2]
                                nb += 1
                                S.op("pe", lambda e: e.matmul(out=pt[:], lhsT=kcn[hs, :], rhs=qn[hs, i, qc * 512:(qc + 1) * 512], start=True, stop=False),
                                     reads=[b_kcn, b_qn[i]], writes=[bp])
                                S.op("pe", lambda e: e.matmul(out=pt[:], lhsT=identr[:], rhs=ccm[:, qc, :], start=False, stop=True),
                                     reads=[b_identr, b_ccm], writes=[bp])
                                PT, bPT = ptr_.next()
                                S.op("act", lambda e: e.activation(out=PT[:], in_=pt[:], func=AF.Exp), reads=[bp], writes=[bPT])
                                for j in range(4):
                                    S.op("pe", lambda e: e.matmul(out=po[:, j * 128:j * 128 + 98], lhsT=PT[:, j * 128:(j + 1) * 128], rhs=vcaug[:, hf, :], start=True, stop=True),
                                         reads=[bPT, b_vcaug], writes=[bpo])
                                pov = po[:].rearrange("p (j c) -> p j c", j=4)
                                S.op("dve", lambda e: e.tensor_scalar(out=small[:, 0:4], in0=pov[:, :, 64], scalar1=1e-30, scalar2=None, op0=ALU.add), reads=[bpo], writes=[b_small])
                                S.op("dve", lambda e: e.reciprocal(out=small[:, 0:4], in_=small[:, 0:4]), reads=[b_small], writes=[b_small])
                                S.op("dve", lambda e: e.tensor_tensor(out=coef[:], in0=small[:, 0:4], in1=gsig[:, qc * 4:(qc + 1) * 4, h * 3], op=ALU.mult),
                                     reads=[b_small, b_gsig], writes=[b_coef])
                                for j in range(4):
                                    tt = qc * 4 + j
                                    S.op("dve", lambda e: e.tensor_scalar(out=oacc[:, tt, hl * 64:(hl + 1) * 64], in0=po[:, j * 128:j * 128 + 64], scalar1=coef[:, j:j + 1], scalar2=None, op0=ALU.mult),
                                         reads=[bpo, b_coef], writes=[b_oacc[tt]])
                                    if i == 0:
                                        S.op("dve", lambda e: e.tensor_scalar(out=impacc[:, tt, hf, :], in0=po[:, j * 128 + 66:j * 128 + 98], scalar1=small[:, j:j + 1], scalar2=None, op0=ALU.mult),
                                             reads=[bpo, b_small], writes=[b_imp[tt]])
                                    else:
                                        S.op("dve", lambda e: e.scalar_tensor_tensor(out=impacc[:, tt, hf, :], in0=po[:, j * 128 + 66:j * 128 + 98], scalar=small[:, j:j + 1], in1=impacc[:, tt, hf, :], op0=ALU.mult, op1=ALU.add),
                                             reads=[bpo, b_small, b_imp[tt]], writes=[b_imp[tt]])
                        for hf in range(2):
                            for tt in range(NT):
                                S.op("dve", lambda e: e.tensor_tensor(out=impp[:], in0=impacc[:, tt, hf, :], in1=selv[:, tt, :], op=ALU.mult), reads=[b_imp[tt], b_selv], writes=[b_impp])
                                S.op("dve", lambda e: e.tensor_tensor(out=impp[:], in0=impp[:], in1=self_[:, tt, :], op=ALU.add), reads=[b_impp, b_self], writes=[b_impp])
                                S.op("dve", lambda e: e.max(out=top8[:], in_=impp[:]), reads=[b_impp], writes=[b_top8])
                                S.op("dve", lambda e: e.tensor_scalar(out=negs[:], in0=impp[:], scalar1=top8[:, 7:8], scalar2=NEG, op0=ALU.is_lt, op1=ALU.mult),
                                     reads=[b_impp, b_top8], writes=[b_negs])
                                pt, bp = P[6 + (tt // 4) % 2]
                                S.op("pe", lambda e: e.transpose(out=pt[0:32, (tt % 4) * 128:(tt % 4 + 1) * 128], in_=negs[:], identity=ident[:]),
                                     reads=[b_negs, b_ident], writes=[bp])
                                if tt % 4 == 3:
                                    S.op("act", lambda e: e.copy(out=negselT[0:32, hf, (tt // 4) * 512:(tt // 4 + 1) * 512], in_=pt[0:32, :]), reads=[bp], writes=[b_nst[hf]])
                        units = []
                        gi_ = 0
                        for (i, hf, h, hl, hs) in heads():
                            for qc in range(4):
                                for br in (1, 2):
                                    kts = list(range(0, 4 * qc + 4)) if br == 1 else list(range(max(0, 4 * qc - 4), 4 * qc + 4))
                                    for n, kt in enumerate(kts):
                                        units.append((i, hf, h, hl, hs, qc, br, n, kt, len(kts), gi_))
                                    gi_ += 1
                        sbank = {}
                        oTr = Ring([(sa(f"oT{i}", [66, 512]), Buf()) for i in range(2)])

                        def emit_S(ui):
                            (i, hf, h, hl, hs, qc, br, n, kt, nk, gix) = units[ui]
                            kn, b_kn = (kns, b_kns) if br == 1 else (knw, b_knw)
                            pt, bp = P[ui % 2]
                            sbank[ui] = (pt, bp)
                            S.op("pe", lambda e: e.matmul(out=pt[:], lhsT=kn[:, hf, kt * 128:(kt + 1) * 128], rhs=qn[:, i, qc * 512:(qc + 1) * 512], start=True, stop=False),
                                 reads=[b_kn, b_qn[i]], writes=[bp])
                            if br == 1:
                                diag = kt >= 4 * qc
                                S.op("pe", lambda e: e.matmul(out=pt[:], lhsT=esel[:, kt, :], rhs=negselT[:, hf, qc * 512:(qc + 1) * 512], start=False, stop=not diag),
                                     reads=[b_esel, b_nst[hf]], writes=[bp])
                                if diag:
                                    S.op("pe", lambda e: e.matmul(out=pt[:], lhsT=identr[:], rhs=cmask[:, kt - 4 * qc, :], start=False, stop=True),
                                         reads=[b_identr, b_cmask], writes=[bp])
                            else:
                                r = kt - (4 * qc - 4)
                                mk_, bmk = (wmask[:, r, :], b_wmask) if r < 4 else (cmask[:, r - 4, :], b_cmask)
                                S.op("pe", lambda e: e.matmul(out=pt[:], lhsT=identr[:], rhs=mk_, start=False, stop=True),
                                     reads=[b_identr, bmk], writes=[bp])

                        def emit_rest(ui):
                            (i, hf, h, hl, hs, qc, br, n, kt, nk, gix) = units[ui]
                            vt, bvt = (vs, b_vs) if br == 1 else (vw, b_vw)
                            pt, bp = sbank.pop(ui)
                            PT, bPT = ptr_.next()
                            S.op("act", lambda e: e.activation(out=PT[:], in_=pt[:], func=AF.Exp), reads=[bp], writes=[bPT])
                            pacc, bpacc = P[2 + gix % 2]
                            S.op("pe", lambda e: e.matmul(out=pacc[0:66, :], lhsT=vt[:, kt, hf, :], rhs=PT[:], start=(n == 0), stop=(n == nk - 1)),
                                 reads=[bPT, bvt], writes=[bpacc])
                            if n == nk - 1:
                                oT, boT = oTr.next()
                                S.op("dve", lambda e: e.tensor_copy(out=oT[:], in_=pacc[0:66, :]), reads=[bpacc], writes=[boT])
                                ptp, bptp = P[4 + gix % 2]
                                for j in range(4):
                                    S.op("pe", lambda e: e.transpose(out=ptp[:, j * 128:j * 128 + 66], in_=oT[:, j * 128:(j + 1) * 128], identity=ident[0:66, 0:66]),
                                         reads=[boT, b_ident], writes=[bptp])
                                for j in range(4):
                                    tt = qc * 4 + j
                                    pa, bpa = ptp[:, j * 128:(j + 1) * 128], bptp
                                    S.op("dve", lambda e: e.reciprocal(out=small[:, 4 + j:5 + j], in_=pa[:, 64:65]), reads=[bpa], writes=[b_small])
                                    S.op("dve", lambda e: e.tensor_tensor(out=small[:, 4 + j:5 + j], in0=small[:, 4 + j:5 + j], in1=gsig[:, tt, h * 3 + br:h * 3 + br + 1], op=ALU.mult),
                                         reads=[b_small, b_gsig], writes=[b_small])
                                    S.op("dve", lambda e: e.scalar_tensor_tensor(out=oacc[:, tt, hl * 64:(hl + 1) * 64], in0=pa[:, 0:64], scalar=small[:, 4 + j:5 + j], in1=oacc[:, tt, hl * 64:(hl + 1) * 64], op0=ALU.mult, op1=ALU.add),
                                         reads=[bpa, b_small, b_oacc[tt]], writes=[b_oacc[tt]])

                        emit_S(0)
                        for ui in range(len(units)):
                            if ui + 1 < len(units):
                                emit_S(ui + 1)
                            emit_rest(ui)
                        for tt in range(NT):
                            S.dma("pool", onsa_d[tt * 128:(tt + 1) * 128, pair * 512:(pair + 1) * 512], oacc[:, tt, :], reads=[b_oacc[tt]], writes=[b_onsa[tt][pair]])
            if lvl == 3:
                final_bufs += [b for bb in b_onsa for b in bb]

        if lvl >= 4:
            with phase() as sb:
                gonb = sb("gonb", [128, 1024]); b_gonb = Buf()
                S.dma("sp", gonb[:], gon_d[0:1, :].partition_broadcast(128), writes=[b_gonb])
                oring = Ring([(sb(f"ot{i}", [128, 1024]), Buf()) for i in range(2)])
                sqj = sb("sqjF", [128, 1024]); b_sqj = Buf()
                ss = sb("ssF", [128, 1]); b_ss = Buf()
                stgF = Ring([(sb(f"stgF{i}", [128, 8, 512]), Buf()) for i in range(2)])
                st_, bs = None, None
                for tt in range(NT):
                    ot, bo = oring.next()
                    S.dma("sp", ot[:], onsa_d[tt * 128:(tt + 1) * 128, :], reads=b_onsa[tt], writes=[bo])
                    S.op("act", lambda e: e.activation(out=sqj[:], in_=ot[:], func=AF.Square, accum_out=ss[:]), reads=[bo], writes=[b_sqj, b_ss])
                    S.op("dve", lambda e: e.tensor_scalar(out=ss[:], in0=ss[:], scalar1=1.0 / 1024, scalar2=EPS, op0=ALU.mult, op1=ALU.add), reads=[b_ss], writes=[b_ss])
                    S.op("act", lambda e: e.activation(out=ss[:], in_=ss[:], func=AF.Sqrt), reads=[b_ss], writes=[b_ss])
                    S.op("dve", lambda e: e.reciprocal(out=ss[:], in_=ss[:]), reads=[b_ss], writes=[b_ss])
                    S.op("dve", lambda e: e.scalar_tensor_tensor(out=ot[:], in0=ot[:], scalar=ss[:, 0:1], in1=gonb[:], op0=ALU.mult, op1=ALU.mult),
                         reads=[bo, b_ss, b_gonb], writes=[bo])
                    if tt % 4 == 0:
                        st_, bs = stgF.next()
                    for half in range(2):
                        pt, bp = P[(2 * tt + half) % 4]
                        for j in range(4):
                            ft = half * 4 + j
                            S.op("pe", lambda e: e.transpose(out=pt[:, j * 128:(j + 1) * 128], in_=ot[:, ft * 128:(ft + 1) * 128], identity=ident[:]),
                                 reads=[bo, b_ident], writes=[bp])
                        S.op("act", lambda e: e.copy(out=st_[:, half * 4:(half + 1) * 4, (tt % 4) * 128:(tt % 4 + 1) * 128], in_=pt[:].rearrange("p (a b) -> p a b", a=4)),
                             reads=[bp], writes=[bs])
                    if tt % 4 == 3:
                        tq = tt // 4
                        S.dma("pool", onT_d[0:1024, tq * 512:(tq + 1) * 512].rearrange("(f p) t -> p f t", p=128), st_[:], reads=[bs], writes=b_onT[0:8])
            if lvl == 4:
                final_bufs += b_onT

        if lvl >= 5:
            with phase() as sb:
                onT = sb("onT", [128, 16, S_LEN], F32R); b_onTs = Buf()
                for kt in range(16):
                    S.dma("sp", onT[:, kt, :], r32(onT_d[kt * 128:(kt + 1) * 128, :]), reads=[b_onT[kt]], writes=[b_onTs])
                ws = WStream(mkwring(sb), [wsrc(wout_d, c) for c in range(D // WCOL)])
                g1ring = Ring([(sb(f"g1b{i}", [128, WCOL]), Buf()) for i in range(2)])
                xcr = Ring([(sb(f"xc{i}", [128, WCOL]), Buf()) for i in range(3)])
                ocr = Ring([(sb(f"oc{i}", [128, WCOL]), Buf()) for i in range(3)])
                pi = 0
                for c in range(D // WCOL):
                    wt, bw = ws.get(c)
                    g1b, bg1 = g1ring.next()
                    S.dma("sp", g1b[:], mod_d[0:1, 2 * D + c * WCOL:2 * D + (c + 1) * WCOL].partition_broadcast(128), reads=[b_mod], writes=[bg1])
                    for tt in range(NT):
                        pt, bp = P[pi % 4]; pi += 1
                        for kt in range(16):
                            S.op("pe", lambda e: e.matmul(out=pt[:, 0:WCOL], lhsT=onT[:, kt, tt * 128:(tt + 1) * 128], rhs=wt[:, kt, :], start=(kt == 0), stop=(kt == 15)),
                                 reads=[b_onTs, bw], writes=[bp])
                        xt, bx = xcr.next()
                        S.dma("sp", xt[:], x_d[tt * 128:(tt + 1) * 128, c * WCOL:(c + 1) * WCOL], writes=[bx])
                        ot, bo = ocr.next()
                        S.op("dve", lambda e: e.tensor_tensor(out=ot[:], in0=pt[:, 0:WCOL], in1=g1b[:], op=ALU.mult), reads=[bp, bg1], writes=[bo])
                        S.op("dve", lambda e: e.tensor_tensor(out=ot[:], in0=ot[:], in1=xt[:], op=ALU.add), reads=[bo, bx], writes=[bo])
                        S.dma("pool", x1_d[tt * 128:(tt + 1) * 128, c * WCOL:(c + 1) * WCOL], ot[:], reads=[bo], writes=[b_x1[tt]])
            if lvl == 5:
                final_bufs += b_x1

        if lvl >= 6:
            with phase() as sbo:
                tokid = sbo("tokid", [128, NT, 2], I32); b_tokid = Buf()
                S.dma("sp", tokid[:], tokid_d[:, :, :], writes=[b_tokid])
                sidx = sbo("sidx", [128, NT, 4], I32); b_sidx = Buf()
                gk = sbo("gk", [128, NT, 4]); b_gk = Buf()
                with phase() as sb:
                    def ld(name, shp, src, dt=F32, reads=()):
                        t = sb(name, shp, dt); b = Buf()
                        S.dma("sp", t[:], src, reads=list(reads), writes=[b])
                        return t, b
                    maskall = sb("maskall", [128, NT, NE], F32R); b_mask = [Buf() for _ in range(NT)]
                    posall = sb("posall", [128, NT, NE]); b_pos = [Buf() for _ in range(NT)]
                    oobt = sb("oobt", [128, 2 * NE * CAP // 128], I32); b_oobt = Buf()
                    S.dma("sp", oobt[:], oobidx_d[:, :], writes=[b_oobt])
                    S.dma("sp", tokidx_d.rearrange("(p j) o -> p (j o)", p=128), oobt[:], reads=[b_oobt], writes=[b_tokidx])
                    A2b, b_A2b = ld("A2b", [128, D], mod_d[0:1, 4 * D:5 * D].partition_broadcast(128), reads=[b_mod])
                    g2b, b_g2b = ld("g2b", [128, D], g2_d[0:1, :].partition_broadcast(128))
                    B2b, b_B2b = ld("B2b", [128, D], mod_d[0:1, 3 * D:4 * D].partition_broadcast(128), reads=[b_mod])
                    S.op("dve", lambda e: e.scalar_tensor_tensor(out=A2b[:], in0=A2b[:], scalar=1.0, in1=g2b[:], op0=ALU.add, op1=ALU.mult),
                         reads=[b_A2b, b_g2b], writes=[b_A2b])
                    wr, b_wr = ld("wr", [128, 16, NE], wr_d[:, :, :])
                    brb, b_brb = ld("brb", [128, NE], br_d[0:1, :].partition_broadcast(128))
                    triu, b_triu = ld("triu", [128, 128], r32(triu_d[:, :]), F32R)
                    ones_r, b_ones = ld("ones_r", [128, 128], r32(ones_d[:, :]), F32R)
                    ebase, b_ebase = ld("ebase", [128, NE], ebase_d[:, :])
                    x1r = Ring([(sb(f"x1t{i}", [128, D]), Buf()) for i in range(2)])
                    sqj = sb("sqjH", [128, D]); b_sqj = Buf()
                    ss = sb("ssH", [128, 1]); b_ss = Buf()
                    h2f = sb("h2f", [128, D]); b_h2f = Buf()
                    h2T = sb("h2T", [128, 16, 128]); b_h2T = Buf()
                    lg = sb("lg", [128, NE]); b_lg = Buf()
                    t8 = sb("t8", [128, 8]); b_t8 = Buf()
                    ex = sb("ex", [128, NE]); b_ex = Buf()
                    nm = sb("nm", [128, 1]); b_nm = Buf()
                    gd = sb("gd", [128, 1]); b_gd = Buf()
                    e4 = sb("e4", [128, 4]); b_e4 = Buf()
                    slot = sb("slot", [128, NE]); b_slot = Buf()
                    oh = sb("oh", [128, NE]); b_oh = Buf()
                    sf = sb("sf", [128, 4]); b_sf = Buf()
                    rinfo = sb("rinfo", [128, NT, 8]); b_ri = Buf()
                    for tt in range(NT):
                        xt, bx = x1r.next()
                        S.dma("sp", xt[:], x1_d[tt * 128:(tt + 1) * 128, :], reads=[b_x1[tt]], writes=[bx])
                        S.op("act", lambda e: e.activation(out=sqj[:], in_=xt[:], func=AF.Square, accum_out=ss[:]), reads=[bx], writes=[b_sqj, b_ss])
                        S.op("dve", lambda e: e.tensor_scalar(out=ss[:], in0=ss[:], scalar1=1.0 / D, scalar2=EPS, op0=ALU.mult, op1=ALU.add), reads=[b_ss], writes=[b_ss])
                        S.op("act", lambda e: e.activation(out=ss[:], in_=ss[:], func=AF.Sqrt), reads=[b_ss], writes=[b_ss])
                        S.op("dve", lambda e: e.reciprocal(out=ss[:], in_=ss[:]), reads=[b_ss], writes=[b_ss])
                        S.op("dve", lambda e: e.scalar_tensor_tensor(out=h2f[:], in0=xt[:], scalar=ss[:, 0:1], in1=A2b[:], op0=ALU.mult, op1=ALU.mult),
                             reads=[bx, b_ss, b_A2b], writes=[b_h2f])
                        S.op("dve", lambda e: e.tensor_tensor(out=h2f[:], in0=h2f[:], in1=B2b[:], op=ALU.add), reads=[b_h2f, b_B2b], writes=[b_h2f])
                        S.dma("pool", h2_d[tt * 128:(tt + 1) * 128, :], h2f[:], reads=[b_h2f], writes=[b_h2d[tt]])
                        for g in range(4):
                            pt, bp = P[g]
                            for j in range(4):
                                dc = g * 4 + j
                                S.op("pe", lambda e: e.transpose(out=pt[:, j * 128:(j + 1) * 128], in_=h2f[:, dc * 128:(dc + 1) * 128], identity=ident[:]),
                                     reads=[b_h2f, b_ident], writes=[bp])
                            if g % 2 == 0:
                                S.op("act", lambda e: e.copy(out=h2T[:, g * 4:(g + 1) * 4, :], in_=pt[:].rearrange("p (a b) -> p a b", a=4)), reads=[bp], writes=[b_h2T])
                            else:
                                S.op("dve", lambda e: e.tensor_copy(out=h2T[:, g * 4:(g + 1) * 4, :], in_=pt[:].rearrange("p (a b) -> p a b", a=4)), reads=[bp], writes=[b_h2T])
                        pl, bpl = P[4]
                        for dc in range(16):
                            S.op("pe", lambda e: e.matmul(out=pl[:, 0:NE], lhsT=h2T[:, dc, :], rhs=wr[:, dc, :], start=(dc == 0), stop=(dc == 15)),
                                 reads=[b_h2T, b_wr], writes=[bpl])
                        S.op("dve", lambda e: e.tensor_tensor(out=lg[:], in0=pl[:, 0:NE], in1=brb[:], op=ALU.add), reads=[bpl, b_brb], writes=[b_lg])
                        S.op("dve", lambda e: e.max(out=t8[:], in_=lg[:]), reads=[b_lg], writes=[b_t8])
                        S.op("dve", lambda e: e.tensor_scalar(out=maskall[:, tt, :], in0=lg[:], scalar1=t8[:, 3:4], scalar2=None, op0=ALU.is_ge),
                             reads=[b_lg, b_t8], writes=[b_mask[tt]])
                        S.op("dve", lambda e: e.tensor_scalar(out=nm[:], in0=t8[:, 0:1], scalar1=-1.0, scalar2=None, op0=ALU.mult), reads=[b_t8], writes=[b_nm])
                        S.op("act", lambda e: e.activation(out=e4[:], in_=t8[:, 0:4], func=AF.Exp, bias=nm[:, 0:1], accum_out=gd[:]), reads=[b_t8, b_nm], writes=[b_e4, b_gd])
                        S.op("dve", lambda e: e.reciprocal(out=gd[:], in_=gd[:]), reads=[b_gd], writes=[b_gd])
                        S.op("dve", lambda e: e.tensor_scalar(out=gk[:, tt, :], in0=e4[:], scalar1=gd[:, 0:1], scalar2=None, op0=ALU.mult), reads=[b_e4, b_gd], writes=[b_gk])
                        pp, bpp = P[5 + tt % 2]
                        for t2 in range(tt):
                            S.op("pe", lambda e: e.matmul(out=pp[:, 0:NE], lhsT=ones_r[:], rhs=maskall[:, t2, :], start=(t2 == 0), stop=False),
                                 reads=[b_ones, b_mask[t2]], writes=[bpp])
                        S.op("pe", lambda e: e.matmul(out=pp[:, 0:NE], lhsT=triu[:], rhs=maskall[:, tt, :], start=(tt == 0), stop=True),
                             reads=[b_triu, b_mask[tt]], writes=[bpp])
                        S.op("act", lambda e: e.copy(out=posall[:, tt, :], in_=pp[:, 0:NE]), reads=[bpp], writes=[b_pos[tt]])
                        S.op("dve", lambda e: e.tensor_tensor(out=slot[:], in0=posall[:, tt, :], in1=ebase[:], op=ALU.add), reads=[b_pos[tt], b_ebase], writes=[b_slot])
                        for k in range(4):
                            S.op("dve", lambda e: e.tensor_scalar(out=oh[:], in0=lg[:], scalar1=t8[:, k:k + 1], scalar2=None, op0=ALU.is_equal), reads=[b_lg, b_t8], writes=[b_oh])
                            S.op("dve", lambda e: e.tensor_tensor(out=oh[:], in0=oh[:], in1=slot[:], op=ALU.mult), reads=[b_oh, b_slot], writes=[b_oh])
                            S.op("dve", lambda e: e.reduce_sum(out=sf[:, k:k + 1], in_=oh[:], axis=mybir.AxisListType.X), reads=[b_oh], writes=[b_sf])
                        S.op("dve", lambda e: e.tensor_copy(out=sidx[:, tt, :], in_=sf[:]), reads=[b_sf], writes=[b_sidx])
                        for k in range(4):
                            S.dma("pool", None, None, reads=[b_sidx, b_tokid], writes=[b_tokidx],
                                  fn=lambda e: e.indirect_dma_start(out=tokidx_d[:, :], out_offset=bass.IndirectOffsetOnAxis(ap=sidx[:, tt, k:k + 1], axis=0),
                                                                    in_=tokid[:, tt, :], in_offset=None,
                                                                    bounds_check=reg_slot, oob_is_err=False))
                        if dbg:
                            S.op("dve", lambda e: e.tensor_copy(out=rinfo[:, tt, 0:4], in_=sf[:]), reads=[b_sf], writes=[b_ri])
                            S.op("dve", lambda e: e.tensor_copy(out=rinfo[:, tt, 4:8], in_=gk[:, tt, :]), reads=[b_gk], writes=[b_ri])
                    if dbg:
                        S.dma("sp", rinfo_d.rearrange("(t p) c -> p t c", p=128), rinfo[:], reads=[b_ri], writes=[b_rinfo])
                if lvl == 6:
                    final_bufs += [b_rinfo]

                if lvl >= 7:
                    with phase() as sb:
                        b1r = Ring([(sb(f"b1c{i}", [128, 32]), Buf()) for i in range(2)])
                        ones_r = sb("ones_rE", [1, 128], F32R); b_ones = Buf()
                        S.dma("sp", ones_r[:], r32(ones_d[0:1, :]), writes=[b_ones])
                        xbT = sb("xbT", [128, 16, CAP], F32R); b_xbT = [Buf() for _ in range(NST)]
                        tact = sb("tact", [128, 16, CAP], F32R); b_tact = [Buf() for _ in range(16)]
                        ug = sb("ug", [128, 512]); b_ug = Buf()
                        sg = sb("sg", [128, 512]); b_sg = Buf()
                        idxr = Ring([(sb(f"idxe{i}", [128, NST], I32), Buf()) for i in range(2)])
                        xbr = Ring([(sb(f"xb{i}", [128, D]), Buf()) for i in range(2)])
                        for xt_, bx_ in xbr.items:
                            S.op("pool", lambda e: e.memset(xt_[:], 0.0), writes=[bx_])
                        g2ring = Ring([(sb(f"g2b{i}", [128, WCOL]), Buf()) for i in range(2)])
                        b2ring = Ring([(sb(f"b2r{i}", [1, WCOL], F32R), Buf()) for i in range(4)])
                        yring = Ring([(sb(f"yst{i}", [128, WCOL]), Buf()) for i in range(3)])
                        srcs = []
                        for e_ in range(NE):
                            srcs += [wsrc(we1_d[e_], c) for c in range(2 * D // WCOL)]
                            srcs += [wsrc(we2_d[e_], c) for c in range(D // WCOL)]
                        ws = WStream(mkwring(sb, 3), srcs, ahead=2)
                        wi = 0
                        pi = 0

                        def load_idx(e_):
                            idxe, bidx = idxr.next()
                            S.dma("sp", None, None, reads=[b_tokidx], writes=[bidx],
                                  fn=lambda e: e.dma_start(out=idxe[:], in_=tokidx_d[e_ * CAP:(e_ + 1) * CAP, 0:1].rearrange("(s p) o -> p (s o)", p=128),
                                                           allow_slow_non_contiguous=True))
                            return idxe, bidx

                        def gather(idxe, bidx, st):
                            xb, bxb = xbr.next()
                            S.dma("pool", None, None, reads=[bidx] + b_h2d, writes=[bxb],
                                  fn=lambda e: e.indirect_dma_start(out=xb[:, :], out_offset=None, in_=h2_d[:, :],
                                                                    in_offset=bass.IndirectOffsetOnAxis(ap=idxe[:, st:st + 1], axis=0),
                                                                    bounds_check=reg_tok, oob_is_err=False))
                            return xb, bxb

                        def transp(xb, bxb, st):
                            for g in range(4):
                                pt, bp = P[4 + g]
                                for j in range(4):
                                    dc = g * 4 + j
                                    S.op("pe", lambda e: e.transpose(out=pt[:, j * 128:(j + 1) * 128], in_=xb[:, dc * 128:(dc + 1) * 128], identity=ident[:]),
                                         reads=[bxb, b_ident], writes=[bp])
                                if g % 2 == 0:
                                    S.op("act", lambda e: e.copy(out=xbT[:, g * 4:(g + 1) * 4, st * 128:(st + 1) * 128], in_=pt[:].rearrange("p (a b) -> p a b", a=4)),
                                         reads=[bp], writes=[b_xbT[st]])
                                else:
                                    S.op("dve", lambda e: e.tensor_copy(out=xbT[:, g * 4:(g + 1) * 4, st * 128:(st + 1) * 128], in_=pt[:].rearrange("p (a b) -> p a b", a=4)),
                                         reads=[bp], writes=[b_xbT[st]])

                        idx0, bidx0 = load_idx(0)
                        for st in range(NST):
                            xb, bxb = gather(idx0, bidx0, st)
                            transp(xb, bxb, st)
                        for e_ in range(NE):
                            b1c, b_b1c = b1r.next()
                            S.dma("sp", b1c[:], b1c_d[:, e_, :], writes=[b_b1c])
                            for c in range(2 * D // WCOL):
                                wt, bw = ws.get(wi); wi += 1
                                for ftl in range(2):
                                    F = c * 2 + ftl
                                    for hv in range(2):
                                        cs = slice(hv * 512, (hv + 1) * 512)
                                        pt, bp = P[pi % 4]; pi += 1
                                        for kt in range(16):
                                            S.op("pe", lambda e: e.matmul(out=pt[:], lhsT=wt[:, kt, ftl * 128:(ftl + 1) * 128], rhs=xbT[:, kt, cs], start=(kt == 0), stop=(kt == 15)),
                                                 reads=[bw] + b_xbT[hv * 4:(hv + 1) * 4], writes=[bp])
                                        S.op("dve", lambda e: e.tensor_scalar(out=ug[:], in0=pt[:], scalar1=b1c[:, F:F + 1], scalar2=7.0, op0=ALU.add, op1=ALU.min),
                                             reads=[bp, b_b1c], writes=[b_ug])
                                        if F < 16:
                                            S.op("act", lambda e: e.activation(out=sg[:], in_=ug[:], func=AF.Sigmoid, scale=1.702), reads=[b_ug], writes=[b_sg])
                                            S.op("dve", lambda e: e.tensor_tensor(out=tact[:, F, cs], in0=ug[:], in1=sg[:], op=ALU.mult), reads=[b_ug, b_sg], writes=[b_tact[F]])
                                        else:
                                            Fl = F - 16
                                            S.op("dve", lambda e: e.tensor_scalar(out=ug[:], in0=ug[:], scalar1=-7.0, scalar2=1.0, op0=ALU.max, op1=ALU.add), reads=[b_ug], writes=[b_ug])
                                            S.op("dve", lambda e: e.tensor_tensor(out=tact[:, Fl, cs], in0=f32(tact[:, Fl, cs]), in1=ug[:], op=ALU.mult), reads=[b_ug, b_tact[Fl]], writes=[b_tact[Fl]])
                            if e_ + 1 < NE:
                                idxn, bidxn = load_idx(e_ + 1)
                            for c in range(D // WCOL):
                                wt, bw = ws.get(wi); wi += 1
                                if e_ + 1 < NE:
                                    xbn, bxbn = gather(idxn, bidxn, c)
                                g2b_, bg2 = g2ring.next()
                                S.dma("sp", g2b_[:], mod_d[0:1, 5 * D + c * WCOL:5 * D + (c + 1) * WCOL].partition_broadcast(128), reads=[b_mod], writes=[bg2])
                                b2r, bb2 = b2ring.next()
                                S.dma("sp", b2r[:], r32(be2_d[e_:e_ + 1, c * WCOL:(c + 1) * WCOL]), writes=[bb2])
                                for st in range(NST):
                                    pt, bp = P[pi % 4]; pi += 1
                                    for kt in range(16):
                                        S.op("pe", lambda e: e.matmul(out=pt[:, 0:WCOL], lhsT=tact[:, kt, st * 128:(st + 1) * 128], rhs=wt[:, kt, :], start=(kt == 0), stop=False),
                                             reads=[b_tact[kt], bw], writes=[bp])
                                    S.op("pe", lambda e: e.matmul(out=pt[:, 0:WCOL], lhsT=ones_r[0:1, :], rhs=b2r[0:1, :], start=False, stop=True),
                                         reads=[b_ones, bb2], writes=[bp])
                                    yt, by = yring.next()
                                    S.op("dve", lambda e: e.tensor_tensor(out=yt[:], in0=pt[:, 0:WCOL], in1=g2b_[:], op=ALU.mult), reads=[bp, bg2], writes=[by])
                                    r0 = e_ * CAP + st * 128
                                    S.dma("pool", Y_d[r0:r0 + 128, c * WCOL:(c + 1) * WCOL], yt[:], reads=[by], writes=[b_Y[e_][st]])
                                if e_ + 1 < NE:
                                    transp(xbn, bxbn, c)
                    if lvl == 7:
                        final_bufs += [b for bb in b_Y for b in bb]

                if lvl >= 8:
                    with phase() as sb:
                        x1r = Ring([(sb(f"x1c{i}", [128, D]), Buf()) for i in range(2)])
                        ykr = Ring([(sb(f"yk{i}", [128, D]), Buf()) for i in range(4)])
                        allY = [b for bb in b_Y for b in bb]
                        for tt in range(NT):
                            xt, bx = x1r.next()
                            S.dma("sp", xt[:], x1_d[tt * 128:(tt + 1) * 128, :], reads=[b_x1[tt]], writes=[bx])
                            for k in range(4):
                                yk, byk = ykr.next()
                                S.dma("pool", None, None, reads=allY + [b_sidx], writes=[byk],
                                      fn=lambda e: e.indirect_dma_start(out=yk[:, :], out_offset=None, in_=Y_d[:, :],
                                                                        in_offset=bass.IndirectOffsetOnAxis(ap=sidx[:, tt, k:k + 1], axis=0),
                                                                        bounds_check=reg_slot, oob_is_err=False))
                                S.op("dve", lambda e: e.scalar_tensor_tensor(out=xt[:], in0=yk[:], scalar=gk[:, tt, k:k + 1], in1=xt[:], op0=ALU.mult, op1=ALU.add),
                                     reads=[byk, b_gk, bx], writes=[bx])
                            S.dma("sp", out_d[tt * 128:(tt + 1) * 128, :], xt[:], reads=[bx], writes=[b_out[tt]])
                    final_bufs += b_out

        S.finish(final_bufs)
        S.barrier()
    return nc


def _consts():
    c = {}
    c["ident"] = np.eye(128, dtype=np.float32)
    bo = np.zeros((128, 128), np.float32); bo[:64, :64] = 1; bo[64:, 64:] = 1
    c["bones"] = bo
    c["ones"] = np.ones((128, 128), np.float32)
    p = np.arange(128)[:, None]; col = np.arange(512)[None, :]
    cm = np.zeros((128, 4, 512), np.float32); wm = np.zeros((128, 4, 512), np.float32); cc = np.zeros((128, 4, 512), np.float32)
    for r in range(4):
        cm[:, r, :] = np.where(r * 128 + p <= col, 0.0, NEG)
        wm[:, r, :] = np.where(p + r * 128 > col, 0.0, NEG)
        cc[:, r, :] = np.where(16 * p + 31 <= r * 512 + col, 0.0, NEG)
    c["cmask"], c["wmask"], c["ccm"] = cm, wm, cc
    es = np.zeros((128, 16, 128), np.float32)
    for kt in range(16):
        for pp in range(128):
            es[2 * kt + pp // 64, kt, pp] = 1.0
    c["esel"] = es
    t = np.arange(S_LEN); qb = t // 64; j = np.arange(32)
    valid = j[None, :] <= qb[:, None]
    forced = (j[None, :] == 0) | (j[None, :] == qb[:, None]) | (j[None, :] == qb[:, None] - 1)
    V = (valid & ~forced).astype(np.float32)
    Fm = np.where(forced, 1e30, np.where(valid, 0.0, -1e30)).astype(np.float32)
    c["selv"] = np.ascontiguousarray(V.reshape(16, 128, 32).transpose(1, 0, 2))
    c["self"] = np.ascontiguousarray(Fm.reshape(16, 128, 32).transpose(1, 0, 2))
    cs = np.arange(128) * 16; ss = np.arange(32) * 64
    ov = ((cs[:, None] < ss[None, :] + 64) & (cs[:, None] + 32 > ss[None, :])).astype(np.float32)
    vc0 = np.zeros((128, 98), np.float32); vc0[:, 64] = 1.0; vc0[:, 66:98] = ov
    c["vcaug0"] = vc0
    va0 = np.zeros((128, NT, 2, 66), np.float32); va0[..., 64] = 1.0
    c["vaug0"] = va0
    c["triu"] = np.triu(np.ones((128, 128), np.float32), k=1)
    c["zeros"] = np.zeros((128, 2 * S_LEN), np.float32)
    tk = (np.arange(NT, dtype=np.int32)[None, :] * 128 + np.arange(128, dtype=np.int32)[:, None]).astype(np.int32)
    c["tokid"] = np.ascontiguousarray(np.stack([tk, tk], axis=-1))
    c["oobidx"] = np.full((128, 2 * NE * CAP // 128), 4095, np.int32)
    c["iotas"] = np.broadcast_to(np.arange(CAP, dtype=np.float32)[None, :], (128, CAP)).copy()
    c["ebase"] = np.broadcast_to((np.arange(NE, dtype=np.float32) * CAP)[None, :], (128, NE)).copy()
    return c


def _cols(v, n=None):
    v = np.asarray(v, np.float32).reshape(-1, 128)
    return np.ascontiguousarray(v.T)


def prep_shared(inp):
    f = lambda k: np.asarray(inp[k], np.float32)[0]
    sh = dict(_consts())
    w_in = f("w_in")
    sh["wA"] = np.ascontiguousarray(np.concatenate([w_in[:, 0:1024], w_in[:, 1024:1280], w_in[:, 1280:1536], w_in[:, 1536:1792],
                                                    w_in[:, 2048:2304], w_in[:, 2608:3632], w_in[:, 3632:4656]], axis=1))
    wB = np.zeros((D, 768), np.float32)
    wB[:, 0:256] = w_in[:, 1792:2048]; wB[:, 256:512] = w_in[:, 2304:2560]; wB[:, 512:560] = w_in[:, 2560:2608]
    sh["wB"] = wB
    sh["w_ada"] = f("w_ada"); sh["b_ada"] = f("b_ada").reshape(1, -1)
    sh["g1c"] = _cols(f("g_norm1"))
    wck = f("w_cmp_k").reshape(32, 64, 64).transpose(1, 0, 2)
    wcv = f("w_cmp_v").reshape(32, 64, 64).transpose(1, 0, 2)
    wbd = np.zeros((128, 32, 128), np.float32)
    wbd[0:64, :, 0:64] = wck; wbd[64:128, :, 64:128] = wck
    sh["wck"] = wbd
    sh["wcv"] = np.ascontiguousarray(np.concatenate([wcv, wcv], axis=0))
    pek = f("pe_cmp_k").T; pev = f("pe_cmp_v").T
    pek = np.concatenate([pek, pek], axis=0)
    sh["pek2"] = np.ascontiguousarray(np.stack([pek, pek], axis=-1))
    sh["pevT"] = np.ascontiguousarray(np.concatenate([pev, pev], axis=0))
    qg = f("q_gain"); sh["qg"] = np.concatenate([qg, qg]).reshape(128, 1).copy()
    kgn = f("k_gain").T; sh["kg"] = np.ascontiguousarray(np.concatenate([kgn, kgn], axis=0))
    sh["cw"] = np.ascontiguousarray(f("conv_w").reshape(4, 8, 128).transpose(2, 1, 0))
    sh["cb"] = _cols(f("conv_b"))
    for nm, key in (("wrg", "w_rg"), ("wig", "w_ig")):
        w = f(key)
        bd = np.zeros((128, 8, 128), np.float32)
        for ct in range(8):
            bd[0:64, ct, 0:64] = w[2 * ct]; bd[64:128, ct, 64:128] = w[2 * ct + 1]
        sh[nm] = bd
    sh["brg"] = _cols(f("b_rg").reshape(-1)); sh["big"] = _cols(f("b_ig").reshape(-1))
    sh["lam"] = _cols(f("lru_lambda"))
    sh["gon"] = f("g_out_nsa").reshape(1, -1); sh["gol"] = _cols(f("g_out_lru"))
    sh["w_out"] = f("w_out"); sh["g2"] = f("g_norm2").reshape(1, -1)
    sh["wr"] = np.ascontiguousarray(f("w_router").reshape(16, 128, NE).transpose(1, 0, 2))
    sh["br"] = f("b_router").reshape(1, -1)
    sh["w_e1"] = f("w_e1"); sh["w_e2"] = f("w_e2")
    sh["b1c"] = np.ascontiguousarray(f("b_e1").reshape(NE, 32, 128).transpose(2, 0, 1))
    sh["b_e2"] = f("b_e2")
    return sh


def core_inputs(inp, sh, b):
    m = dict(sh)
    m["x"] = np.ascontiguousarray(np.asarray(inp["x"], np.float32)[b])
    m["csil"] = _cols(np.asarray(inp["c"], np.float32)[b])
    return m


_NC_CACHE = {}


def kernel(**inputs):
    sh = prep_shared(inputs)
    if "nc" not in _NC_CACHE:
        _NC_CACHE["nc"] = build_nc("all", False)
    nc = _NC_CACHE["nc"]
    in_maps = [core_inputs(inputs, sh, b) for b in range(8)]
    res = run_bass_kernel_spmd(nc, in_maps, core_ids=list(range(8)))
    return np.stack([np.asarray(r["out"], np.float32) for r in res.results], axis=0)
```
